# Optimizing a Trainium2 kernel written in Bass

```python
import math
import jax, jax.numpy as jnp
from jax import lax
import numpy as np

D_MODEL = 1024
BATCH = 8
SEQ = 2048
DEPTH = 2

CHUNK = 64
Q_BLOCK = 128
N_MIXERS = 2
DIFF_HEADS = 8
DIFF_HEAD_DIM = D_MODEL // (2 * DIFF_HEADS)
SB_HEADS = 16
SB_HEAD_DIM = D_MODEL // SB_HEADS
N_GROUPS = 4
EXPERTS_PER_GROUP = 8
N_EXPERTS = N_GROUPS * EXPERTS_PER_GROUP
TOP_K = 2
EXPERT_HIDDEN = D_MODEL // 2
MOE_BLOCK = 128
RMS_EPS = 1e-6
SUBLN_EPS = 1e-5

kernel_name = "hybrid_diffattn_stickbreak_hmoe_adaln"


def rms_norm(x, g, eps=RMS_EPS):
    xf = x.astype(jnp.float32)
    y = xf * lax.rsqrt(jnp.mean(xf * xf, axis=-1, keepdims=True) + eps)
    return (y * g.astype(jnp.float32)).astype(x.dtype)


def alibi_slopes(n_heads):
    return jnp.exp2(-8.0 * jnp.arange(1, n_heads + 1, dtype=jnp.float32) / n_heads)


def diff_attention(h, w_in, w_out, lq1, lk1, lq2, lk2, subln_g, lambda_init):
    B, S, D = h.shape
    H, d = DIFF_HEADS, DIFF_HEAD_DIM
    q, k, v = jnp.split(h @ w_in, 3, axis=-1)
    q = q.reshape(B, S, H, 2, d)
    k = k.reshape(B, S, H, 2, d)
    v = v.reshape(B, S, H, 2 * d)
    lam = (jnp.exp(jnp.sum(lq1.astype(jnp.float32) * lk1.astype(jnp.float32)))
           - jnp.exp(jnp.sum(lq2.astype(jnp.float32) * lk2.astype(jnp.float32)))
           + lambda_init)
    slopes = alibi_slopes(H)[None, :, None, None, None]
    scale = 1.0 / math.sqrt(d)
    pos = jnp.arange(S, dtype=jnp.int32)
    outs = []
    for i in range(S // Q_BLOCK):
        q0, q1 = i * Q_BLOCK, (i + 1) * Q_BLOCK
        qb, kb, vb = q[:, q0:q1], k[:, :q1], v[:, :q1]
        tq, tk = pos[q0:q1], pos[:q1]
        s = jnp.einsum('bqhmd,bkhmd->bhmqk', qb, kb).astype(jnp.float32) * scale
        dist = jnp.abs(tq[:, None] - tk[None, :]).astype(jnp.float32)
        allowed = (tk[None, :] // CHUNK) <= (tq[:, None] // CHUNK)
        s = jnp.where(allowed, s - slopes * dist, -jnp.inf)
        p = jax.nn.softmax(s, axis=-1)
        a = p[:, :, 0] - lam * p[:, :, 1]
        outs.append(jnp.einsum('bhqk,bkhe->bqhe', a.astype(vb.dtype), vb))
    o = jnp.concatenate(outs, axis=1)
    o = rms_norm(o, subln_g, SUBLN_EPS) * (1.0 - lambda_init)
    return o.reshape(B, S, D) @ w_out


def stick_breaking_attention(h, w_in, w_out):
    B, S, D = h.shape
    H, d = SB_HEADS, SB_HEAD_DIM
    q, k, v = jnp.split(h @ w_in, 3, axis=-1)
    q = q.reshape(B, S, H, d)
    k = k.reshape(B, S, H, d)
    v = v.reshape(B, S, H, d)
    scale = 1.0 / math.sqrt(d)
    pos = jnp.arange(S, dtype=jnp.int32)
    outs = []
    for i in range(S // Q_BLOCK):
        q0, q1 = i * Q_BLOCK, (i + 1) * Q_BLOCK
        qb, kb, vb = q[:, q0:q1], k[:, :q1], v[:, :q1]
        tq, tk = pos[q0:q1], pos[:q1]
        z = jnp.einsum('bqhd,bkhd->bhqk', qb, kb).astype(jnp.float32) * scale
        strict = tk[None, :] < tq[:, None]
        log_fail = jnp.where(strict, jax.nn.log_sigmoid(-z), 0.0)
        between = lax.cumsum(log_fail, axis=log_fail.ndim - 1, reverse=True) - log_fail
        a = jnp.where(strict, jnp.exp(jax.nn.log_sigmoid(z) + between), 0.0)
        outs.append(jnp.einsum('bhqk,bkhd->bqhd', a.astype(vb.dtype), vb))
    o = jnp.concatenate(outs, axis=1)
    return o.reshape(B, S, D) @ w_out


def expert_dispatch(xf, expert_ids, weights, w_gate, w_up, w_down):
    N, D = xf.shape
    NK = expert_ids.shape[0]
    n_slots = NK + N_EXPERTS * MOE_BLOCK
    n_blocks = n_slots // MOE_BLOCK
    token_ids = jnp.arange(NK, dtype=jnp.int32) // TOP_K
    order = jnp.argsort(expert_ids)
    e_sorted = expert_ids[order]
    counts = jnp.zeros((N_EXPERTS,), jnp.int32).at[expert_ids].add(1)
    padded = (counts + MOE_BLOCK - 1) // MOE_BLOCK * MOE_BLOCK
    starts = jnp.cumsum(counts) - counts
    pad_ends = jnp.cumsum(padded)
    pad_starts = pad_ends - padded
    rank = jnp.arange(NK, dtype=jnp.int32) - starts[e_sorted]
    dest = pad_starts[e_sorted] + rank
    slot_tok = jnp.full((n_slots,), N, jnp.int32).at[dest].set(token_ids[order])
    slot_w = jnp.zeros((n_slots,), jnp.float32).at[dest].set(weights[order])
    block_start = jnp.arange(n_blocks, dtype=jnp.int32) * MOE_BLOCK
    block_expert = jnp.minimum(jnp.searchsorted(pad_ends, block_start, side='right'),
                               N_EXPERTS - 1).astype(jnp.int32)
    x_pad = jnp.concatenate([xf, jnp.zeros((1, D), xf.dtype)], axis=0)
    xs = x_pad[slot_tok].reshape(n_blocks, MOE_BLOCK, D)

    def run_block(args):
        xb, e = args
        hid = jax.nn.silu(xb @ w_gate[e]) * (xb @ w_up[e])
        return hid @ w_down[e]

    ys = lax.map(run_block, (xs, block_expert)).reshape(n_slots, D)
    ys = ys * slot_w[:, None].astype(ys.dtype)
    out = jnp.zeros((N + 1, D), ys.dtype).at[slot_tok].add(ys)
    return out[:N]


def hierarchical_moe(h, w_group, b_group, w_expert, b_expert, w_gate, w_up, w_down):
    B, S, D = h.shape
    N = B * S
    xf = h.reshape(N, D)
    rows = jnp.arange(N, dtype=jnp.int32)
    g_logits = (xf @ w_group).astype(jnp.float32) + b_group.astype(jnp.float32)
    g_prob = jax.nn.softmax(g_logits, axis=-1)
    g_idx = jnp.argmax(g_logits, axis=-1).astype(jnp.int32)
    g_w = g_prob[rows, g_idx][:, None]
    e_all = ((xf @ w_expert).astype(jnp.float32).reshape(N, N_GROUPS, EXPERTS_PER_GROUP)
             + b_expert.astype(jnp.float32))
    e_logits = e_all[rows, g_idx]
    top_v, top_i = lax.top_k(e_logits, TOP_K)
    e_w = jax.nn.softmax(top_v, axis=-1)
    weights = (g_w * e_w).reshape(-1)
    expert_ids = (g_idx[:, None] * EXPERTS_PER_GROUP + top_i.astype(jnp.int32)).reshape(-1)
    y = expert_dispatch(xf, expert_ids, weights, w_gate, w_up, w_down)
    return y.reshape(B, S, D)


def setup_inputs(seed: int = 0) -> dict:
    key = jax.random.key(seed)
    ks = jax.random.split(key, 24)
    D, F, E = D_MODEL, EXPERT_HIDDEN, N_EXPERTS
    nA = (DEPTH + 1) // 2
    nB = DEPTH // 2
    dA = DIFF_HEAD_DIM
    nrm = jax.random.normal
    f32 = jnp.float32
    inv = D ** -0.5
    return {
        "x": nrm(ks[0], (BATCH, SEQ, D), f32),
        "c": nrm(ks[1], (BATCH, D), f32),
        "norm1_g": 1.0 + 0.02 * nrm(ks[2], (DEPTH, D), f32),
        "norm2_g": 1.0 + 0.02 * nrm(ks[3], (DEPTH, D), f32),
        "ada_w": 0.5 * inv * nrm(ks[4], (DEPTH, D, 6 * D), f32),
        "ada_b": 0.01 * nrm(ks[5], (DEPTH, 6 * D), f32),
        "diff_w_in": inv * nrm(ks[6], (nA, D, 3 * D), f32),
        "diff_w_out": inv * nrm(ks[7], (nA, D, D), f32),
        "diff_lambda_q1": 0.1 * nrm(ks[8], (nA, dA), f32),
        "diff_lambda_k1": 0.1 * nrm(ks[9], (nA, dA), f32),
        "diff_lambda_q2": 0.1 * nrm(ks[10], (nA, dA), f32),
        "diff_lambda_k2": 0.1 * nrm(ks[11], (nA, dA), f32),
        "diff_subln_g": 1.0 + 0.02 * nrm(ks[12], (nA, 2 * dA), f32),
        "sb_w_in": inv * nrm(ks[13], (nB, D, 3 * D), f32),
        "sb_w_out": inv * nrm(ks[14], (nB, D, D), f32),
        "router_group_w": inv * nrm(ks[15], (DEPTH, D, N_GROUPS), f32),
        "router_group_b": 0.01 * nrm(ks[16], (DEPTH, N_GROUPS), f32),
        "router_expert_w": inv * nrm(ks[17], (DEPTH, D, N_GROUPS * EXPERTS_PER_GROUP), f32),
        "router_expert_b": 0.01 * nrm(ks[18], (DEPTH, N_GROUPS, EXPERTS_PER_GROUP), f32),
        "expert_w_gate": inv * nrm(ks[19], (DEPTH, E, D, F), f32),
        "expert_w_up": inv * nrm(ks[20], (DEPTH, E, D, F), f32),
        "expert_w_down": (F ** -0.5) * nrm(ks[21], (DEPTH, E, F, D), f32),
        "final_norm_g": 1.0 + 0.02 * nrm(ks[22], (D,), f32),
    }


def reference(x, c, norm1_g, norm2_g, ada_w, ada_b, diff_w_in, diff_w_out,
              diff_lambda_q1, diff_lambda_k1, diff_lambda_q2, diff_lambda_k2, diff_subln_g,
              sb_w_in, sb_w_out, router_group_w, router_group_b, router_expert_w,
              router_expert_b, expert_w_gate, expert_w_up, expert_w_down, final_norm_g):
    cond = jax.nn.silu(c)
    for i in range(DEPTH):
        mod = cond @ ada_w[i] + ada_b[i]
        sh1, sc1, g1, sh2, sc2, g2 = jnp.split(mod[:, None, :], 6, axis=-1)
        h = rms_norm(x, norm1_g[i]) * (1.0 + sc1) + sh1
        j = i // N_MIXERS
        if i % N_MIXERS == 0:
            lambda_init = 0.8 - 0.6 * math.exp(-0.3 * i)
            y = diff_attention(h, diff_w_in[j], diff_w_out[j], diff_lambda_q1[j],
                               diff_lambda_k1[j], diff_lambda_q2[j], diff_lambda_k2[j],
                               diff_subln_g[j], lambda_init)
        else:
            y = stick_breaking_attention(h, sb_w_in[j], sb_w_out[j])
        x = x + g1 * y
        h = rms_norm(x, norm2_g[i]) * (1.0 + sc2) + sh2
        y = hierarchical_moe(h, router_group_w[i], router_group_b[i], router_expert_w[i],
                             router_expert_b[i], expert_w_gate[i], expert_w_up[i],
                             expert_w_down[i])
        x = x + g2 * y
    return rms_norm(x, final_norm_g)
```

```python
import math
from contextlib import ExitStack
import numpy as np
import concourse.bass as bass
import concourse.mybir as mybir
from concourse.bass_utils import run_bass_kernel_spmd
from concourse.alu_op_type import AluOpType as ALU

F32 = mybir.dt.float32
BF16 = mybir.dt.bfloat16
I32 = mybir.dt.int32
AF = mybir.ActivationFunctionType
AX = mybir.AxisListType

D = 1024
S = 2048
NCH = 16
KC = 8
NE = 32
FH = 512
RMS_EPS = 1e-6
SUBLN_EPS = 1e-5
NEG = -30000.0


class Trk:
    __slots__ = ("w", "r", "dsem", "dval", "name")

    def __init__(self, name=""):
        self.w = None
        self.r = {}
        self.dsem = None
        self.dval = 0
        self.name = name


class Eng:
    def __init__(self, nc, name, eng, selfsync):
        self.name = name
        self.eng = eng
        self.selfsync = selfsync
        self.sem = nc.alloc_semaphore(name="es_" + name)
        self.count = 0
        self.waited = {}


class FW:
    def __init__(self, nc):
        self.nc = nc
        self.pe = Eng(nc, "pe", nc.tensor, False)
        self.act = Eng(nc, "act", nc.scalar, True)
        self.dve = Eng(nc, "dve", nc.vector, True)
        self.pool = Eng(nc, "pool", nc.gpsimd, True)
        self.sp = Eng(nc, "sp", nc.sync, False)
        self.engs = [self.pe, self.act, self.dve, self.pool, self.sp]
        self.dsems = []
        self._bregs = {}
        self.ninst = 0

    def _wait(self, e, deps):
        best = {}
        for d in deps:
            if d is None:
                continue
            sem, val = d
            k = id(sem)
            if k not in best or best[k][1] < val:
                best[k] = (sem, val)
        for k, (sem, val) in best.items():
            if sem is e.sem and not e.selfsync:
                continue
            if e.waited.get(k, 0) >= val:
                continue
            e.eng.wait_ge(sem, val)
            e.waited[k] = val

    @staticmethod
    def _deps(reads, writes, waw=True):
        deps = []
        for t in reads:
            deps.append(t.w)
        for t in writes:
            if waw:
                deps.append(t.w)
            deps.extend(t.r.values())
        return deps

    def op(self, e, fns, reads=(), writes=()):
        self._wait(e, self._deps(reads, writes))
        if callable(fns):
            fns = [fns]
        inst = None
        for f in fns:
            inst = f()
            self.ninst += 1
        e.count += 1
        inst.then_inc(e.sem, 1)
        tag = (e.sem, e.count)
        for t in writes:
            t.w = tag
            t.r = {}
        for t in reads:
            t.r[e.name] = tag
        return tag

    def dma(self, e, pairs, reads=(), writes=(), waw=True, **kw):
        self._wait(e, self._deps(reads, writes, waw))
        owner = writes[0] if writes else reads[0]
        if owner.dsem is None:
            owner.dsem = self.nc.alloc_semaphore(name="ds%d" % len(self.dsems))
            self.dsems.append(owner)
        for (o, i) in pairs:
            e.eng.dma_start(out=o, in_=i, **kw).then_inc(owner.dsem, 16)
            owner.dval += 16
            self.ninst += 1
        tag = (owner.dsem, owner.dval)
        for t in writes:
            t.w = tag
            if waw:
                t.r = {}
        for t in reads:
            t.r["dma%d" % id(owner)] = tag
        return tag

    def dma_ind(self, out_ap, in_ap, idx_ap, scatter, reads=(), writes=(), waw=True, bounds=None):
        e = self.pool
        self._wait(e, self._deps(reads, writes, waw))
        owner = writes[0]
        if owner.dsem is None:
            owner.dsem = self.nc.alloc_semaphore(name="ds%d" % len(self.dsems))
            self.dsems.append(owner)
        if not isinstance(out_ap, list):
            out_ap, in_ap, idx_ap = [out_ap], [in_ap], [idx_ap]
        for o_, i_, x_ in zip(out_ap, in_ap, idx_ap):
            off = bass.IndirectOffsetOnAxis(ap=x_, axis=0)
            if scatter:
                self.nc.gpsimd.indirect_dma_start(out=o_, out_offset=off, in_=i_, in_offset=None).then_inc(owner.dsem, 16)
            else:
                kw = {}
                if bounds is not None:
                    if bounds not in self._bregs:
                        self._bregs[bounds] = self.nc.gpsimd.to_reg(bounds)
                    kw = dict(bounds_check=self._bregs[bounds], oob_is_err=False)
                self.nc.gpsimd.indirect_dma_start(out=o_, out_offset=None, in_=i_, in_offset=off, **kw).then_inc(owner.dsem, 16)
            owner.dval += 16
            self.ninst += 1
        tag = (owner.dsem, owner.dval)
        for t in writes:
            t.w = tag
            if waw:
                t.r = {}
        for t in reads:
            t.r["dma%d" % id(owner)] = tag
        return tag

    def barrier(self):
        deps = [(e.sem, e.count) for e in self.engs if e.count > 0]
        deps += [(t.dsem, t.dval) for t in self.dsems if t.dval > 0]
        for e in self.engs:
            self._wait(e, deps)


class Buf:
    def __init__(self, ap, name=""):
        self.ap = ap
        self.t = Trk(name)


def _host_consts():
    p = np.arange(128, dtype=np.float32)[:, None]
    j = np.arange(128, dtype=np.float32)[None, :]
    ident = (p == j).astype(np.float32)
    ones = np.ones((128, 128), np.float32)
    t_gt = (p > j).astype(np.float32)
    mb = np.zeros((128, 8, 128), np.float32)
    for h in range(8):
        slope = 2.0 ** (-(h + 1))
        allowed = (p // 64) <= (j // 64)
        corr = -2.0 * slope * np.maximum(p - j, 0.0)
        mb[:, h, :] = np.where(allowed, corr, NEG)
    sbm = (p < j).astype(np.float32)
    cbf = np.concatenate([ident, ones, t_gt, mb.reshape(128, 1024), sbm], axis=1)
    ab = np.zeros((128, 8, 16), np.float32)
    for h in range(8):
        slope = 2.0 ** (-(h + 1))
        for r in range(16):
            ab[:, h, r] = slope * (p[:, 0] + 1.0 - 128.0 * (r + 1))
    c32 = np.concatenate([ident, ones, ab.reshape(128, 128), sbm, np.repeat(p, 8, axis=1)], axis=1)
    return np.ascontiguousarray(cbf), np.ascontiguousarray(c32)


CBF_IDENT, CBF_ONES, CBF_TGT, CBF_MB, CBF_SBM = 0, 128, 256, 384, 1408
CBF_W = 1536
C32_IDENT, C32_ONES, C32_AB, C32_SBM = 0, 128, 256, 384
C32_IOTA = 512
C32_W = 520

SPARSE_MOE = True
NSLOT = 6


def build_program(upto="all", debug=None):
    nc = bass.Bass("TRN2", target_bir_lowering=False)
    fw = FW(nc)
    dbg_outs = {}

    def dump(name, ap, shape, trks):
        if debug is None or name not in debug:
            return
        d = nc.dram_tensor("dbg_" + name, list(shape), F32, kind="ExternalOutput").ap()
        fw.dma(fw.pool, [(d, ap)], reads=list(trks))
        dbg_outs[name] = d

    PE, ACT, DVE, POOL, SP = fw.pe, fw.act, fw.dve, fw.pool, fw.sp

    def dram_in(name, shape):
        return nc.dram_tensor(name, list(shape), F32, kind="ExternalInput").ap()

    x_d = dram_in("x", [S, D])
    cT_d = dram_in("cT", [128, KC])
    n1_d = dram_in("norm1_g", [2, D])
    n2_d = dram_in("norm2_g", [2, D])
    adaw_d = dram_in("ada_w", [2, D, 6 * D])
    adab_d = dram_in("ada_b", [2, 6 * D])
    dwin_d = dram_in("diff_w_in", [1, D, 3 * D])
    dwout_d = dram_in("diff_w_out", [1, D, D])
    lq1_d = dram_in("diff_lambda_q1", [1, 64])
    lk1_d = dram_in("diff_lambda_k1", [1, 64])
    lq2_d = dram_in("diff_lambda_q2", [1, 64])
    lk2_d = dram_in("diff_lambda_k2", [1, 64])
    subg_d = dram_in("subgT", [128, 1])
    swin_d = dram_in("sb_w_in", [1, D, 3 * D])
    swout_d = dram_in("sb_w_out", [1, D, D])
    rw_d = dram_in("router_w", [2, D, 36])
    rb_d = dram_in("router_b", [2, 36])
    wg_d = dram_in("expert_w_gate", [2, NE, D, FH])
    wu_d = dram_in("expert_w_up", [2, NE, D, FH])
    wd_d = dram_in("expert_w_down", [2, NE, FH, D])
    fng_d = dram_in("final_norm_g", [1, D])
    cbf_d = dram_in("cbf", [128, CBF_W])
    c32_d = dram_in("c32", [128, C32_W])
    out_d = nc.dram_tensor("out", [S, D], F32, kind="ExternalOutput").ap()
    NSLOTS = 8192
    xs_d = nc.dram_tensor("xs_scratch", [NSLOTS, D], BF16, kind="Internal").ap()
    ys_d = nc.dram_tensor("ys_scratch", [NSLOTS, D], F32, kind="Internal").ap()
    xs_t = Trk("xs_d")
    ys_t = Trk("ys_d")

    def sb(name, shape, dt=F32):
        return Buf(nc.alloc_sbuf_tensor("s_" + name, list(shape), dt).ap(), name)

    X = nc.alloc_sbuf_tensor("s_X", [128, NCH, D], F32).ap()
    TX = [Trk("X%d" % c) for c in range(NCH)]
    cbf = sb("cbf", [128, CBF_W], BF16)
    c32 = sb("c32", [128, C32_W], F32)
    BA = sb("BA", [128, D])
    BS = sb("BS", [128, D])
    BG = sb("BG", [128, D])
    condT = sb("condT", [128, KC])
    condrep = sb("condrep", [128, KC, 128], BF16)
    neghalf = sb("neghalf", [128, NCH])
    small = sb("small", [128, 64])
    WB = [sb("WB%d" % i, [128, 4096], BF16) for i in range(NSLOT)]
    PS = [Buf(nc.alloc_psum_tensor("ps%d" % i, [128, 512], F32).ap(), "ps%d" % i) for i in range(8)]

    ident_bf = cbf.ap[:, CBF_IDENT:CBF_IDENT + 128]
    ones_bf = cbf.ap[:, CBF_ONES:CBF_ONES + 128]
    tgt_bf = cbf.ap[:, CBF_TGT:CBF_TGT + 128]
    ident32 = c32.ap[:, C32_IDENT:C32_IDENT + 128]
    ones32 = c32.ap[:, C32_ONES:C32_ONES + 128]

    class WStream:
        def __init__(self):
            self.units = []
            self.slot_of = {}
            self.free = list(range(NSLOT))

        def add(self, pairs_fn):
            self.units.append(pairs_fn)
            return len(self.units) - 1

        def ensure(self, u):
            if u is None or u in self.slot_of:
                return
            assert self.free, "weight ring exhausted"
            si = self.free.pop(0)
            self.slot_of[u] = si
            slot = WB[si]
            r = self.units[u](slot.ap)
            if callable(r):
                r(slot)
            else:
                fw.dma(POOL, r, writes=[slot.t])

        def get(self, u):
            return WB[self.slot_of[u]]

        def release(self, u):
            self.free.append(self.slot_of[u])

    ws = WStream()

    for g in range(4):
        src = x_d[g * 512:(g + 1) * 512, :].rearrange("(c p) d -> p c d", p=128)
        fw.dma(SP, [(X[:, 4 * g:4 * g + 4, :], src)], writes=TX[4 * g:4 * g + 4])
    fw.dma(POOL, [(cbf.ap[:], cbf_d[:])], writes=[cbf.t])
    fw.dma(SP, [(c32.ap[:], c32_d[:])], writes=[c32.t])
    fw.dma(SP, [(condT.ap[:], cT_d[:])], writes=[condT.t])
    fw.op(DVE, lambda: nc.vector.memset(neghalf.ap[:], -0.5), writes=[neghalf.t])
    fw.op(ACT, lambda: nc.scalar.activation(out=condT.ap[:], in_=condT.ap[:], func=AF.Silu), reads=[condT.t], writes=[condT.t])
    for kc in range(KC):
        fw.op(DVE, lambda kc=kc: nc.vector.tensor_scalar(out=condrep.ap[:, kc, :], in0=ones32, scalar1=condT.ap[:, kc:kc + 1],
                                                         scalar2=None, op0=ALU.mult),
              reads=[condT.t, c32.t], writes=[condrep.t])

    if SPARSE_MOE:
        zt = sb("zt", [128, D], BF16)
        fw.op(DVE, lambda: nc.vector.memset(zt.ap[:], 0.0), writes=[zt.t])
        for b in range(64):
            fw.dma(SP, [(xs_d[b * 128:(b + 1) * 128, :], zt.ap[:])], reads=[zt.t], writes=[xs_t], waw=False)

    def add_mod_units(i, j):
        us = []
        for hf in range(2):
            src = adaw_d[i, :, j * D + hf * 512: j * D + (hf + 1) * 512].rearrange("(kc p) f -> p kc f", p=128)
            us.append(ws.add(lambda slot, src=src: [(slot[:, :].rearrange("p (kc f) -> p kc f", kc=KC), src)]))
        return us

    def gen_mod(i, j, dst, units, norm_g=None, tmp=None):
        fw.dma(SP, [(dst.ap[:], adab_d[i:i + 1, j * D:(j + 1) * D].broadcast_to([128, D]))], writes=[dst.t])
        if norm_g is not None:
            fw.dma(SP, [(tmp.ap[:], norm_g.broadcast_to([128, D]))], writes=[tmp.t])
        for hf in range(2):
            u = units[hf]
            ws.ensure(u)
            w = ws.get(u)
            wv = w.ap[:, :].rearrange("p (kc f) -> p kc f", kc=KC)
            ps = PS[hf]
            fw.op(PE, [lambda kc=kc: nc.tensor.matmul(ps.ap[:], condrep.ap[:, kc, :], wv[:, kc, :], start=(kc == 0), stop=(kc == KC - 1))
                       for kc in range(KC)], reads=[condrep.t, w.t], writes=[ps.t])
            ws.release(u)
            sl = slice(hf * 512, (hf + 1) * 512)
            fw.op(DVE, lambda: nc.vector.tensor_tensor(out=dst.ap[:, sl], in0=ps.ap[:], in1=dst.ap[:, sl], op=ALU.add),
                  reads=[ps.t, dst.t], writes=[dst.t])
        if norm_g is not None:
            fw.op(DVE, lambda: nc.vector.scalar_tensor_tensor(out=dst.ap[:], in0=dst.ap[:], scalar=1.0, in1=tmp.ap[:],
                                                             op0=ALU.add, op1=ALU.mult),
                  reads=[dst.t, tmp.t], writes=[dst.t])

    def norm_modulate(a, sh, hT, scr, ss, rstd, hT32_cb=None, hT_chunks=None):
        for c in range(NCH):
            fw.op(ACT, lambda c=c: nc.scalar.activation(out=scr[0].ap[:], in_=X[:, c, :], func=AF.Square,
                                                        accum_out=ss.ap[:, c:c + 1]),
                  reads=[TX[c]], writes=[scr[0].t, ss.t])
        fw.op(DVE, lambda: nc.vector.tensor_scalar(out=rstd.ap[:], in0=ss.ap[:], scalar1=1.0 / D, scalar2=RMS_EPS, op0=ALU.mult, op1=ALU.add),
              reads=[ss.t], writes=[rstd.t])
        fw.op(POOL, lambda: nc.gpsimd.tensor_tensor(out=rstd.ap[:], in0=rstd.ap[:], in1=neghalf.ap[:, 0:NCH], op=ALU.pow),
              reads=[rstd.t, neghalf.t], writes=[rstd.t])
        for c in range(NCH):
            h = scr[c % 2]
            fw.op(DVE, lambda c=c, h=h: nc.vector.scalar_tensor_tensor(out=h.ap[:], in0=X[:, c, :], scalar=rstd.ap[:, c:c + 1], in1=a.ap[:],
                                                                      op0=ALU.mult, op1=ALU.mult),
                  reads=[TX[c], rstd.t, a.t], writes=[h.t])
            fw.op(DVE, lambda h=h: nc.vector.tensor_tensor(out=h.ap[:], in0=h.ap[:], in1=sh.ap[:], op=ALU.add),
                  reads=[h.t, sh.t], writes=[h.t])
            pss = []
            for b in range(2):
                ps = PS[4 + (2 * c + b) % 4]
                fw.op(PE, [lambda k=k, ps=ps, h=h, b=b: nc.tensor.transpose(ps.ap[:, k * 128:(k + 1) * 128], h.ap[:, (4 * b + k) * 128:(4 * b + k + 1) * 128], ident32)
                           for k in range(4)], reads=[h.t, c32.t], writes=[ps.t])
                if hT is not None:
                    fw.op(ACT, lambda ps=ps, b=b, c=c: nc.scalar.copy(out=hT.ap[:, 4 * b:4 * b + 4, c * 128:(c + 1) * 128],
                                                                      in_=ps.ap[:, :].rearrange("p (k t) -> p k t", k=4)),
                          reads=[ps.t], writes=[hT.t])
                if hT_chunks is not None:
                    hk = hT_chunks[c % 2]
                    fw.op(ACT, lambda ps=ps, b=b, hk=hk: nc.scalar.copy(out=hk.ap[:, 4 * b:4 * b + 4, :], in_=ps.ap[:, :].rearrange("p (k t) -> p k t", k=4)),
                          reads=[ps.t], writes=[hk.t])
                pss.append(ps)
            if hT32_cb is not None:
                if hT_chunks is not None:
                    hT32_cb(c, h, pss, hT_chunks[c % 2])
                else:
                    hT32_cb(c, h, pss)

    def attn_norm(i, st, st2):
        lambda_init = 0.8 - 0.6 * math.exp(-0.3 * i)
        hT = Buf(st.enter_context(nc.sbuf_tensor("s_hT%d" % i, [128, KC, S], BF16)).ap(), "hT")
        scr = [Buf(st2.enter_context(nc.sbuf_tensor("s_scr%d_%d" % (k, i), [128, D], F32)).ap(), "scr%d" % k) for k in range(2)]
        ss = Buf(st2.enter_context(nc.sbuf_tensor("s_ss%d" % i, [128, NCH], F32)).ap(), "ss")
        rstd = Buf(st2.enter_context(nc.sbuf_tensor("s_rstd%d" % i, [128, NCH], F32)).ap(), "rstd")
        u_sh = add_mod_units(i, 0)
        u_sc = add_mod_units(i, 1)
        u_g = add_mod_units(i, 2)
        gen_mod(i, 0, BS, u_sh)
        gen_mod(i, 1, BA, u_sc, norm_g=n1_d[i:i + 1, :], tmp=scr[0])
        gen_mod(i, 2, BG, u_g)
        dump("BS", BS.ap[:], [128, D], [BS.t]); dump("BA", BA.ap[:], [128, D], [BA.t]); dump("BG", BG.ap[:], [128, D], [BG.t])
        norm_modulate(BA, BS, hT, scr, ss, rstd)
        dump("hT", hT.ap[:], [128, KC, S], [hT.t])
        dump("rstd", rstd.ap[:], [128, NCH], [rstd.t])
        return hT, lambda_init

    uniq = [0]

    def mk(st, name, shape, dt=F32):
        uniq[0] += 1
        return Buf(st.enter_context(nc.sbuf_tensor("s_%s_%d" % (name, uniq[0]), list(shape), dt)).ap(), name)

    def diff_attention_heads(st, hT, lambda_init):
        QT = mk(st, "QT", [128, 2, S], BF16)
        fw.op(DVE, lambda: nc.vector.memset(QT.ap[:], 0.0), writes=[QT.t])
        KT = mk(st, "KT", [128, S], BF16)
        V = mk(st, "V", [128, NCH, 128], BF16)
        Eb = [mk(st, "E%d" % k, [128, 512], BF16) for k in range(4)]
        ou = mk(st, "ou", [128, S])
        osq = [mk(st, "osq%d" % k, [128, 512], BF16) for k in range(2)]
        OT = mk(st, "OT", [128, S], BF16)
        tA = mk(st, "tA", [128, 512])
        tB = mk(st, "tB", [128, 512])
        zer = mk(st, "dzer", [128, 512], BF16)
        fw.op(DVE, lambda: nc.vector.memset(zer.ap[:], 0.0), writes=[zer.t])
        class _V:
            def __init__(self, ap, t):
                self.ap = ap
                self.t = t
        lam4 = [_V(tA.ap[:, 0:64], tA.t), _V(tA.ap[:, 64:128], tA.t), _V(tB.ap[:, 0:64], tB.t), _V(tB.ap[:, 64:128], tB.t)]
        MB = cbf.ap[:, CBF_MB:CBF_MB + 1024]
        AB = c32.ap[:, C32_AB:C32_AB + 128]
        for k, src in enumerate([lq1_d, lk1_d, lq2_d, lk2_d]):
            fw.dma(SP, [(lam4[k].ap[:], src[0:1, :].broadcast_to([128, 64]))], writes=[lam4[k].t])
        sm = small.ap
        for k in range(2):
            fw.op(DVE, lambda k=k: nc.vector.tensor_tensor(out=lam4[2 * k].ap[:], in0=lam4[2 * k].ap[:], in1=lam4[2 * k + 1].ap[:], op=ALU.mult),
                  reads=[lam4[2 * k].t, lam4[2 * k + 1].t], writes=[lam4[2 * k].t])
            fw.op(DVE, lambda k=k: nc.vector.reduce_sum(out=sm[:, k:k + 1], in_=lam4[2 * k].ap[:], axis=AX.X),
                  reads=[lam4[2 * k].t], writes=[small.t])
        fw.op(ACT, lambda: nc.scalar.activation(out=sm[:, 2:4], in_=sm[:, 0:2], func=AF.Exp), reads=[small.t], writes=[small.t])
        fw.op(DVE, lambda: nc.vector.tensor_tensor(out=sm[:, 4:5], in0=sm[:, 3:4], in1=sm[:, 2:3], op=ALU.subtract), reads=[small.t], writes=[small.t])
        fw.op(DVE, lambda: nc.vector.tensor_scalar(out=sm[:, 4:5], in0=sm[:, 4:5], scalar1=-lambda_init, scalar2=None, op0=ALU.add), reads=[small.t], writes=[small.t])
        neglam = sm[:, 4:5]
        fw.dma(SP, [(sm[:, 5:6], subg_d[:])], writes=[small.t])
        fw.op(DVE, lambda: nc.vector.tensor_scalar(out=sm[:, 5:6], in0=sm[:, 5:6], scalar1=1.0 - lambda_init, scalar2=None, op0=ALU.mult), reads=[small.t], writes=[small.t])
        gsub = sm[:, 5:6]
        fw.op(DVE, lambda: nc.vector.memset(sm[:, 6:7], SUBLN_EPS), writes=[small.t])
        epsb = sm[:, 6:7]
        dump("small", sm[:, 0:8], [128, 8], [small.t])

        def head_unit(h):
            def f(slot):
                v = slot[:, 0:3072].rearrange("p (kc f) -> p kc f", kc=KC)
                return [(v[:, :, j * 128:(j + 1) * 128], dwin_d[0, :, j * D + h * 128: j * D + (h + 1) * 128].rearrange("(kc p) f -> p kc f", p=128))
                        for j in range(3)]
            return ws.add(f)
        u_head = [head_unit(h) for h in range(8)]
        u_wo = [ws.add(lambda slot, hf=hf: [(slot[:, :].rearrange("p (h f) -> p h f", h=8),
                                              dwout_d[0, :, hf * 512:(hf + 1) * 512].rearrange("(h p) f -> p h f", p=128))]) for hf in range(2)]
        ws.ensure(u_head[0])
        ws.ensure(u_wo[0])
        ws.ensure(u_wo[1])
        Wo = []
        for hf in range(2):
            w = ws.get(u_wo[hf])
            wv = w.ap[:, :].rearrange("p (h f) -> p h f", h=8)
            for h in range(8):
                fw.op(DVE, lambda wv=wv, h=h, hf=hf: nc.vector.tensor_tensor(out=wv[:, h, :], in0=wv[:, h, :], in1=BG.ap[:, hf * 512:(hf + 1) * 512], op=ALU.mult),
                      reads=[w.t, BG.t], writes=[w.t])
            Wo.append((w, wv))

        evac_i = [0]

        def evac(dst_ap, dst_t, ps, scale=None, src_ap=None):
            src = ps.ap[:] if src_ap is None else src_ap
            evac_i[0] += 1
            if evac_i[0] % 2 == 0:
                if scale is None:
                    fw.op(ACT, lambda: nc.scalar.copy(out=dst_ap, in_=src), reads=[ps.t], writes=[dst_t])
                else:
                    fw.op(ACT, lambda: nc.scalar.mul(out=dst_ap, in_=src, mul=scale), reads=[ps.t], writes=[dst_t])
            else:
                if scale is None:
                    fw.op(DVE, lambda: nc.vector.tensor_copy(out=dst_ap, in_=src), reads=[ps.t], writes=[dst_t])
                else:
                    fw.op(DVE, lambda: nc.vector.tensor_scalar(out=dst_ap, in0=src, scalar1=scale, scalar2=None, op0=ALU.mult), reads=[ps.t], writes=[dst_t])

        rot = [0]

        def sbank():
            rot[0] += 1
            return PS[4 + rot[0] % 4]

        for h in range(8):
            slope = 2.0 ** (-(h + 1))
            Wref = 128 if h == 0 else (256 if h == 1 else 512)
            if h + 1 < 8:
                ws.ensure(u_head[h + 1])
            W = ws.get(u_head[h])
            Wv = W.ap[:, 0:3072].rearrange("p (kc f) -> p kc f", kc=KC)
            for which, dst, scale in ((0, QT, 0.125), (1, KT, None)):
                for tt in range(4):
                    ps = sbank()
                    fw.op(PE, [lambda kc=kc, ps=ps, which=which, tt=tt: nc.tensor.matmul(ps.ap[:], Wv[:, kc, which * 128:(which + 1) * 128],
                                                                                        hT.ap[:, kc, tt * 512:(tt + 1) * 512], start=(kc == 0), stop=(kc == KC - 1))
                               for kc in range(KC)], reads=[W.t, hT.t], writes=[ps.t])
                    if which == 0:
                        for m in range(2):
                            rows = slice(m * 64, (m + 1) * 64)
                            evac(QT.ap[rows, m, tt * 512:(tt + 1) * 512], QT.t, ps, scale, src_ap=ps.ap[rows, :])
                    else:
                        evac(dst.ap[:, tt * 512:(tt + 1) * 512], dst.t, ps, scale)
            for g in range(4):
                ps = sbank()
                fw.op(PE, [lambda kc=kc, k=k, ps=ps, g=g: nc.tensor.matmul(ps.ap[:, k * 128:(k + 1) * 128], hT.ap[:, kc, (4 * g + k) * 128:(4 * g + k + 1) * 128],
                                                                          Wv[:, kc, 256:384], start=(kc == 0), stop=(kc == KC - 1))
                           for k in range(4) for kc in range(KC)], reads=[W.t, hT.t], writes=[ps.t])
                evac(V.ap[:, 4 * g:4 * g + 4, :], V.t, ps, None, src_ap=ps.ap[:, :].rearrange("p (k e) -> p k e", k=4))
            ws.release(u_head[h])
            if h == 0:
                dump("KT0", KT.ap[:], [128, S], [KT.t]); dump("V0", V.ap[:], [128, NCH, 128], [V.t])
            O = [PS[0], PS[1]]
            Dn = [PS[2], PS[3]]
            steps = [(qt, kb) for qt in range(4) for kb in range(4 * qt + 4)]

            def stage1(n):
                qt, kb = steps[n]
                r = kb - 4 * qt
                c0 = max(0, r) * 128
                q0 = qt * 512
                for m in range(2):
                    ps = PS[4 + (2 * n + m) % 4]
                    E = Eb[(2 * n + m) % 4]
                    lhs = KT.ap[:, kb * 128:(kb + 1) * 128]
                    fns = []
                    if r < 0:
                        fns.append(lambda ps=ps, lhs=lhs, m=m, q0=q0: nc.tensor.matmul(ps.ap[:, 0:512], lhs, QT.ap[:, m, q0:q0 + 512], start=True, stop=True))
                    else:
                        fns.append(lambda ps=ps, lhs=lhs, m=m, q0=q0, c0=c0: nc.tensor.matmul(ps.ap[:, c0:512], lhs, QT.ap[:, m, q0 + c0:q0 + 512], start=True, stop=False))
                        fns.append(lambda ps=ps, c0=c0: nc.tensor.matmul(ps.ap[:, c0:c0 + 128], ident_bf, MB[:, h * 128:(h + 1) * 128], start=False, stop=True))
                    fw.op(PE, fns, reads=[KT.t, QT.t, cbf.t], writes=[ps.t])
                    fns = []
                    a = (c0 // Wref) * Wref
                    while a < 512:
                        b = a + Wref
                        lo = max(a, c0)
                        rr = (q0 + b - kb * 128) // 128 - 1
                        fns.append(lambda ps=ps, E=E, lo=lo, b=b, rr=rr: nc.scalar.activation(out=E.ap[:, lo:b], in_=ps.ap[:, lo:b], func=AF.Exp,
                                                                                         bias=AB[:, h * 16 + rr:h * 16 + rr + 1], scale=1.0))
                        a = b
                    fw.op(ACT, fns, reads=[ps.t, c32.t], writes=[E.t])

            def stage2(n):
                qt, kb = steps[n]
                r = kb - 4 * qt
                c0 = max(0, r) * 128
                lastk = (kb == 4 * qt + 3)
                for m in range(2):
                    E = Eb[(2 * n + m) % 4]
                    Vk = V.ap[:, kb, :]
                    fns = []
                    for acc, lt in ((O[m], Vk), (Dn[m], ones_bf)):
                        if kb == 0:
                            fns.append(lambda acc=acc: nc.tensor.matmul(acc.ap[:, 0:512], ones_bf, zer.ap[:, 0:512], start=True, stop=False))
                        fns.append(lambda acc=acc, lt=lt, E=E, c0=c0, lastk=lastk: nc.tensor.matmul(acc.ap[:, c0:512], lt, E.ap[:, c0:512], start=False, stop=lastk))
                    fw.op(PE, fns, reads=[V.t, E.t, cbf.t, zer.t], writes=[O[m].t, Dn[m].t])

            stage1(0)
            for n in range(len(steps)):
                if n + 1 < len(steps):
                    stage1(n + 1)
                stage2(n)
                qt, kb = steps[n]
                if kb == 4 * qt + 3:
                    qs = slice(qt * 512, (qt + 1) * 512)
                    fw.op(DVE, lambda: nc.vector.reciprocal(out=tA.ap[:], in_=Dn[0].ap[:]), reads=[Dn[0].t], writes=[tA.t])
                    fw.op(DVE, lambda: nc.vector.tensor_tensor(out=tA.ap[:], in0=O[0].ap[:], in1=tA.ap[:], op=ALU.mult), reads=[O[0].t, tA.t], writes=[tA.t])
                    fw.op(DVE, lambda: nc.vector.reciprocal(out=tB.ap[:], in_=Dn[1].ap[:]), reads=[Dn[1].t], writes=[tB.t])
                    fw.op(DVE, lambda: nc.vector.tensor_tensor(out=tB.ap[:], in0=O[1].ap[:], in1=tB.ap[:], op=ALU.mult), reads=[O[1].t, tB.t], writes=[tB.t])
                    fw.op(DVE, lambda qs=qs: nc.vector.scalar_tensor_tensor(out=ou.ap[:, qs], in0=tB.ap[:], scalar=neglam, in1=tA.ap[:], op0=ALU.mult, op1=ALU.add),
                          reads=[tA.t, tB.t, small.t], writes=[ou.t])
            if h == 0:
                dump("ou0", ou.ap[:], [128, S], [ou.t])
            for tt in range(4):
                ps = sbank()
                ts = slice(tt * 512, (tt + 1) * 512)
                oq = osq[tt % 2]
                fw.op(DVE, lambda ts=ts, oq=oq: nc.vector.tensor_tensor(out=oq.ap[:], in0=ou.ap[:, ts], in1=ou.ap[:, ts], op=ALU.mult), reads=[ou.t], writes=[oq.t])
                fw.op(PE, lambda ps=ps, oq=oq: nc.tensor.matmul(ps.ap[:], ones_bf, oq.ap[:], start=True, stop=True), reads=[oq.t, cbf.t], writes=[ps.t])
                tS = tA if tt % 2 == 0 else tB
                fw.op(ACT, lambda ps=ps, tS=tS: nc.scalar.activation(out=tS.ap[:], in_=ps.ap[:], func=AF.Sqrt, bias=epsb, scale=1.0 / 128),
                      reads=[ps.t, small.t], writes=[tS.t])
                fw.op(DVE, lambda tS=tS: nc.vector.reciprocal(out=tS.ap[:], in_=tS.ap[:]), reads=[tS.t], writes=[tS.t])
                fw.op(DVE, lambda ts=ts, tS=tS: nc.vector.scalar_tensor_tensor(out=OT.ap[:, ts], in0=ou.ap[:, ts], scalar=gsub, in1=tS.ap[:], op0=ALU.mult, op1=ALU.mult),
                      reads=[ou.t, tS.t, small.t], writes=[OT.t])
            if h == 0:
                dump("OT0", OT.ap[:], [128, S], [OT.t])
            for c in range(NCH):
                for hf in range(2):
                    ps = sbank()
                    w, wv = Wo[hf]
                    fw.op(PE, lambda ps=ps, c=c, wv=wv, h=h: nc.tensor.matmul(ps.ap[:], OT.ap[:, c * 128:(c + 1) * 128], wv[:, h, :], start=True, stop=True),
                          reads=[OT.t, w.t], writes=[ps.t])
                    xs = X[:, c, hf * 512:(hf + 1) * 512]
                    fw.op(DVE, lambda ps=ps, xs=xs: nc.vector.tensor_tensor(out=xs, in0=ps.ap[:], in1=xs, op=ALU.add), reads=[ps.t, TX[c]], writes=[TX[c]])
        ws.release(u_wo[0])
        ws.release(u_wo[1])

    def moe_layer(i):
        with ExitStack() as st:
            hT = mk(st, "hTm", [128, KC, S], BF16)
            scr = [mk(st, "mscr%d" % k, [128, D]) for k in range(2)]
            ss = mk(st, "mss", [128, NCH])
            rstd = mk(st, "mrstd", [128, NCH])
            hlo = mk(st, "hlo", [128, KC, 128], BF16)
            rwhi = mk(st, "rwhi", [128, KC, 36], BF16)
            rwlo = mk(st, "rwlo", [128, KC, 36], BF16)
            rw = mk(st, "rw", [128, KC, 36])
            rb = mk(st, "rb", [128, 36])
            lg = mk(st, "lg", [128, 36])
            rt = mk(st, "rt", [128, 64])
            em = mk(st, "em", [128, 32])
            oh = mk(st, "oh", [128, 32])
            gw = mk(st, "gw", [128, NCH, NE])
            hid = [mk(st, "hid%d" % k, [128, 4, 512], BF16) for k in range(2)]
            sg = [mk(st, "sg%d" % k, [128, 512], BF16) for k in range(2)]
            u_sh = add_mod_units(i, 3)
            u_sc = add_mod_units(i, 4)
            u_g = add_mod_units(i, 5)
            fw.dma(SP, [(rw.ap[:], rw_d[i].rearrange("(kc p) n -> p kc n", p=128))], writes=[rw.t])
            fw.dma(SP, [(rb.ap[:], rb_d[i:i + 1, :].broadcast_to([128, 36]))], writes=[rb.t])
            fw.op(DVE, lambda: nc.vector.tensor_copy(out=rwhi.ap[:], in_=rw.ap[:]), reads=[rw.t], writes=[rwhi.t])
            fw.op(DVE, lambda: nc.vector.tensor_tensor(out=rwlo.ap[:], in0=rw.ap[:], in1=rwhi.ap[:], op=ALU.subtract), reads=[rw.t, rwhi.t], writes=[rwlo.t])
            gen_mod(i, 3, BS, u_sh)
            gen_mod(i, 4, BA, u_sc, norm_g=n2_d[i:i + 1, :], tmp=scr[0])
            gen_mod(i, 5, BG, u_g)

            def expert_units(e):
                ug = ws.add(lambda slot, e=e: [(slot[:, :].rearrange("p (kc f) -> p kc f", kc=KC), wg_d[i, e].rearrange("(kc p) f -> p kc f", p=128))])
                uu = ws.add(lambda slot, e=e: [(slot[:, :].rearrange("p (kc f) -> p kc f", kc=KC), wu_d[i, e].rearrange("(kc p) f -> p kc f", p=128))])
                ud = ws.add(lambda slot, e=e: [(slot[:, :].rearrange("p (fc d) -> p fc d", fc=4), wd_d[i, e].rearrange("(fc p) d -> p fc d", p=128))])
                return (ug, uu, ud)
            eu = [expert_units(e) for e in range(NE)]
            for u in eu[0]:
                ws.ensure(u)

            r_ = rt.ap

            def router_cb(c, h, pss):
                cs = slice(c * 128, (c + 1) * 128)
                for b in range(2):
                    fw.op(DVE, lambda b=b: nc.vector.tensor_tensor(out=hlo.ap[:, 4 * b:4 * b + 4, :], in0=pss[b].ap[:, :].rearrange("p (k t) -> p k t", k=4),
                                                                  in1=hT.ap[:, 4 * b:4 * b + 4, cs], op=ALU.subtract),
                          reads=[pss[b].t, hT.t], writes=[hlo.t])
                ps = PS[0]
                fns = []
                for kc in range(KC):
                    fns.append(lambda kc=kc: nc.tensor.matmul(ps.ap[:, 0:36], hT.ap[:, kc, cs], rwhi.ap[:, kc, :], start=(kc == 0), stop=False))
                    fns.append(lambda kc=kc: nc.tensor.matmul(ps.ap[:, 0:36], hlo.ap[:, kc, :], rwhi.ap[:, kc, :], start=False, stop=False))
                    fns.append(lambda kc=kc: nc.tensor.matmul(ps.ap[:, 0:36], hT.ap[:, kc, cs], rwlo.ap[:, kc, :], start=False, stop=(kc == KC - 1)))
                fw.op(PE, fns, reads=[hT.t, hlo.t, rwhi.t, rwlo.t], writes=[ps.t])
                V_ = nc.vector
                import os
                if os.environ.get('ROUTER_MM_ONLY'):
                    fw.op(DVE, lambda: V_.tensor_tensor(out=lg.ap[:], in0=ps.ap[:, 0:36], in1=rb.ap[:], op=ALU.add), reads=[ps.t, rb.t], writes=[lg.t])
                    return
                ops = []
                ops.append((lambda: V_.tensor_tensor(out=lg.ap[:], in0=ps.ap[:, 0:36], in1=rb.ap[:], op=ALU.add), [ps.t, rb.t], [lg.t]))
                ops.append((lambda: V_.reduce_max(out=r_[:, 0:1], in_=lg.ap[:, 0:4], axis=AX.X), [lg.t], [rt.t]))
                ops.append((lambda: V_.tensor_scalar(out=r_[:, 1:2], in0=r_[:, 0:1], scalar1=-1.0, scalar2=None, op0=ALU.mult), [rt.t], [rt.t]))
                ops.append((lambda: V_.tensor_scalar(out=r_[:, 4:8], in0=lg.ap[:, 0:4], scalar1=r_[:, 0:1], scalar2=None, op0=ALU.is_equal), [lg.t, rt.t], [rt.t]))
                for o_ in ops:
                    fw.op(DVE, o_[0], reads=o_[1], writes=o_[2])
                fw.op(ACT, lambda: nc.scalar.activation(out=r_[:, 8:12], in_=lg.ap[:, 0:4], func=AF.Exp, bias=r_[:, 1:2], scale=1.0), reads=[lg.t, rt.t], writes=[rt.t])
                ops = []
                ops.append((lambda: V_.reduce_sum(out=r_[:, 2:3], in_=r_[:, 8:12], axis=AX.X), [rt.t], [rt.t]))
                ops.append((lambda: V_.reciprocal(out=r_[:, 3:4], in_=r_[:, 2:3]), [rt.t], [rt.t]))
                ops.append((lambda: V_.tensor_scalar(out=r_[:, 12:16], in0=r_[:, 4:8], scalar1=-1.0, scalar2=1.0e9, op0=ALU.add, op1=ALU.mult), [rt.t], [rt.t]))
                for g in range(4):
                    ops.append((lambda g=g: V_.tensor_scalar(out=em.ap[:, 8 * g:8 * g + 8], in0=lg.ap[:, 4 + 8 * g:12 + 8 * g], scalar1=r_[:, 12 + g:13 + g], scalar2=None, op0=ALU.add),
                                [lg.t, rt.t], [em.t]))
                ops.append((lambda: V_.max(out=r_[:, 16:24], in_=em.ap[:]), [em.t], [rt.t]))
                ops.append((lambda: V_.tensor_tensor(out=r_[:, 24:25], in0=r_[:, 17:18], in1=r_[:, 16:17], op=ALU.subtract), [rt.t], [rt.t]))
                for o_ in ops:
                    fw.op(DVE, o_[0], reads=o_[1], writes=o_[2])
                fw.op(ACT, lambda: nc.scalar.activation(out=r_[:, 25:26], in_=r_[:, 24:25], func=AF.Exp), reads=[rt.t], writes=[rt.t])
                ops = []
                ops.append((lambda: V_.tensor_scalar(out=r_[:, 26:27], in0=r_[:, 25:26], scalar1=1.0, scalar2=None, op0=ALU.add), [rt.t], [rt.t]))
                ops.append((lambda: V_.reciprocal(out=r_[:, 27:28], in_=r_[:, 26:27]), [rt.t], [rt.t]))
                ops.append((lambda: V_.tensor_tensor(out=r_[:, 28:29], in0=r_[:, 27:28], in1=r_[:, 3:4], op=ALU.mult), [rt.t], [rt.t]))
                ops.append((lambda: V_.tensor_tensor(out=r_[:, 29:30], in0=r_[:, 28:29], in1=r_[:, 25:26], op=ALU.mult), [rt.t], [rt.t]))
                ops.append((lambda: V_.tensor_scalar(out=oh.ap[:], in0=em.ap[:], scalar1=r_[:, 16:17], scalar2=r_[:, 28:29], op0=ALU.is_equal, op1=ALU.mult), [em.t, rt.t], [oh.t]))
                ops.append((lambda: V_.tensor_scalar(out=em.ap[:], in0=em.ap[:], scalar1=r_[:, 17:18], scalar2=r_[:, 29:30], op0=ALU.is_equal, op1=ALU.mult), [em.t, rt.t], [em.t]))
                ops.append((lambda c=c: V_.tensor_tensor(out=gw.ap[:, c, :], in0=oh.ap[:], in1=em.ap[:], op=ALU.add), [oh.t, em.t], [gw.t]))
                for o_ in ops:
                    fw.op(DVE, o_[0], reads=o_[1], writes=o_[2])

            import os
            norm_modulate(BA, BS, hT, scr, ss, rstd, hT32_cb=(None if os.environ.get('NOROUTER') else router_cb))
            dump("gw%d" % i, gw.ap[:], [128, NCH, NE], [gw.t])
            dump("hTm%d" % i, hT.ap[:], [128, KC, S], [hT.t])

            n_exp = NE if upto not in ("moe_few", "moe_router") else (2 if upto == "moe_few" else 0)
            rot = [0, 0]
            for e in range(n_exp):
                if e + 1 < n_exp:
                    for u in eu[e + 1]:
                        ws.ensure(u)
                Wg, Wu, Wd = [ws.get(u) for u in eu[e]]
                wgv = Wg.ap[:, :].rearrange("p (kc f) -> p kc f", kc=KC)
                wuv = Wu.ap[:, :].rearrange("p (kc f) -> p kc f", kc=KC)
                wdv = Wd.ap[:, :].rearrange("p (fc d) -> p fc d", fc=4)
                for fc in range(4):
                    fw.op(DVE, lambda fc=fc: nc.vector.tensor_tensor(out=wdv[:, fc, :], in0=wdv[:, fc, :], in1=BG.ap[:], op=ALU.mult), reads=[Wd.t, BG.t], writes=[Wd.t])
                for tt in range(4):
                    ts = slice(tt * 512, (tt + 1) * 512)
                    hd = hid[tt % 2]
                    for f in range(4):
                        rot[0] += 1
                        pg = PS[(2 * rot[0]) % 4]
                        pu = PS[(2 * rot[0]) % 4 + 1]
                        fw.op(PE, [lambda kc=kc, pg=pg, f=f: nc.tensor.matmul(pg.ap[:], wgv[:, kc, f * 128:(f + 1) * 128], hT.ap[:, kc, ts], start=(kc == 0), stop=(kc == KC - 1))
                                   for kc in range(KC)], reads=[Wg.t, hT.t], writes=[pg.t])
                        fw.op(PE, [lambda kc=kc, pu=pu, f=f: nc.tensor.matmul(pu.ap[:], wuv[:, kc, f * 128:(f + 1) * 128], hT.ap[:, kc, ts], start=(kc == 0), stop=(kc == KC - 1))
                                   for kc in range(KC)], reads=[Wu.t, hT.t], writes=[pu.t])
                        s_ = sg[rot[0] % 2]
                        fw.op(ACT, lambda pg=pg, s_=s_: nc.scalar.activation(out=s_.ap[:], in_=pg.ap[:], func=AF.Silu), reads=[pg.t], writes=[s_.t])
                        fw.op(DVE, lambda pu=pu, s_=s_, f=f, hd=hd: nc.vector.tensor_tensor(out=hd.ap[:, f, :], in0=pu.ap[:], in1=s_.ap[:], op=ALU.mult),
                              reads=[pu.t, s_.t], writes=[hd.t])
                    for cc in range(4):
                        c = 4 * tt + cc
                        for hf in range(2):
                            rot[1] += 1
                            ps = PS[4 + rot[1] % 4]
                            fw.op(PE, [lambda f=f, ps=ps, cc=cc, hf=hf, hd=hd: nc.tensor.matmul(ps.ap[:], hd.ap[:, f, cc * 128:(cc + 1) * 128], wdv[:, f, hf * 512:(hf + 1) * 512],
                                                                                         start=(f == 0), stop=(f == 3)) for f in range(4)],
                                  reads=[hd.t, Wd.t], writes=[ps.t])
                            xs = X[:, c, hf * 512:(hf + 1) * 512]
                            fw.op(DVE, lambda ps=ps, xs=xs, c=c, e=e: nc.vector.scalar_tensor_tensor(out=xs, in0=ps.ap[:], scalar=gw.ap[:, c, e:e + 1], in1=xs, op0=ALU.mult, op1=ALU.add),
                                  reads=[ps.t, gw.t, TX[c]], writes=[TX[c]])
                for u in eu[e]:
                    ws.release(u)
            fw.barrier()

    def moe_layer_sparse(i):
        V_ = nc.vector
        sbm_bf = cbf.ap[:, CBF_SBM:CBF_SBM + 128]
        iota_p = c32.ap[:, C32_IOTA:C32_IOTA + 1]
        with ExitStack() as st:
            dest = [mk(st, "dest%d" % k, [128, NCH], I32) for k in range(2)]
            w12 = mk(st, "w12", [128, NCH, 2])
            idxb = mk(st, "idxb", [128, 2, 64], I32)
            u_sh = add_mod_units(i, 3)
            u_sc = add_mod_units(i, 4)
            u_g = add_mod_units(i, 5)
            with ExitStack() as st1:
                hb = mk(st1, "hb", [128, NCH, D], BF16)
                hTc = [mk(st1, "hTc%d" % k, [128, KC, 128], BF16) for k in range(2)]
                scr = [mk(st1, "sscr%d" % k, [128, D]) for k in range(2)]
                ss = mk(st1, "sss", [128, NCH])
                rstd = mk(st1, "srstd", [128, NCH])
                hlo = mk(st1, "shlo", [128, KC, 128], BF16)
                rwhi = mk(st1, "srwhi", [128, KC, 36], BF16)
                rwlo = mk(st1, "srwlo", [128, KC, 36], BF16)
                rw = mk(st1, "srw", [128, KC, 36])
                rb = mk(st1, "srb", [128, 36])
                lg = mk(st1, "slg", [128, 36])
                rt = mk(st1, "srt", [128, 64])
                em = mk(st1, "sem", [128, 32])
                m12 = [mk(st1, "m12_%d" % k, [128, NCH, NE]) for k in range(2)]
                ohb = mk(st1, "ohb", [128, NCH, NE], BF16)
                rank = mk(st1, "rank", [128, NCH, NE])
                cmpb = mk(st1, "cmpb", [128, 64, NE])
                cnt = mk(st1, "cnt", [128, 6, NE])
                ebf = mk(st1, "ebf", [128, 64])
                destf = mk(st1, "destf", [128, NCH])
                fw.dma(SP, [(rw.ap[:], rw_d[i].rearrange("(kc p) n -> p kc n", p=128))], writes=[rw.t])
                fw.dma(SP, [(rb.ap[:], rb_d[i:i + 1, :].broadcast_to([128, 36]))], writes=[rb.t])
                fw.op(DVE, lambda: V_.tensor_copy(out=rwhi.ap[:], in_=rw.ap[:]), reads=[rw.t], writes=[rwhi.t])
                fw.op(DVE, lambda: V_.tensor_tensor(out=rwlo.ap[:], in0=rw.ap[:], in1=rwhi.ap[:], op=ALU.subtract), reads=[rw.t, rwhi.t], writes=[rwlo.t])
                gen_mod(i, 3, BS, u_sh)
                gen_mod(i, 4, BA, u_sc, norm_g=n2_d[i:i + 1, :], tmp=scr[0])
                gen_mod(i, 5, BG, u_g)
                r_ = rt.ap

                lga = mk(st1, "lga", [128, NCH, 36])
                ema = mk(st1, "ema", [128, NCH, NE])
                g4 = [mk(st1, "g4_%d" % k, [128, NCH, 4]) for k in range(3)]
                r16 = [mk(st1, "r16_%d" % k, [128, NCH]) for k in range(8)]

                def router_cb(c, h, pss, hk):
                    for b in range(2):
                        fw.op(DVE, lambda b=b: V_.tensor_tensor(out=hlo.ap[:, 4 * b:4 * b + 4, :], in0=pss[b].ap[:, :].rearrange("p (k t) -> p k t", k=4),
                                                               in1=hk.ap[:, 4 * b:4 * b + 4, :], op=ALU.subtract), reads=[pss[b].t, hk.t], writes=[hlo.t])
                    fw.op(ACT, lambda: nc.scalar.copy(out=hb.ap[:, c, :], in_=h.ap[:]), reads=[h.t], writes=[hb.t])
                    ps = PS[c % 2]
                    fns = []
                    for kc in range(KC):
                        fns.append(lambda kc=kc: nc.tensor.matmul(ps.ap[:, 0:36], hk.ap[:, kc, :], rwhi.ap[:, kc, :], start=(kc == 0), stop=False))
                        fns.append(lambda kc=kc: nc.tensor.matmul(ps.ap[:, 0:36], hlo.ap[:, kc, :], rwhi.ap[:, kc, :], start=False, stop=False))
                        fns.append(lambda kc=kc: nc.tensor.matmul(ps.ap[:, 0:36], hk.ap[:, kc, :], rwlo.ap[:, kc, :], start=False, stop=(kc == KC - 1)))
                    fw.op(PE, fns, reads=[hk.t, hlo.t, rwhi.t, rwlo.t], writes=[ps.t])
                    fw.op(DVE, lambda: V_.tensor_tensor(out=lga.ap[:, c, :], in0=ps.ap[:, 0:36], in1=rb.ap[:], op=ALU.add), reads=[ps.t, rb.t], writes=[lga.t])

                norm_modulate(BA, BS, None, scr, ss, rstd, hT32_cb=router_cb, hT_chunks=hTc)

                def bc(ap2, n):
                    return ap2.unsqueeze(2).to_broadcast([128, NCH, n])
                gl = lga.ap[:, :, 0:4]
                gmax, gsum, pg, top1, top2, dd, ed, tmp = [r.ap[:] for r in r16]
                T16 = [r.t for r in r16]
                G4 = [g.t for g in g4]

                def dv(fn, reads, writes):
                    fw.op(DVE, fn, reads=reads, writes=writes)
                dv(lambda: V_.tensor_reduce(out=gmax, in_=gl, axis=AX.X, op=ALU.max), [lga.t], [T16[0]])
                dv(lambda: V_.tensor_tensor(out=g4[0].ap[:], in0=gl, in1=bc(gmax, 4), op=ALU.is_equal), [lga.t, T16[0]], [G4[0]])
                dv(lambda: V_.tensor_tensor(out=g4[1].ap[:], in0=gl, in1=bc(gmax, 4), op=ALU.subtract), [lga.t, T16[0]], [G4[1]])
                fw.op(ACT, lambda: nc.scalar.activation(out=g4[1].ap[:], in_=g4[1].ap[:], func=AF.Exp), reads=[G4[1]], writes=[G4[1]])
                dv(lambda: V_.tensor_reduce(out=gsum, in_=g4[1].ap[:], axis=AX.X, op=ALU.add), [G4[1]], [T16[1]])
                dv(lambda: V_.reciprocal(out=pg, in_=gsum), [T16[1]], [T16[2]])
                dv(lambda: V_.tensor_scalar(out=g4[2].ap[:], in0=g4[0].ap[:], scalar1=-1.0, scalar2=1.0e9, op0=ALU.add, op1=ALU.mult), [G4[0]], [G4[2]])
                em4 = ema.ap[:, :, :].rearrange("p c (g e) -> p c g e", g=4)
                ea4 = lga.ap[:, :, 4:36].rearrange("p c (g e) -> p c g e", g=4)
                dv(lambda: V_.tensor_tensor(out=em4, in0=ea4, in1=g4[2].ap[:].unsqueeze(3).to_broadcast([128, NCH, 4, 8]), op=ALU.add), [lga.t, G4[2]], [ema.t])
                dv(lambda: V_.tensor_reduce(out=top1, in_=ema.ap[:], axis=AX.X, op=ALU.max), [ema.t], [T16[3]])
                dv(lambda: V_.tensor_tensor(out=m12[0].ap[:], in0=ema.ap[:], in1=bc(top1, NE), op=ALU.is_equal), [ema.t, T16[3]], [m12[0].t])
                dv(lambda: V_.scalar_tensor_tensor(out=ema.ap[:], in0=m12[0].ap[:], scalar=-1.0e9, in1=ema.ap[:], op0=ALU.mult, op1=ALU.add), [m12[0].t, ema.t], [ema.t])
                dv(lambda: V_.tensor_reduce(out=top2, in_=ema.ap[:], axis=AX.X, op=ALU.max), [ema.t], [T16[4]])
                dv(lambda: V_.tensor_tensor(out=m12[1].ap[:], in0=ema.ap[:], in1=bc(top2, NE), op=ALU.is_equal), [ema.t, T16[4]], [m12[1].t])
                dv(lambda: V_.tensor_tensor(out=dd, in0=top2, in1=top1, op=ALU.subtract), [T16[3], T16[4]], [T16[5]])
                fw.op(ACT, lambda: nc.scalar.activation(out=ed, in_=dd, func=AF.Exp), reads=[T16[5]], writes=[T16[6]])
                dv(lambda: V_.tensor_scalar(out=tmp, in0=ed, scalar1=1.0, scalar2=None, op0=ALU.add), [T16[6]], [T16[7]])
                dv(lambda: V_.reciprocal(out=tmp, in_=tmp), [T16[7]], [T16[7]])
                dv(lambda: V_.tensor_tensor(out=w12.ap[:, :, 0], in0=tmp, in1=pg, op=ALU.mult), [T16[7], T16[2]], [w12.t])
                dv(lambda: V_.tensor_tensor(out=w12.ap[:, :, 1], in0=w12.ap[:, :, 0], in1=ed, op=ALU.mult), [w12.t, T16[6]], [w12.t])
                dv(lambda: V_.tensor_tensor(out=ohb.ap[:], in0=m12[0].ap[:], in1=m12[1].ap[:], op=ALU.add), [m12[0].t, m12[1].t], [ohb.t])
                pr = PS[2]
                fns = []
                for c in range(NCH):
                    for c2 in range(c):
                        fns.append(lambda c=c, c2=c2: nc.tensor.matmul(pr.ap[:, c * 32:(c + 1) * 32], ones_bf, ohb.ap[:, c2, :], start=(c2 == 0), stop=False))
                    fns.append(lambda c=c: nc.tensor.matmul(pr.ap[:, c * 32:(c + 1) * 32], sbm_bf, ohb.ap[:, c, :], start=(c == 0), stop=True))
                fw.op(PE, fns, reads=[ohb.t, cbf.t], writes=[pr.t])
                dv(lambda: V_.tensor_copy(out=rank.ap[:], in_=pr.ap[:, :].rearrange("p (c e) -> p c e", e=NE)), [pr.t], [rank.t])
                pc = PS[1]
                fw.op(PE, [lambda c2=c2: nc.tensor.matmul(pc.ap[:, 0:32], ones_bf, ohb.ap[:, c2, :], start=(c2 == 0), stop=(c2 == NCH - 1)) for c2 in range(NCH)],
                      reads=[ohb.t, cbf.t], writes=[pc.t])
                cn = cnt.ap
                fw.op(DVE, lambda: V_.tensor_copy(out=cn[:, 0, :], in_=pc.ap[:, 0:32]), reads=[pc.t], writes=[cnt.t])
                fw.op(DVE, lambda: V_.memset(cn[:, 1, :], 0.0), writes=[cnt.t])
                for j in range(16):
                    fw.op(DVE, lambda j=j: V_.scalar_tensor_tensor(out=cn[:, 1, :], in0=cn[:, 0, :], scalar=float(128 * j), in1=cn[:, 1, :], op0=ALU.is_gt, op1=ALU.add),
                          reads=[cnt.t], writes=[cnt.t])
                fw.op(DVE, lambda: V_.tensor_tensor_scan(out=cn[:, 2, :], data0=cn[:, 1, :], data1=cn[:, 1, :], initial=0.0, op0=ALU.add, op1=ALU.bypass),
                      reads=[cnt.t], writes=[cnt.t])
                fw.op(DVE, lambda: V_.tensor_tensor(out=cn[:, 3, :], in0=cn[:, 2, :], in1=cn[:, 1, :], op=ALU.subtract), reads=[cnt.t], writes=[cnt.t])
                fw.op(DVE, lambda: V_.tensor_scalar(out=cn[:, 3, :], in0=cn[:, 3, :], scalar1=128.0, scalar2=None, op0=ALU.mult), reads=[cnt.t], writes=[cnt.t])
                dump("cnt%d" % i, cn[:, 0:4, :], [128, 4, NE], [cnt.t])
                for c in range(NCH):
                    fw.op(DVE, lambda c=c: V_.tensor_tensor(out=rank.ap[:, c, :], in0=rank.ap[:, c, :], in1=cn[:, 3, :], op=ALU.add), reads=[rank.t, cnt.t], writes=[rank.t])
                for k in range(2):
                    fw.op(DVE, lambda k=k: V_.tensor_tensor(out=m12[k].ap[:], in0=m12[k].ap[:], in1=rank.ap[:], op=ALU.mult), reads=[m12[k].t, rank.t], writes=[m12[k].t])
                    fw.op(DVE, lambda k=k: V_.reduce_sum(out=destf.ap[:], in_=m12[k].ap[:], axis=AX.X), reads=[m12[k].t], writes=[destf.t])
                    fw.op(DVE, lambda k=k: V_.tensor_copy(out=dest[k].ap[:], in_=destf.ap[:]), reads=[destf.t], writes=[dest[k].t])
                    dump("dest%d_%d" % (k, i), destf.ap[:], [128, NCH], [destf.t])
                for b in range(64):
                    fw.op(DVE, lambda b=b: V_.tensor_scalar(out=cmpb.ap[:, b, :], in0=cn[:, 2, :], scalar1=float(b), scalar2=None, op0=ALU.is_le), reads=[cnt.t], writes=[cmpb.t])
                fw.op(DVE, lambda: V_.reduce_sum(out=ebf.ap[:], in_=cmpb.ap[:], axis=AX.X), reads=[cmpb.t], writes=[ebf.t])
                fw.op(DVE, lambda: V_.tensor_scalar(out=destf.ap[:, 0:1], in0=ebf.ap[:, 0:1], scalar1=0.0, scalar2=None, op0=ALU.mult), reads=[ebf.t], writes=[destf.t])
                ebig = mk(st1, "ebig", [128, 64])
                fw.op(DVE, lambda: V_.tensor_scalar(out=ebig.ap[:], in0=ebf.ap[:], scalar1=float(NE), scalar2=1.0e9, op0=ALU.is_ge, op1=ALU.mult), reads=[ebf.t], writes=[ebig.t])
                esame = mk(st1, "esame", [128, 64])
                fw.op(DVE, lambda: V_.memset(esame.ap[:], 0.0), writes=[esame.t])
                fw.op(DVE, lambda: V_.tensor_tensor(out=esame.ap[:, 2:64], in0=ebf.ap[:, 2:64], in1=ebf.ap[:, 0:62], op=ALU.is_equal), reads=[ebf.t], writes=[esame.t])
                fw.op(DVE, lambda: V_.scalar_tensor_tensor(out=ebig.ap[:], in0=esame.ap[:], scalar=1.0e9, in1=ebig.ap[:], op0=ALU.mult, op1=ALU.add),
                      reads=[esame.t, ebig.t], writes=[ebig.t])
                fw.op(DVE, lambda: V_.tensor_scalar(out=ebf.ap[:], in0=ebf.ap[:], scalar1=float(NE - 1), scalar2=128.0, op0=ALU.min, op1=ALU.mult), reads=[ebf.t], writes=[ebf.t])
                fw.op(DVE, lambda: V_.tensor_scalar(out=ebf.ap[:], in0=ebf.ap[:], scalar1=iota_p, scalar2=None, op0=ALU.add), reads=[ebf.t, c32.t], writes=[ebf.t])
                fw.op(DVE, lambda: V_.tensor_scalar(out=ebf.ap[:], in0=ebf.ap[:], scalar1=2.0, scalar2=float(i * NE * 128 * 2), op0=ALU.mult, op1=ALU.add), reads=[ebf.t], writes=[ebf.t])
                fw.op(DVE, lambda: V_.tensor_tensor(out=ebf.ap[:], in0=ebf.ap[:], in1=ebig.ap[:], op=ALU.add), reads=[ebf.t, ebig.t], writes=[ebf.t])
                fw.op(DVE, lambda: V_.tensor_copy(out=idxb.ap[:, 0, :], in_=ebf.ap[:]), reads=[ebf.t], writes=[idxb.t])
                fw.op(DVE, lambda: V_.tensor_scalar(out=ebf.ap[:], in0=ebf.ap[:], scalar1=1.0, scalar2=None, op0=ALU.add), reads=[ebf.t], writes=[ebf.t])
                fw.op(DVE, lambda: V_.tensor_copy(out=idxb.ap[:, 1, :], in_=ebf.ap[:]), reads=[ebf.t], writes=[idxb.t])
                dump("ebf%d" % i, ebf.ap[:], [128, 64], [ebf.t])
                for c in range(NCH):
                    for k in range(2):
                        fw.dma_ind(xs_d[:, :], hb.ap[:, c, :], dest[k].ap[:, c:c + 1], True, reads=[hb.t, dest[k].t], writes=[xs_t], waw=False)
            fw.barrier()
            with ExitStack() as st2:
                xsb = [mk(st2, "xsb%d" % k, [128, D], BF16) for k in range(3)]
                xsT = [mk(st2, "xsT%d" % k, [128, KC, 128], BF16) for k in range(2)]
                sgb = [mk(st2, "sgb%d" % k, [128, 512], BF16) for k in range(2)]
                hdb = [mk(st2, "hdb%d" % k, [128, 512], BF16) for k in range(2)]
                hdT = [mk(st2, "hdT%d" % k, [128, 4, 128], BF16) for k in range(2)]
                yst = [mk(st2, "yst%d" % k, [128, D]) for k in range(2)]
                tabs = [wg_d.rearrange("l e (p a k) f -> (l e p a) (k f)", a=2, k=4), wu_d.rearrange("l e (p a k) f -> (l e p a) (k f)", a=2, k=4),
                        wd_d.rearrange("l e (p a k) d -> (l e p a) (k d)", a=2, k=2)]

                def blk_units(b):
                    us = []
                    for tab in tabs:
                        def f(slot_ap, tab=tab, b=b):
                            def emit(slot):
                                fw.dma_ind([slot.ap[:, hf * 2048:(hf + 1) * 2048] for hf in range(2)], [tab[:, :]] * 2,
                                           [idxb.ap[:, hf, b:b + 1] for hf in range(2)], False, reads=[idxb.t], writes=[slot.t], bounds=2 * NE * 128 * 2 - 1)
                            return emit
                        us.append(ws.add(f))
                    return us
                NB = 64 if upto != "moe_few" else 3
                bu = [blk_units(b) for b in range(NB)]
                for u in bu[0]:
                    ws.ensure(u)
                pT = PS[0].ap[:, :].bitcast(BF16)
                pH = PS[5].ap[:, :].bitcast(BF16)

                def stA(b):
                    for u in bu[b]:
                        ws.ensure(u)
                    x_ = xsb[b % 3]
                    if b == 0:
                        fw.dma(SP, [(x_.ap[:], xs_d[0:128, :])], reads=[xs_t], writes=[x_.t])
                    if b + 1 < NB:
                        xn = xsb[(b + 1) % 3]
                        fw.dma(SP, [(xn.ap[:], xs_d[(b + 1) * 128:(b + 2) * 128, :])], reads=[xs_t], writes=[xn.t])
                    xv = x_.ap[:, :].rearrange("s (p k) -> s k p", k=8)
                    fw.op(PE, [lambda kc=kc: nc.tensor.transpose(pT[:, kc * 128:(kc + 1) * 128], xv[:, kc, :], ident_bf) for kc in range(KC)],
                          reads=[x_.t, cbf.t], writes=[PS[0].t])
                    xT = xsT[b % 2]
                    fw.op(ACT, lambda: nc.scalar.copy(out=xT.ap[:, 0:4, :], in_=pT[:, 0:512].rearrange("p (k t) -> p k t", k=4)), reads=[PS[0].t], writes=[xT.t])
                    fw.op(DVE, lambda: V_.tensor_copy(out=xT.ap[:, 4:8, :], in_=pT[:, 512:1024].rearrange("p (k t) -> p k t", k=4)), reads=[PS[0].t], writes=[xT.t])
                    Wg, Wu, Wd = [ws.get(u) for u in bu[b]]
                    wgv = Wg.ap[:, :].rearrange("p (k f) -> p k f", k=8)
                    wuv = Wu.ap[:, :].rearrange("p (k f) -> p k f", k=8)
                    pg = PS[1 + b % 2]
                    pu = PS[3 + b % 2]
                    fw.op(PE, [lambda kc=kc: nc.tensor.matmul(pg.ap[:], xT.ap[:, kc, :], wgv[:, kc, :], start=(kc == 0), stop=(kc == KC - 1)) for kc in range(KC)],
                          reads=[xT.t, Wg.t], writes=[pg.t])
                    fw.op(PE, [lambda kc=kc: nc.tensor.matmul(pu.ap[:], xT.ap[:, kc, :], wuv[:, kc, :], start=(kc == 0), stop=(kc == KC - 1)) for kc in range(KC)],
                          reads=[xT.t, Wu.t], writes=[pu.t])

                def stB(b):
                    Wg, Wu, Wd = [ws.get(u) for u in bu[b]]
                    wdv = Wd.ap[:, :].rearrange("p (k d) -> p k d", k=4)
                    pg = PS[1 + b % 2]
                    pu = PS[3 + b % 2]
                    sg_ = sgb[b % 2]
                    hd = hdb[b % 2]
                    hT_ = hdT[b % 2]
                    ys_ = yst[b % 2]
                    fw.op(ACT, lambda: nc.scalar.activation(out=sg_.ap[:], in_=pg.ap[:], func=AF.Silu), reads=[pg.t], writes=[sg_.t])
                    fw.op(DVE, lambda: V_.tensor_tensor(out=hd.ap[:], in0=pu.ap[:], in1=sg_.ap[:], op=ALU.mult), reads=[pu.t, sg_.t], writes=[hd.t])
                    hv = hd.ap[:, :].rearrange("s (p k) -> s k p", k=4)
                    fw.op(PE, [lambda fc=fc: nc.tensor.transpose(pH[:, fc * 128:(fc + 1) * 128], hv[:, fc, :], ident_bf) for fc in range(4)],
                          reads=[hd.t, cbf.t], writes=[PS[5].t])
                    fw.op(DVE, lambda: V_.tensor_copy(out=hT_.ap[:], in_=pH[:, 0:512].rearrange("p (k t) -> p k t", k=4)), reads=[PS[5].t], writes=[hT_.t])
                    for hf in range(2):
                        py = PS[6 + hf]
                        fw.op(PE, [lambda fc=fc, py=py, hf=hf: nc.tensor.matmul(py.ap[:], hT_.ap[:, fc, :], wdv[:, fc, hf * 512:(hf + 1) * 512], start=(fc == 0), stop=(fc == 3))
                                   for fc in range(4)], reads=[hT_.t, Wd.t], writes=[py.t])
                        fw.op(DVE, lambda py=py, hf=hf: V_.tensor_tensor(out=ys_.ap[:, hf * 512:(hf + 1) * 512], in0=py.ap[:], in1=BG.ap[:, hf * 512:(hf + 1) * 512], op=ALU.mult),
                              reads=[py.t, BG.t], writes=[ys_.t])
                    fw.dma(SP, [(ys_d[b * 128:(b + 1) * 128, :], ys_.ap[:])], reads=[ys_.t], writes=[ys_t], waw=False)
                    for u in bu[b]:
                        ws.release(u)

                stA(0)
                for b in range(NB):
                    if b + 1 < NB:
                        stA(b + 1)
                    stB(b)
            fw.barrier()
            with ExitStack() as st3:
                y0 = [mk(st3, "y0_%d" % k, [128, D]) for k in range(2)]
                y1 = [mk(st3, "y1_%d" % k, [128, D]) for k in range(2)]
                for c in range(NCH):
                    a0 = y0[c % 2]
                    a1 = y1[c % 2]
                    fw.dma_ind(a0.ap[:], ys_d[:, :], dest[0].ap[:, c:c + 1], False, reads=[ys_t, dest[0].t], writes=[a0.t])
                    fw.dma_ind(a1.ap[:], ys_d[:, :], dest[1].ap[:, c:c + 1], False, reads=[ys_t, dest[1].t], writes=[a1.t])
                    fw.op(DVE, lambda: V_.scalar_tensor_tensor(out=X[:, c, :], in0=a0.ap[:], scalar=w12.ap[:, c, 0:1], in1=X[:, c, :], op0=ALU.mult, op1=ALU.add),
                          reads=[a0.t, w12.t, TX[c]], writes=[TX[c]])
                    fw.op(DVE, lambda: V_.scalar_tensor_tensor(out=X[:, c, :], in0=a1.ap[:], scalar=w12.ap[:, c, 1:2], in1=X[:, c, :], op0=ALU.mult, op1=ALU.add),
                          reads=[a1.t, w12.t, TX[c]], writes=[TX[c]])
            fw.barrier()

    def sb_attention_heads(st, hT):
        QT = mk(st, "sQT", [128, 2, S], BF16)
        fw.op(DVE, lambda: nc.vector.memset(QT.ap[:], 0.0), writes=[QT.t])
        KT = mk(st, "sKT", [128, S], BF16)
        V = mk(st, "sV", [128, NCH, 128], BF16)
        OT = mk(st, "sOT", [128, S], BF16)
        e1 = [mk(st, "e1_%d" % k, [128, 512]) for k in range(1)]
        spb = [mk(st, "sp_%d" % k, [128, 512], BF16) for k in range(6)]
        tt_ = [mk(st, "t_%d" % k, [128, 512]) for k in range(3)]
        Ab = [mk(st, "A_%d" % k, [128, 512], BF16) for k in range(4)]
        sbm_bf = cbf.ap[:, CBF_SBM:CBF_SBM + 128]
        tle = mk(st, "tle", [128, 128], BF16)
        zer = mk(st, "zer", [128, 512], BF16)
        fw.op(DVE, lambda: nc.vector.memset(zer.ap[:], 0.0), writes=[zer.t])
        fw.op(DVE, lambda: nc.vector.tensor_tensor(out=tle.ap[:], in0=ones_bf, in1=tgt_bf, op=ALU.subtract), reads=[cbf.t], writes=[tle.t])

        def pair_unit(p):
            def f(slot):
                v = slot[:, 0:3072].rearrange("p (kc f) -> p kc f", kc=KC)
                return [(v[:, :, j * 128:(j + 1) * 128], swin_d[0, :, j * D + p * 128: j * D + (p + 1) * 128].rearrange("(kc p) f -> p kc f", p=128))
                        for j in range(3)]
            return ws.add(f)
        u_pair = [pair_unit(p) for p in range(8)]
        u_wo = [ws.add(lambda slot, hf=hf: [(slot[:, :].rearrange("p (h f) -> p h f", h=8),
                                              swout_d[0, :, hf * 512:(hf + 1) * 512].rearrange("(h p) f -> p h f", p=128))]) for hf in range(2)]
        ws.ensure(u_pair[0])
        ws.ensure(u_wo[0])
        ws.ensure(u_wo[1])
        Wo = []
        for hf in range(2):
            w = ws.get(u_wo[hf])
            wv = w.ap[:, :].rearrange("p (h f) -> p h f", h=8)
            for h in range(8):
                fw.op(DVE, lambda wv=wv, h=h, hf=hf: nc.vector.tensor_tensor(out=wv[:, h, :], in0=wv[:, h, :], in1=BG.ap[:, hf * 512:(hf + 1) * 512], op=ALU.mult),
                      reads=[w.t, BG.t], writes=[w.t])
            Wo.append((w, wv))
        ev = [0]

        def evac(dst_ap, dst_t, ps, scale=None, src_ap=None):
            src = ps.ap[:] if src_ap is None else src_ap
            ev[0] += 1
            if ev[0] % 2 == 0:
                if scale is None:
                    fw.op(ACT, lambda: nc.scalar.copy(out=dst_ap, in_=src), reads=[ps.t], writes=[dst_t])
                else:
                    fw.op(ACT, lambda: nc.scalar.mul(out=dst_ap, in_=src, mul=scale), reads=[ps.t], writes=[dst_t])
            else:
                if scale is None:
                    fw.op(DVE, lambda: nc.vector.tensor_copy(out=dst_ap, in_=src), reads=[ps.t], writes=[dst_t])
                else:
                    fw.op(DVE, lambda: nc.vector.tensor_scalar(out=dst_ap, in0=src, scalar1=scale, scalar2=None, op0=ALU.mult), reads=[ps.t], writes=[dst_t])
        rot = [0]

        def zbank():
            rot[0] += 1
            return PS[4 + rot[0] % 4]

        n_pairs = 8 if upto != "attn1_few" else 1
        for p in range(n_pairs):
            if p + 1 < n_pairs:
                ws.ensure(u_pair[p + 1])
            W = ws.get(u_pair[p])
            Wv = W.ap[:, 0:3072].rearrange("p (kc f) -> p kc f", kc=KC)
            for which, dst, scale in ((0, QT, 0.125), (1, KT, None)):
                for tq in range(4):
                    ps = zbank()
                    fw.op(PE, [lambda kc=kc, ps=ps, which=which, tq=tq: nc.tensor.matmul(ps.ap[:], Wv[:, kc, which * 128:(which + 1) * 128],
                                                                                        hT.ap[:, kc, tq * 512:(tq + 1) * 512], start=(kc == 0), stop=(kc == KC - 1))
                               for kc in range(KC)], reads=[W.t, hT.t], writes=[ps.t])
                    if which == 0:
                        for m in range(2):
                            rws = slice(m * 64, (m + 1) * 64)
                            evac(QT.ap[rws, m, tq * 512:(tq + 1) * 512], QT.t, ps, scale, src_ap=ps.ap[rws, :])
                    else:
                        evac(dst.ap[:, tq * 512:(tq + 1) * 512], dst.t, ps, scale)
            for g in range(4):
                ps = zbank()
                fw.op(PE, [lambda kc=kc, k=k, ps=ps, g=g: nc.tensor.matmul(ps.ap[:, k * 128:(k + 1) * 128], hT.ap[:, kc, (4 * g + k) * 128:(4 * g + k + 1) * 128],
                                                                          Wv[:, kc, 256:384], start=(kc == 0), stop=(kc == KC - 1))
                           for k in range(4) for kc in range(KC)], reads=[W.t, hT.t], writes=[ps.t])
                evac(V.ap[:, 4 * g:4 * g + 4, :], V.t, ps, None, src_ap=ps.ap[:, :].rearrange("p (k e) -> p k e", k=4))
            ws.release(u_pair[p])
            Oacc = [PS[0], PS[1]]
            Bacc = [PS[2], PS[3]]
            items = [(qt, sblk, hh) for qt in range(4) for sblk in range(4 * qt + 3, -1, -1) for hh in range(2)]
            LOOK = 2

            def bufs(n):
                return PS[4 + n % 4], e1[0], spb[n % 6], tt_[n % 3], Ab[n % 4]

            def geo(n):
                qt, sblk, hh = items[n]
                r = sblk - 4 * qt
                c0 = max(0, r) * 128
                return qt, sblk, hh, qt * 512, r, c0, slice(c0, 512)

            def s1_pa(n):
                qt, sblk, hh, q0, r, c0, cs = geo(n)
                pz, E1, SP_, T_, A_ = bufs(n)
                rows = slice(hh * 64, (hh + 1) * 64)
                fw.op(PE, lambda: nc.tensor.matmul(pz.ap[:, c0:512], KT.ap[:, sblk * 128:(sblk + 1) * 128], QT.ap[:, hh, q0 + c0:q0 + 512], start=True, stop=True),
                      reads=[KT.t, QT.t], writes=[pz.t])
                fw.op(ACT, lambda: nc.scalar.activation(out=E1.ap[:, cs], in_=pz.ap[:, cs], func=AF.Exp), reads=[pz.t], writes=[E1.t])
                fw.op(ACT, lambda: nc.scalar.activation(out=SP_.ap[:, cs], in_=E1.ap[:, cs], func=AF.Ln, bias=1.0, scale=1.0), reads=[E1.t], writes=[SP_.t])

            def s1_d(n):
                qt, sblk, hh, q0, r, c0, cs = geo(n)
                pz, E1, SP_, T_, A_ = bufs(n)
                if r >= 0:
                    fw.op(DVE, lambda: nc.vector.tensor_tensor(out=SP_.ap[:, c0:c0 + 128], in0=SP_.ap[:, c0:c0 + 128], in1=sbm_bf, op=ALU.mult),
                          reads=[SP_.t, cbf.t], writes=[SP_.t])
                fw.op(DVE, lambda: nc.vector.tensor_tensor(out=T_.ap[:, cs], in0=pz.ap[:, cs], in1=SP_.ap[:, cs], op=ALU.subtract), reads=[pz.t, SP_.t], writes=[T_.t])

            def s2_p(n):
                qt, sblk, hh, q0, r, c0, cs = geo(n)
                pz, E1, SP_, T_, A_ = bufs(n)
                B = Bacc[hh]
                Oa = Oacc[hh]
                if sblk == 4 * qt + 3:
                    for acc in (B, Oa):
                        fw.op(PE, lambda acc=acc: nc.tensor.matmul(acc.ap[:, 0:512], tgt_bf, zer.ap[:, 0:512], start=True, stop=False), reads=[zer.t, cbf.t], writes=[acc.t])
                fw.op(PE, lambda: nc.tensor.matmul(B.ap[:, cs], tgt_bf, SP_.ap[:, cs], start=False, stop=(sblk == 0)), reads=[SP_.t, cbf.t], writes=[B.t])

            def s2_d(n):
                qt, sblk, hh, q0, r, c0, cs = geo(n)
                pz, E1, SP_, T_, A_ = bufs(n)
                B = Bacc[hh]
                fw.op(DVE, lambda: nc.vector.tensor_tensor(out=T_.ap[:, cs], in0=T_.ap[:, cs], in1=B.ap[:, cs], op=ALU.subtract), reads=[T_.t, B.t], writes=[T_.t])

            def s2_a(n):
                qt, sblk, hh, q0, r, c0, cs = geo(n)
                pz, E1, SP_, T_, A_ = bufs(n)
                fw.op(ACT, lambda: nc.scalar.activation(out=A_.ap[:, cs], in_=T_.ap[:, cs], func=AF.Exp), reads=[T_.t], writes=[A_.t])
                if r >= 0:
                    fw.op(DVE, lambda: nc.vector.tensor_tensor(out=A_.ap[:, c0:c0 + 128], in0=A_.ap[:, c0:c0 + 128], in1=sbm_bf, op=ALU.mult),
                          reads=[A_.t, cbf.t], writes=[A_.t])

            def s2b(n):
                qt, sblk, hh, q0, r, c0, cs = geo(n)
                pz, E1, SP_, T_, A_ = bufs(n)
                B = Bacc[hh]
                Oa = Oacc[hh]
                last = (sblk == 0)
                if not last:
                    fw.op(PE, lambda: nc.tensor.matmul(B.ap[:, cs], tle.ap[:], SP_.ap[:, cs], start=False, stop=False), reads=[SP_.t, tle.t], writes=[B.t])
                fw.op(PE, lambda: nc.tensor.matmul(Oa.ap[:, cs], V.ap[:, sblk, :], A_.ap[:, cs], start=False, stop=last), reads=[V.t, A_.t], writes=[Oa.t])
                if last:
                    rows = slice(hh * 64, (hh + 1) * 64)
                    evac(OT.ap[rows, q0:q0 + 512], OT.t, Oa, None, src_ap=Oa.ap[rows, :])

            NI = len(items)
            for n in range(min(LOOK, NI)):
                s1_pa(n)
                s1_d(n)
            for t in range(NI + 1):
                if t + LOOK < NI:
                    s1_pa(t + LOOK)
                if t < NI:
                    s2_p(t)
                    s2_d(t)
                if t + LOOK < NI:
                    s1_d(t + LOOK)
                if t < NI:
                    s2_a(t)
                if t >= 1:
                    s2b(t - 1)
            if p == 0:
                dump("sOT0", OT.ap[:], [128, S], [OT.t])
            for c in range(NCH):
                for hf in range(2):
                    ps = zbank()
                    w, wv = Wo[hf]
                    fw.op(PE, lambda ps=ps, c=c, wv=wv, p=p: nc.tensor.matmul(ps.ap[:], OT.ap[:, c * 128:(c + 1) * 128], wv[:, p, :], start=True, stop=True),
                          reads=[OT.t, w.t], writes=[ps.t])
                    xs = X[:, c, hf * 512:(hf + 1) * 512]
                    fw.op(DVE, lambda ps=ps, xs=xs: nc.vector.tensor_tensor(out=xs, in0=ps.ap[:], in1=xs, op=ALU.add), reads=[ps.t, TX[c]], writes=[TX[c]])
        ws.release(u_wo[0])
        ws.release(u_wo[1])

    def final_norm():
        with ExitStack() as st:
            scr = [mk(st, "fscr%d" % k, [128, D]) for k in range(2)]
            ss = mk(st, "fss", [128, NCH])
            rstd = mk(st, "frstd", [128, NCH])
            fw.dma(SP, [(BA.ap[:], fng_d[0:1, :].broadcast_to([128, D]))], writes=[BA.t])
            for c in range(NCH):
                fw.op(ACT, lambda c=c: nc.scalar.activation(out=scr[0].ap[:], in_=X[:, c, :], func=AF.Square, accum_out=ss.ap[:, c:c + 1]),
                      reads=[TX[c]], writes=[scr[0].t, ss.t])
            fw.op(DVE, lambda: nc.vector.tensor_scalar(out=rstd.ap[:], in0=ss.ap[:], scalar1=1.0 / D, scalar2=RMS_EPS, op0=ALU.mult, op1=ALU.add),
                  reads=[ss.t], writes=[rstd.t])
            fw.op(POOL, lambda: nc.gpsimd.tensor_tensor(out=rstd.ap[:], in0=rstd.ap[:], in1=neghalf.ap[:, 0:NCH], op=ALU.pow),
                  reads=[rstd.t, neghalf.t], writes=[rstd.t])
            for c in range(NCH):
                o = scr[c % 2]
                fw.op(DVE, lambda c=c, o=o: nc.vector.scalar_tensor_tensor(out=o.ap[:], in0=X[:, c, :], scalar=rstd.ap[:, c:c + 1], in1=BA.ap[:], op0=ALU.mult, op1=ALU.mult),
                      reads=[TX[c], rstd.t, BA.t], writes=[o.t])
                fw.dma(SP, [(out_d[c * 128:(c + 1) * 128, :], o.ap[:])], reads=[o.t])
            fw.barrier()

    stages = ["attn0", "moe0", "attn1", "moe1", "all"]
    lvl = {"norm1": 0, "attn": 0, "attn0": 0, "moe_router": 1, "moe_few": 1, "moe0": 1, "attn1_few": 2, "attn1": 2, "moe1": 3, "all": 4}[upto]
    with ExitStack() as st:
        with ExitStack() as st2:
            hT, lambda_init = attn_norm(0, st, st2)
        fw.barrier()
        if upto != "norm1":
            diff_attention_heads(st, hT, lambda_init)
        fw.barrier()
    moe_fn = moe_layer if SPARSE_MOE is False else moe_layer_sparse
    if lvl >= 1:
        moe_fn(0)
    if lvl >= 2:
        with ExitStack() as st:
            with ExitStack() as st2:
                hT, _ = attn_norm(1, st, st2)
            fw.barrier()
            sb_attention_heads(st, hT)
            fw.barrier()
    if lvl >= 3:
        moe_fn(1)
    if lvl >= 4:
        final_norm()
    else:
        for g in range(4):
            dst = out_d[g * 512:(g + 1) * 512, :].rearrange("(c p) d -> p c d", p=128)
            fw.dma(SP, [(dst, X[:, 4 * g:4 * g + 4, :])], reads=TX[4 * g:4 * g + 4])
        fw.barrier()
    return nc


_CACHE = {}


def _prep_inputs(inputs):
    f = lambda a: np.ascontiguousarray(np.asarray(a, dtype=np.float32))
    cbf, c32 = _host_consts()
    rw = np.concatenate([np.asarray(inputs["router_group_w"]), np.asarray(inputs["router_expert_w"])], axis=-1)
    rb = np.concatenate([np.asarray(inputs["router_group_b"]), np.asarray(inputs["router_expert_b"]).reshape(2, 32)], axis=-1)
    shared = {
        "norm1_g": f(inputs["norm1_g"]), "norm2_g": f(inputs["norm2_g"]),
        "ada_w": f(inputs["ada_w"]), "ada_b": f(inputs["ada_b"]),
        "diff_w_in": f(inputs["diff_w_in"]), "diff_w_out": f(inputs["diff_w_out"]),
        "diff_lambda_q1": f(inputs["diff_lambda_q1"]), "diff_lambda_k1": f(inputs["diff_lambda_k1"]),
        "diff_lambda_q2": f(inputs["diff_lambda_q2"]), "diff_lambda_k2": f(inputs["diff_lambda_k2"]),
        "subgT": f(np.asarray(inputs["diff_subln_g"]).reshape(128, 1)),
        "sb_w_in": f(inputs["sb_w_in"]), "sb_w_out": f(inputs["sb_w_out"]),
        "router_w": f(rw), "router_b": f(rb),
        "expert_w_gate": f(inputs["expert_w_gate"]), "expert_w_up": f(inputs["expert_w_up"]),
        "expert_w_down": f(inputs["expert_w_down"]),
        "final_norm_g": f(np.asarray(inputs["final_norm_g"]).reshape(1, D)),
        "cbf": cbf, "c32": c32,
    }
    x = np.asarray(inputs["x"], dtype=np.float32)
    c = np.asarray(inputs["c"], dtype=np.float32)
    maps = []
    for b in range(x.shape[0]):
        m = dict(shared)
        m["x"] = np.ascontiguousarray(x[b])
        m["cT"] = np.ascontiguousarray(c[b].reshape(KC, 128).T)
        maps.append(m)
    return maps


def kernel(**inputs):
    maps = _prep_inputs(inputs)
    if "nc" not in _CACHE:
        _CACHE["nc"] = build_program()
    nc = _CACHE["nc"]
    res = run_bass_kernel_spmd(nc, maps, core_ids=list(range(len(maps))))
    out = np.stack([np.asarray(r["out"], dtype=np.float32) for r in res.results], axis=0)
    return out
```

```python
import math
from contextlib import ExitStack
import numpy as np
import concourse.bass as bass
import concourse.mybir as mybir
from concourse.bass_utils import run_bass_kernel_spmd
from concourse.alu_op_type import AluOpType as ALU

F32 = mybir.dt.float32
BF16 = mybir.dt.bfloat16
I32 = mybir.dt.int32
AF = mybir.ActivationFunctionType
AX = mybir.AxisListType

D = 1024
S = 2048
NCH = 16
KC = 8
NE = 32
FH = 512
RMS_EPS = 1e-6
SUBLN_EPS = 1e-5
NEG = -30000.0


class Trk:
    __slots__ = ("w", "r", "dsem", "dval", "name")

    def __init__(self, name=""):
        self.w = None
        self.r = {}
        self.dsem = None
        self.dval = 0
        self.name = name


class Eng:
    def __init__(self, nc, name, eng, selfsync):
        self.name = name
        self.eng = eng
        self.selfsync = selfsync
        self.sem = nc.alloc_semaphore(name="es_" + name)
        self.count = 0
        self.waited = {}


class FW:
    def __init__(self, nc):
        self.nc = nc
        self.pe = Eng(nc, "pe", nc.tensor, False)
        self.act = Eng(nc, "act", nc.scalar, True)
        self.dve = Eng(nc, "dve", nc.vector, True)
        self.pool = Eng(nc, "pool", nc.gpsimd, True)
        self.sp = Eng(nc, "sp", nc.sync, False)
        self.engs = [self.pe, self.act, self.dve, self.pool, self.sp]
        self.dsems = []
        self._bregs = {}
        self.ninst = 0

    def _wait(self, e, deps):
        best = {}
        for d in deps:
            if d is None:
                continue
            sem, val = d
            k = id(sem)
            if k not in best or best[k][1] < val:
                best[k] = (sem, val)
        for k, (sem, val) in best.items():
            if sem is e.sem and not e.selfsync:
                continue
            if e.waited.get(k, 0) >= val:
                continue
            e.eng.wait_ge(sem, val)
            e.waited[k] = val

    @staticmethod
    def _deps(reads, writes, waw=True):
        deps = []
        for t in reads:
            deps.append(t.w)
        for t in writes:
            if waw:
                deps.append(t.w)
            deps.extend(t.r.values())
        return deps

    def op(self, e, fns, reads=(), writes=()):
        self._wait(e, self._deps(reads, writes))
        if callable(fns):
            fns = [fns]
        inst = None
        for f in fns:
            inst = f()
            self.ninst += 1
        e.count += 1
        inst.then_inc(e.sem, 1)
        tag = (e.sem, e.count)
        for t in writes:
            t.w = tag
            t.r = {}
        for t in reads:
            t.r[e.name] = tag
        return tag

    def dma(self, e, pairs, reads=(), writes=(), waw=True, **kw):
        self._wait(e, self._deps(reads, writes, waw))
        owner = writes[0] if writes else reads[0]
        if owner.dsem is None:
            owner.dsem = self.nc.alloc_semaphore(name="ds%d" % len(self.dsems))
            self.dsems.append(owner)
        for (o, i) in pairs:
            e.eng.dma_start(out=o, in_=i, **kw).then_inc(owner.dsem, 16)
            owner.dval += 16
            self.ninst += 1
        tag = (owner.dsem, owner.dval)
        for t in writes:
            t.w = tag
            if waw:
                t.r = {}
        for t in reads:
            t.r["dma%d" % id(owner)] = tag
        return tag

    def dma_ind(self, out_ap, in_ap, idx_ap, scatter, reads=(), writes=(), waw=True, bounds=None):
        e = self.pool
        self._wait(e, self._deps(reads, writes, waw))
        owner = writes[0]
        if owner.dsem is None:
            owner.dsem = self.nc.alloc_semaphore(name="ds%d" % len(self.dsems))
            self.dsems.append(owner)
        if not isinstance(out_ap, list):
            out_ap, in_ap, idx_ap = [out_ap], [in_ap], [idx_ap]
        for o_, i_, x_ in zip(out_ap, in_ap, idx_ap):
            off = bass.IndirectOffsetOnAxis(ap=x_, axis=0)
            if scatter:
                self.nc.gpsimd.indirect_dma_start(out=o_, out_offset=off, in_=i_, in_offset=None).then_inc(owner.dsem, 16)
            else:
                kw = {}
                if bounds is not None:
                    if bounds not in self._bregs:
                        self._bregs[bounds] = self.nc.gpsimd.to_reg(bounds)
                    kw = dict(bounds_check=self._bregs[bounds], oob_is_err=False)
                self.nc.gpsimd.indirect_dma_start(out=o_, out_offset=None, in_=i_, in_offset=off, **kw).then_inc(owner.dsem, 16)
            owner.dval += 16
            self.ninst += 1
        tag = (owner.dsem, owner.dval)
        for t in writes:
            t.w = tag
            if waw:
                t.r = {}
        for t in reads:
            t.r["dma%d" % id(owner)] = tag
        return tag

    def barrier(self):
        deps = [(e.sem, e.count) for e in self.engs if e.count > 0]
        deps += [(t.dsem, t.dval) for t in self.dsems if t.dval > 0]
        for e in self.engs:
            self._wait(e, deps)


class Buf:
    def __init__(self, ap, name=""):
        self.ap = ap
        self.t = Trk(name)


def _host_consts():
    p = np.arange(128, dtype=np.float32)[:, None]
    j = np.arange(128, dtype=np.float32)[None, :]
    ident = (p == j).astype(np.float32)
    ones = np.ones((128, 128), np.float32)
    t_gt = (p > j).astype(np.float32)
    mb = np.zeros((128, 8, 128), np.float32)
    for h in range(8):
        slope = 2.0 ** (-(h + 1))
        allowed = (p // 64) <= (j // 64)
        corr = -2.0 * slope * np.maximum(p - j, 0.0)
        mb[:, h, :] = np.where(allowed, corr, NEG)
    sbm = (p < j).astype(np.float32)
    cbf = np.concatenate([ident, ones, t_gt, mb.reshape(128, 1024), sbm], axis=1)
    ab = np.zeros((128, 8, 16), np.float32)
    for h in range(8):
        slope = 2.0 ** (-(h + 1))
        for r in range(16):
            ab[:, h, r] = slope * (p[:, 0] + 1.0 - 128.0 * (r + 1))
    c32 = np.concatenate([ident, ones, ab.reshape(128, 128), sbm, np.repeat(p, 8, axis=1)], axis=1)
    return np.ascontiguousarray(cbf), np.ascontiguousarray(c32)


CBF_IDENT, CBF_ONES, CBF_TGT, CBF_MB, CBF_SBM = 0, 128, 256, 384, 1408
CBF_W = 1536
C32_IDENT, C32_ONES, C32_AB, C32_SBM = 0, 128, 256, 384
C32_IOTA = 512
C32_W = 520

SPARSE_MOE = True
NSLOT = 6


def build_program(upto="all", debug=None):
    nc = bass.Bass("TRN2", target_bir_lowering=False)
    fw = FW(nc)
    dbg_outs = {}

    def dump(name, ap, shape, trks):
        if debug is None or name not in debug:
            return
        d = nc.dram_tensor("dbg_" + name, list(shape), F32, kind="ExternalOutput").ap()
        fw.dma(fw.pool, [(d, ap)], reads=list(trks))
        dbg_outs[name] = d

    PE, ACT, DVE, POOL, SP = fw.pe, fw.act, fw.dve, fw.pool, fw.sp

    def dram_in(name, shape):
        return nc.dram_tensor(name, list(shape), F32, kind="ExternalInput").ap()

    x_d = dram_in("x", [S, D])
    cT_d = dram_in("cT", [128, KC])
    n1_d = dram_in("norm1_g", [2, D])
    n2_d = dram_in("norm2_g", [2, D])
    adaw_d = dram_in("ada_w", [2, D, 6 * D])
    adab_d = dram_in("ada_b", [2, 6 * D])
    dwin_d = dram_in("diff_w_in", [1, D, 3 * D])
    dwout_d = dram_in("diff_w_out", [1, D, D])
    lq1_d = dram_in("diff_lambda_q1", [1, 64])
    lk1_d = dram_in("diff_lambda_k1", [1, 64])
    lq2_d = dram_in("diff_lambda_q2", [1, 64])
    lk2_d = dram_in("diff_lambda_k2", [1, 64])
    subg_d = dram_in("subgT", [128, 1])
    swin_d = dram_in("sb_w_in", [1, D, 3 * D])
    swout_d = dram_in("sb_w_out", [1, D, D])
    rw_d = dram_in("router_w", [2, D, 36])
    rb_d = dram_in("router_b", [2, 36])
    wg_d = dram_in("expert_w_gate", [2, NE, D, FH])
    wu_d = dram_in("expert_w_up", [2, NE, D, FH])
    wd_d = dram_in("expert_w_down", [2, NE, FH, D])
    fng_d = dram_in("final_norm_g", [1, D])
    cbf_d = dram_in("cbf", [128, CBF_W])
    c32_d = dram_in("c32", [128, C32_W])
    out_d = nc.dram_tensor("out", [S, D], F32, kind="ExternalOutput").ap()
    NSLOTS = 8192
    xs_d = nc.dram_tensor("xs_scratch", [NSLOTS, D], BF16, kind="Internal").ap()
    ys_d = nc.dram_tensor("ys_scratch", [NSLOTS, D], F32, kind="Internal").ap()
    xs_t = Trk("xs_d")
    ys_t = Trk("ys_d")

    def sb(name, shape, dt=F32):
        return Buf(nc.alloc_sbuf_tensor("s_" + name, list(shape), dt).ap(), name)

    X = nc.alloc_sbuf_tensor("s_X", [128, NCH, D], F32).ap()
    TX = [Trk("X%d" % c) for c in range(NCH)]
    cbf = sb("cbf", [128, CBF_W], BF16)
    c32 = sb("c32", [128, C32_W], F32)
    BA = sb("BA", [128, D])
    BS = sb("BS", [128, D])
    BG = sb("BG", [128, D])
    condT = sb("condT", [128, KC])
    condrep = sb("condrep", [128, KC, 128], BF16)
    neghalf = sb("neghalf", [128, NCH])
    small = sb("small", [128, 64])
    WB = [sb("WB%d" % i, [128, 4096], BF16) for i in range(NSLOT)]
    PS = [Buf(nc.alloc_psum_tensor("ps%d" % i, [128, 512], F32).ap(), "ps%d" % i) for i in range(8)]

    ident_bf = cbf.ap[:, CBF_IDENT:CBF_IDENT + 128]
    ones_bf = cbf.ap[:, CBF_ONES:CBF_ONES + 128]
    tgt_bf = cbf.ap[:, CBF_TGT:CBF_TGT + 128]
    ident32 = c32.ap[:, C32_IDENT:C32_IDENT + 128]
    ones32 = c32.ap[:, C32_ONES:C32_ONES + 128]

    class WStream:
        def __init__(self):
            self.units = []
            self.slot_of = {}
            self.free = list(range(NSLOT))

        def add(self, pairs_fn):
            self.units.append(pairs_fn)
            return len(self.units) - 1

        def ensure(self, u):
            if u is None or u in self.slot_of:
                return
            assert self.free, "weight ring exhausted"
            si = self.free.pop(0)
            self.slot_of[u] = si
            slot = WB[si]
            r = self.units[u](slot.ap)
            if callable(r):
                r(slot)
            else:
                fw.dma(POOL, r, writes=[slot.t])

        def get(self, u):
            return WB[self.slot_of[u]]

        def release(self, u):
            self.free.append(self.slot_of[u])

    ws = WStream()

    for g in range(4):
        src = x_d[g * 512:(g + 1) * 512, :].rearrange("(c p) d -> p c d", p=128)
        fw.dma(SP, [(X[:, 4 * g:4 * g + 4, :], src)], writes=TX[4 * g:4 * g + 4])
    fw.dma(POOL, [(cbf.ap[:], cbf_d[:])], writes=[cbf.t])
    fw.dma(SP, [(c32.ap[:], c32_d[:])], writes=[c32.t])
    fw.dma(SP, [(condT.ap[:], cT_d[:])], writes=[condT.t])
    fw.op(DVE, lambda: nc.vector.memset(neghalf.ap[:], -0.5), writes=[neghalf.t])
    fw.op(ACT, lambda: nc.scalar.activation(out=condT.ap[:], in_=condT.ap[:], func=AF.Silu), reads=[condT.t], writes=[condT.t])
    for kc in range(KC):
        fw.op(DVE, lambda kc=kc: nc.vector.tensor_scalar(out=condrep.ap[:, kc, :], in0=ones32, scalar1=condT.ap[:, kc:kc + 1],
                                                         scalar2=None, op0=ALU.mult),
              reads=[condT.t, c32.t], writes=[condrep.t])

    if SPARSE_MOE:
        zt = sb("zt", [128, D], BF16)
        fw.op(DVE, lambda: nc.vector.memset(zt.ap[:], 0.0), writes=[zt.t])
        for b in range(64):
            fw.dma(SP, [(xs_d[b * 128:(b + 1) * 128, :], zt.ap[:])], reads=[zt.t], writes=[xs_t], waw=False)

    def add_mod_units(i, j):
        us = []
        for hf in range(2):
            src = adaw_d[i, :, j * D + hf * 512: j * D + (hf + 1) * 512].rearrange("(kc p) f -> p kc f", p=128)
            us.append(ws.add(lambda slot, src=src: [(slot[:, :].rearrange("p (kc f) -> p kc f", kc=KC), src)]))
        return us

    def gen_mod(i, j, dst, units, norm_g=None, tmp=None):
        fw.dma(SP, [(dst.ap[:], adab_d[i:i + 1, j * D:(j + 1) * D].broadcast_to([128, D]))], writes=[dst.t])
        if norm_g is not None:
            fw.dma(SP, [(tmp.ap[:], norm_g.broadcast_to([128, D]))], writes=[tmp.t])
        for hf in range(2):
            u = units[hf]
            ws.ensure(u)
            w = ws.get(u)
            wv = w.ap[:, :].rearrange("p (kc f) -> p kc f", kc=KC)
            ps = PS[hf]
            fw.op(PE, [lambda kc=kc: nc.tensor.matmul(ps.ap[:], condrep.ap[:, kc, :], wv[:, kc, :], start=(kc == 0), stop=(kc == KC - 1))
                       for kc in range(KC)], reads=[condrep.t, w.t], writes=[ps.t])
            ws.release(u)
            sl = slice(hf * 512, (hf + 1) * 512)
            fw.op(DVE, lambda: nc.vector.tensor_tensor(out=dst.ap[:, sl], in0=ps.ap[:], in1=dst.ap[:, sl], op=ALU.add),
                  reads=[ps.t, dst.t], writes=[dst.t])
        if norm_g is not None:
            fw.op(DVE, lambda: nc.vector.scalar_tensor_tensor(out=dst.ap[:], in0=dst.ap[:], scalar=1.0, in1=tmp.ap[:],
                                                             op0=ALU.add, op1=ALU.mult),
                  reads=[dst.t, tmp.t], writes=[dst.t])

    def norm_modulate(a, sh, hT, scr, ss, rstd, hT32_cb=None, hT_chunks=None):
        for c in range(NCH):
            fw.op(ACT, lambda c=c: nc.scalar.activation(out=scr[0].ap[:], in_=X[:, c, :], func=AF.Square,
                                                        accum_out=ss.ap[:, c:c + 1]),
                  reads=[TX[c]], writes=[scr[0].t, ss.t])
        fw.op(DVE, lambda: nc.vector.tensor_scalar(out=rstd.ap[:], in0=ss.ap[:], scalar1=1.0 / D, scalar2=RMS_EPS, op0=ALU.mult, op1=ALU.add),
              reads=[ss.t], writes=[rstd.t])
        fw.op(POOL, lambda: nc.gpsimd.tensor_tensor(out=rstd.ap[:], in0=rstd.ap[:], in1=neghalf.ap[:, 0:NCH], op=ALU.pow),
              reads=[rstd.t, neghalf.t], writes=[rstd.t])
        for c in range(NCH):
            h = scr[c % 2]
            fw.op(DVE, lambda c=c, h=h: nc.vector.scalar_tensor_tensor(out=h.ap[:], in0=X[:, c, :], scalar=rstd.ap[:, c:c + 1], in1=a.ap[:],
                                                                      op0=ALU.mult, op1=ALU.mult),
                  reads=[TX[c], rstd.t, a.t], writes=[h.t])
            fw.op(DVE, lambda h=h: nc.vector.tensor_tensor(out=h.ap[:], in0=h.ap[:], in1=sh.ap[:], op=ALU.add),
                  reads=[h.t, sh.t], writes=[h.t])
            pss = []
            for b in range(2):
                ps = PS[4 + (2 * c + b) % 4]
                fw.op(PE, [lambda k=k, ps=ps, h=h, b=b: nc.tensor.transpose(ps.ap[:, k * 128:(k + 1) * 128], h.ap[:, (4 * b + k) * 128:(4 * b + k + 1) * 128], ident32)
                           for k in range(4)], reads=[h.t, c32.t], writes=[ps.t])
                if hT is not None:
                    fw.op(ACT, lambda ps=ps, b=b, c=c: nc.scalar.copy(out=hT.ap[:, 4 * b:4 * b + 4, c * 128:(c + 1) * 128],
                                                                      in_=ps.ap[:, :].rearrange("p (k t) -> p k t", k=4)),
                          reads=[ps.t], writes=[hT.t])
                if hT_chunks is not None:
                    hk = hT_chunks[c % 2]
                    fw.op(ACT, lambda ps=ps, b=b, hk=hk: nc.scalar.copy(out=hk.ap[:, 4 * b:4 * b + 4, :], in_=ps.ap[:, :].rearrange("p (k t) -> p k t", k=4)),
                          reads=[ps.t], writes=[hk.t])
                pss.append(ps)
            if hT32_cb is not None:
                if hT_chunks is not None:
                    hT32_cb(c, h, pss, hT_chunks[c % 2])
                else:
                    hT32_cb(c, h, pss)

    def attn_norm(i, st, st2):
        lambda_init = 0.8 - 0.6 * math.exp(-0.3 * i)
        hT = Buf(st.enter_context(nc.sbuf_tensor("s_hT%d" % i, [128, KC, S], BF16)).ap(), "hT")
        scr = [Buf(st2.enter_context(nc.sbuf_tensor("s_scr%d_%d" % (k, i), [128, D], F32)).ap(), "scr%d" % k) for k in range(2)]
        ss = Buf(st2.enter_context(nc.sbuf_tensor("s_ss%d" % i, [128, NCH], F32)).ap(), "ss")
        rstd = Buf(st2.enter_context(nc.sbuf_tensor("s_rstd%d" % i, [128, NCH], F32)).ap(), "rstd")
        u_sh = add_mod_units(i, 0)
        u_sc = add_mod_units(i, 1)
        u_g = add_mod_units(i, 2)
        gen_mod(i, 0, BS, u_sh)
        gen_mod(i, 1, BA, u_sc, norm_g=n1_d[i:i + 1, :], tmp=scr[0])
        gen_mod(i, 2, BG, u_g)
        dump("BS", BS.ap[:], [128, D], [BS.t]); dump("BA", BA.ap[:], [128, D], [BA.t]); dump("BG", BG.ap[:], [128, D], [BG.t])
        norm_modulate(BA, BS, hT, scr, ss, rstd)
        dump("hT", hT.ap[:], [128, KC, S], [hT.t])
        dump("rstd", rstd.ap[:], [128, NCH], [rstd.t])
        return hT, lambda_init

    uniq = [0]

    def mk(st, name, shape, dt=F32):
        uniq[0] += 1
        return Buf(st.enter_context(nc.sbuf_tensor("s_%s_%d" % (name, uniq[0]), list(shape), dt)).ap(), name)

    def diff_attention_heads(st, hT, lambda_init):
        QT = mk(st, "QT", [128, 2, S], BF16)
        fw.op(DVE, lambda: nc.vector.memset(QT.ap[:], 0.0), writes=[QT.t])
        KT = mk(st, "KT", [128, S], BF16)
        V = mk(st, "V", [128, NCH, 128], BF16)
        Eb = [mk(st, "E%d" % k, [128, 512], BF16) for k in range(4)]
        ou = mk(st, "ou", [128, S])
        osq = [mk(st, "osq%d" % k, [128, 512], BF16) for k in range(2)]
        OT = mk(st, "OT", [128, S], BF16)
        tA = mk(st, "tA", [128, 512])
        tB = mk(st, "tB", [128, 512])
        zer = mk(st, "dzer", [128, 512], BF16)
        fw.op(DVE, lambda: nc.vector.memset(zer.ap[:], 0.0), writes=[zer.t])
        class _V:
            def __init__(self, ap, t):
                self.ap = ap
                self.t = t
        lam4 = [_V(tA.ap[:, 0:64], tA.t), _V(tA.ap[:, 64:128], tA.t), _V(tB.ap[:, 0:64], tB.t), _V(tB.ap[:, 64:128], tB.t)]
        MB = cbf.ap[:, CBF_MB:CBF_MB + 1024]
        AB = c32.ap[:, C32_AB:C32_AB + 128]
        for k, src in enumerate([lq1_d, lk1_d, lq2_d, lk2_d]):
            fw.dma(SP, [(lam4[k].ap[:], src[0:1, :].broadcast_to([128, 64]))], writes=[lam4[k].t])
        sm = small.ap
        for k in range(2):
            fw.op(DVE, lambda k=k: nc.vector.tensor_tensor(out=lam4[2 * k].ap[:], in0=lam4[2 * k].ap[:], in1=lam4[2 * k + 1].ap[:], op=ALU.mult),
                  reads=[lam4[2 * k].t, lam4[2 * k + 1].t], writes=[lam4[2 * k].t])
            fw.op(DVE, lambda k=k: nc.vector.reduce_sum(out=sm[:, k:k + 1], in_=lam4[2 * k].ap[:], axis=AX.X),
                  reads=[lam4[2 * k].t], writes=[small.t])
        fw.op(ACT, lambda: nc.scalar.activation(out=sm[:, 2:4], in_=sm[:, 0:2], func=AF.Exp), reads=[small.t], writes=[small.t])
        fw.op(DVE, lambda: nc.vector.tensor_tensor(out=sm[:, 4:5], in0=sm[:, 3:4], in1=sm[:, 2:3], op=ALU.subtract), reads=[small.t], writes=[small.t])
        fw.op(DVE, lambda: nc.vector.tensor_scalar(out=sm[:, 4:5], in0=sm[:, 4:5], scalar1=-lambda_init, scalar2=None, op0=ALU.add), reads=[small.t], writes=[small.t])
        neglam = sm[:, 4:5]
        fw.dma(SP, [(sm[:, 5:6], subg_d[:])], writes=[small.t])
        fw.op(DVE, lambda: nc.vector.tensor_scalar(out=sm[:, 5:6], in0=sm[:, 5:6], scalar1=1.0 - lambda_init, scalar2=None, op0=ALU.mult), reads=[small.t], writes=[small.t])
        gsub = sm[:, 5:6]
        fw.op(DVE, lambda: nc.vector.memset(sm[:, 6:7], SUBLN_EPS), writes=[small.t])
        epsb = sm[:, 6:7]
        dump("small", sm[:, 0:8], [128, 8], [small.t])

        def head_unit(h):
            def f(slot):
                v = slot[:, 0:3072].rearrange("p (kc f) -> p kc f", kc=KC)
                return [(v[:, :, j * 128:(j + 1) * 128], dwin_d[0, :, j * D + h * 128: j * D + (h + 1) * 128].rearrange("(kc p) f -> p kc f", p=128))
                        for j in range(3)]
            return ws.add(f)
        u_head = [head_unit(h) for h in range(8)]
        u_wo = [ws.add(lambda slot, hf=hf: [(slot[:, :].rearrange("p (h f) -> p h f", h=8),
                                              dwout_d[0, :, hf * 512:(hf + 1) * 512].rearrange("(h p) f -> p h f", p=128))]) for hf in range(2)]
        ws.ensure(u_head[0])
        ws.ensure(u_wo[0])
        ws.ensure(u_wo[1])
        Wo = []
        for hf in range(2):
            w = ws.get(u_wo[hf])
            wv = w.ap[:, :].rearrange("p (h f) -> p h f", h=8)
            for h in range(8):
                fw.op(DVE, lambda wv=wv, h=h, hf=hf: nc.vector.tensor_tensor(out=wv[:, h, :], in0=wv[:, h, :], in1=BG.ap[:, hf * 512:(hf + 1) * 512], op=ALU.mult),
                      reads=[w.t, BG.t], writes=[w.t])
            Wo.append((w, wv))

        evac_i = [0]

        def evac(dst_ap, dst_t, ps, scale=None, src_ap=None):
            src = ps.ap[:] if src_ap is None else src_ap
            evac_i[0] += 1
            if evac_i[0] % 2 == 0:
                if scale is None:
                    fw.op(ACT, lambda: nc.scalar.copy(out=dst_ap, in_=src), reads=[ps.t], writes=[dst_t])
                else:
                    fw.op(ACT, lambda: nc.scalar.mul(out=dst_ap, in_=src, mul=scale), reads=[ps.t], writes=[dst_t])
            else:
                if scale is None:
                    fw.op(DVE, lambda: nc.vector.tensor_copy(out=dst_ap, in_=src), reads=[ps.t], writes=[dst_t])
                else:
                    fw.op(DVE, lambda: nc.vector.tensor_scalar(out=dst_ap, in0=src, scalar1=scale, scalar2=None, op0=ALU.mult), reads=[ps.t], writes=[dst_t])

        rot = [0]

        def sbank():
            rot[0] += 1
            return PS[4 + rot[0] % 4]

        for h in range(8):
            slope = 2.0 ** (-(h + 1))
            Wref = 128 if h == 0 else (256 if h == 1 else 512)
            if h + 1 < 8:
                ws.ensure(u_head[h + 1])
            W = ws.get(u_head[h])
            Wv = W.ap[:, 0:3072].rearrange("p (kc f) -> p kc f", kc=KC)
            for which, dst, scale in ((0, QT, 0.125), (1, KT, None)):
                for tt in range(4):
                    ps = sbank()
                    fw.op(PE, [lambda kc=kc, ps=ps, which=which, tt=tt: nc.tensor.matmul(ps.ap[:], Wv[:, kc, which * 128:(which + 1) * 128],
                                                                                        hT.ap[:, kc, tt * 512:(tt + 1) * 512], start=(kc == 0), stop=(kc == KC - 1))
                               for kc in range(KC)], reads=[W.t, hT.t], writes=[ps.t])
                    if which == 0:
                        for m in range(2):
                            rows = slice(m * 64, (m + 1) * 64)
                            evac(QT.ap[rows, m, tt * 512:(tt + 1) * 512], QT.t, ps, scale, src_ap=ps.ap[rows, :])
                    else:
                        evac(dst.ap[:, tt * 512:(tt + 1) * 512], dst.t, ps, scale)
            for g in range(4):
                ps = sbank()
                fw.op(PE, [lambda kc=kc, k=k, ps=ps, g=g: nc.tensor.matmul(ps.ap[:, k * 128:(k + 1) * 128], hT.ap[:, kc, (4 * g + k) * 128:(4 * g + k + 1) * 128],
                                                                          Wv[:, kc, 256:384], start=(kc == 0), stop=(kc == KC - 1))
                           for k in range(4) for kc in range(KC)], reads=[W.t, hT.t], writes=[ps.t])
                evac(V.ap[:, 4 * g:4 * g + 4, :], V.t, ps, None, src_ap=ps.ap[:, :].rearrange("p (k e) -> p k e", k=4))
            ws.release(u_head[h])
            if h == 0:
                dump("KT0", KT.ap[:], [128, S], [KT.t]); dump("V0", V.ap[:], [128, NCH, 128], [V.t])
            O = [PS[0], PS[1]]
            Dn = [PS[2], PS[3]]
            steps = [(qt, kb) for qt in range(4) for kb in range(4 * qt + 4)]

            def stage1(n):
                qt, kb = steps[n]
                r = kb - 4 * qt
                c0 = max(0, r) * 128
                q0 = qt * 512
                for m in range(2):
                    ps = PS[4 + (2 * n + m) % 4]
                    E = Eb[(2 * n + m) % 4]
                    lhs = KT.ap[:, kb * 128:(kb + 1) * 128]
                    fns = []
                    if r < 0:
                        fns.append(lambda ps=ps, lhs=lhs, m=m, q0=q0: nc.tensor.matmul(ps.ap[:, 0:512], lhs, QT.ap[:, m, q0:q0 + 512], start=True, stop=True))
                    else:
                        fns.append(lambda ps=ps, lhs=lhs, m=m, q0=q0, c0=c0: nc.tensor.matmul(ps.ap[:, c0:512], lhs, QT.ap[:, m, q0 + c0:q0 + 512], start=True, stop=False))
                        fns.append(lambda ps=ps, c0=c0: nc.tensor.matmul(ps.ap[:, c0:c0 + 128], ident_bf, MB[:, h * 128:(h + 1) * 128], start=False, stop=True))
                    fw.op(PE, fns, reads=[KT.t, QT.t, cbf.t], writes=[ps.t])
                    fns = []
                    a = (c0 // Wref) * Wref
                    while a < 512:
                        b = a + Wref
                        lo = max(a, c0)
                        rr = (q0 + b - kb * 128) // 128 - 1
                        fns.append(lambda ps=ps, E=E, lo=lo, b=b, rr=rr: nc.scalar.activation(out=E.ap[:, lo:b], in_=ps.ap[:, lo:b], func=AF.Exp,
                                                                                         bias=AB[:, h * 16 + rr:h * 16 + rr + 1], scale=1.0))
                        a = b
                    fw.op(ACT, fns, reads=[ps.t, c32.t], writes=[E.t])

            def stage2(n):
                qt, kb = steps[n]
                r = kb - 4 * qt
                c0 = max(0, r) * 128
                lastk = (kb == 4 * qt + 3)
                for m in range(2):
                    E = Eb[(2 * n + m) % 4]
                    Vk = V.ap[:, kb, :]
                    fns = []
                    for acc, lt in ((O[m], Vk), (Dn[m], ones_bf)):
                        if kb == 0:
                            fns.append(lambda acc=acc: nc.tensor.matmul(acc.ap[:, 0:512], ones_bf, zer.ap[:, 0:512], start=True, stop=False))
                        fns.append(lambda acc=acc, lt=lt, E=E, c0=c0, lastk=lastk: nc.tensor.matmul(acc.ap[:, c0:512], lt, E.ap[:, c0:512], start=False, stop=lastk))
                    fw.op(PE, fns, reads=[V.t, E.t, cbf.t, zer.t], writes=[O[m].t, Dn[m].t])

            stage1(0)
            for n in range(len(steps)):
                if n + 1 < len(steps):
                    stage1(n + 1)
                stage2(n)
                qt, kb = steps[n]
                if kb == 4 * qt + 3:
                    qs = slice(qt * 512, (qt + 1) * 512)
                    fw.op(DVE, lambda: nc.vector.reciprocal(out=tA.ap[:], in_=Dn[0].ap[:]), reads=[Dn[0].t], writes=[tA.t])
                    fw.op(DVE, lambda: nc.vector.tensor_tensor(out=tA.ap[:], in0=O[0].ap[:], in1=tA.ap[:], op=ALU.mult), reads=[O[0].t, tA.t], writes=[tA.t])
                    fw.op(DVE, lambda: nc.vector.reciprocal(out=tB.ap[:], in_=Dn[1].ap[:]), reads=[Dn[1].t], writes=[tB.t])
                    fw.op(DVE, lambda: nc.vector.tensor_tensor(out=tB.ap[:], in0=O[1].ap[:], in1=tB.ap[:], op=ALU.mult), reads=[O[1].t, tB.t], writes=[tB.t])
                    fw.op(DVE, lambda qs=qs: nc.vector.scalar_tensor_tensor(out=ou.ap[:, qs], in0=tB.ap[:], scalar=neglam, in1=tA.ap[:], op0=ALU.mult, op1=ALU.add),
                          reads=[tA.t, tB.t, small.t], writes=[ou.t])
            if h == 0:
                dump("ou0", ou.ap[:], [128, S], [ou.t])
            for tt in range(4):
                ps = sbank()
                ts = slice(tt * 512, (tt + 1) * 512)
                oq = osq[tt % 2]
                fw.op(DVE, lambda ts=ts, oq=oq: nc.vector.tensor_tensor(out=oq.ap[:], in0=ou.ap[:, ts], in1=ou.ap[:, ts], op=ALU.mult), reads=[ou.t], writes=[oq.t])
                fw.op(PE, lambda ps=ps, oq=oq: nc.tensor.matmul(ps.ap[:], ones_bf, oq.ap[:], start=True, stop=True), reads=[oq.t, cbf.t], writes=[ps.t])
                tS = tA if tt % 2 == 0 else tB
                fw.op(ACT, lambda ps=ps, tS=tS: nc.scalar.activation(out=tS.ap[:], in_=ps.ap[:], func=AF.Sqrt, bias=epsb, scale=1.0 / 128),
                      reads=[ps.t, small.t], writes=[tS.t])
                fw.op(DVE, lambda tS=tS: nc.vector.reciprocal(out=tS.ap[:], in_=tS.ap[:]), reads=[tS.t], writes=[tS.t])
                fw.op(DVE, lambda ts=ts, tS=tS: nc.vector.scalar_tensor_tensor(out=OT.ap[:, ts], in0=ou.ap[:, ts], scalar=gsub, in1=tS.ap[:], op0=ALU.mult, op1=ALU.mult),
                      reads=[ou.t, tS.t, small.t], writes=[OT.t])
            if h == 0:
                dump("OT0", OT.ap[:], [128, S], [OT.t])
            for c in range(NCH):
                for hf in range(2):
                    ps = sbank()
                    w, wv = Wo[hf]
                    fw.op(PE, lambda ps=ps, c=c, wv=wv, h=h: nc.tensor.matmul(ps.ap[:], OT.ap[:, c * 128:(c + 1) * 128], wv[:, h, :], start=True, stop=True),
                          reads=[OT.t, w.t], writes=[ps.t])
                    xs = X[:, c, hf * 512:(hf + 1) * 512]
                    fw.op(DVE, lambda ps=ps, xs=xs: nc.vector.tensor_tensor(out=xs, in0=ps.ap[:], in1=xs, op=ALU.add), reads=[ps.t, TX[c]], writes=[TX[c]])
        ws.release(u_wo[0])
        ws.release(u_wo[1])

    def moe_layer(i):
        with ExitStack() as st:
            hT = mk(st, "hTm", [128, KC, S], BF16)
            scr = [mk(st, "mscr%d" % k, [128, D]) for k in range(2)]
            ss = mk(st, "mss", [128, NCH])
            rstd = mk(st, "mrstd", [128, NCH])
            hlo = mk(st, "hlo", [128, KC, 128], BF16)
            rwhi = mk(st, "rwhi", [128, KC, 36], BF16)
            rwlo = mk(st, "rwlo", [128, KC, 36], BF16)
            rw = mk(st, "rw", [128, KC, 36])
            rb = mk(st, "rb", [128, 36])
            lg = mk(st, "lg", [128, 36])
            rt = mk(st, "rt", [128, 64])
            em = mk(st, "em", [128, 32])
            oh = mk(st, "oh", [128, 32])
            gw = mk(st, "gw", [128, NCH, NE])
            hid = [mk(st, "hid%d" % k, [128, 4, 512], BF16) for k in range(2)]
            sg = [mk(st, "sg%d" % k, [128, 512], BF16) for k in range(2)]
            u_sh = add_mod_units(i, 3)
            u_sc = add_mod_units(i, 4)
            u_g = add_mod_units(i, 5)
            fw.dma(SP, [(rw.ap[:], rw_d[i].rearrange("(kc p) n -> p kc n", p=128))], writes=[rw.t])
            fw.dma(SP, [(rb.ap[:], rb_d[i:i + 1, :].broadcast_to([128, 36]))], writes=[rb.t])
            fw.op(DVE, lambda: nc.vector.tensor_copy(out=rwhi.ap[:], in_=rw.ap[:]), reads=[rw.t], writes=[rwhi.t])
            fw.op(DVE, lambda: nc.vector.tensor_tensor(out=rwlo.ap[:], in0=rw.ap[:], in1=rwhi.ap[:], op=ALU.subtract), reads=[rw.t, rwhi.t], writes=[rwlo.t])
            gen_mod(i, 3, BS, u_sh)
            gen_mod(i, 4, BA, u_sc, norm_g=n2_d[i:i + 1, :], tmp=scr[0])
            gen_mod(i, 5, BG, u_g)

            def expert_units(e):
                ug = ws.add(lambda slot, e=e: [(slot[:, :].rearrange("p (kc f) -> p kc f", kc=KC), wg_d[i, e].rearrange("(kc p) f -> p kc f", p=128))])
                uu = ws.add(lambda slot, e=e: [(slot[:, :].rearrange("p (kc f) -> p kc f", kc=KC), wu_d[i, e].rearrange("(kc p) f -> p kc f", p=128))])
                ud = ws.add(lambda slot, e=e: [(slot[:, :].rearrange("p (fc d) -> p fc d", fc=4), wd_d[i, e].rearrange("(fc p) d -> p fc d", p=128))])
                return (ug, uu, ud)
            eu = [expert_units(e) for e in range(NE)]
            for u in eu[0]:
                ws.ensure(u)

            r_ = rt.ap

            def router_cb(c, h, pss):
                cs = slice(c * 128, (c + 1) * 128)
                for b in range(2):
                    fw.op(DVE, lambda b=b: nc.vector.tensor_tensor(out=hlo.ap[:, 4 * b:4 * b + 4, :], in0=pss[b].ap[:, :].rearrange("p (k t) -> p k t", k=4),
                                                                  in1=hT.ap[:, 4 * b:4 * b + 4, cs], op=ALU.subtract),
                          reads=[pss[b].t, hT.t], writes=[hlo.t])
                ps = PS[0]
                fns = []
                for kc in range(KC):
                    fns.append(lambda kc=kc: nc.tensor.matmul(ps.ap[:, 0:36], hT.ap[:, kc, cs], rwhi.ap[:, kc, :], start=(kc == 0), stop=False))
                    fns.append(lambda kc=kc: nc.tensor.matmul(ps.ap[:, 0:36], hlo.ap[:, kc, :], rwhi.ap[:, kc, :], start=False, stop=False))
                    fns.append(lambda kc=kc: nc.tensor.matmul(ps.ap[:, 0:36], hT.ap[:, kc, cs], rwlo.ap[:, kc, :], start=False, stop=(kc == KC - 1)))
                fw.op(PE, fns, reads=[hT.t, hlo.t, rwhi.t, rwlo.t], writes=[ps.t])
                V_ = nc.vector
                import os
                if os.environ.get('ROUTER_MM_ONLY'):
                    fw.op(DVE, lambda: V_.tensor_tensor(out=lg.ap[:], in0=ps.ap[:, 0:36], in1=rb.ap[:], op=ALU.add), reads=[ps.t, rb.t], writes=[lg.t])
                    return
                ops = []
                ops.append((lambda: V_.tensor_tensor(out=lg.ap[:], in0=ps.ap[:, 0:36], in1=rb.ap[:], op=ALU.add), [ps.t, rb.t], [lg.t]))
                ops.append((lambda: V_.reduce_max(out=r_[:, 0:1], in_=lg.ap[:, 0:4], axis=AX.X), [lg.t], [rt.t]))
                ops.append((lambda: V_.tensor_scalar(out=r_[:, 1:2], in0=r_[:, 0:1], scalar1=-1.0, scalar2=None, op0=ALU.mult), [rt.t], [rt.t]))
                ops.append((lambda: V_.tensor_scalar(out=r_[:, 4:8], in0=lg.ap[:, 0:4], scalar1=r_[:, 0:1], scalar2=None, op0=ALU.is_equal), [lg.t, rt.t], [rt.t]))
                for o_ in ops:
                    fw.op(DVE, o_[0], reads=o_[1], writes=o_[2])
                fw.op(ACT, lambda: nc.scalar.activation(out=r_[:, 8:12], in_=lg.ap[:, 0:4], func=AF.Exp, bias=r_[:, 1:2], scale=1.0), reads=[lg.t, rt.t], writes=[rt.t])
                ops = []
                ops.append((lambda: V_.reduce_sum(out=r_[:, 2:3], in_=r_[:, 8:12], axis=AX.X), [rt.t], [rt.t]))
                ops.append((lambda: V_.reciprocal(out=r_[:, 3:4], in_=r_[:, 2:3]), [rt.t], [rt.t]))
                ops.append((lambda: V_.tensor_scalar(out=r_[:, 12:16], in0=r_[:, 4:8], scalar1=-1.0, scalar2=1.0e9, op0=ALU.add, op1=ALU.mult), [rt.t], [rt.t]))
                for g in range(4):
                    ops.append((lambda g=g: V_.tensor_scalar(out=em.ap[:, 8 * g:8 * g + 8], in0=lg.ap[:, 4 + 8 * g:12 + 8 * g], scalar1=r_[:, 12 + g:13 + g], scalar2=None, op0=ALU.add),
                                [lg.t, rt.t], [em.t]))
                ops.append((lambda: V_.max(out=r_[:, 16:24], in_=em.ap[:]), [em.t], [rt.t]))
                ops.append((lambda: V_.tensor_tensor(out=r_[:, 24:25], in0=r_[:, 17:18], in1=r_[:, 16:17], op=ALU.subtract), [rt.t], [rt.t]))
                for o_ in ops:
                    fw.op(DVE, o_[0], reads=o_[1], writes=o_[2])
                fw.op(ACT, lambda: nc.scalar.activation(out=r_[:, 25:26], in_=r_[:, 24:25], func=AF.Exp), reads=[rt.t], writes=[rt.t])
                ops = []
                ops.append((lambda: V_.tensor_scalar(out=r_[:, 26:27], in0=r_[:, 25:26], scalar1=1.0, scalar2=None, op0=ALU.add), [rt.t], [rt.t]))
                ops.append((lambda: V_.reciprocal(out=r_[:, 27:28], in_=r_[:, 26:27]), [rt.t], [rt.t]))
                ops.append((lambda: V_.tensor_tensor(out=r_[:, 28:29], in0=r_[:, 27:28], in1=r_[:, 3:4], op=ALU.mult), [rt.t], [rt.t]))
                ops.append((lambda: V_.tensor_tensor(out=r_[:, 29:30], in0=r_[:, 28:29], in1=r_[:, 25:26], op=ALU.mult), [rt.t], [rt.t]))
                ops.append((lambda: V_.tensor_scalar(out=oh.ap[:], in0=em.ap[:], scalar1=r_[:, 16:17], scalar2=r_[:, 28:29], op0=ALU.is_equal, op1=ALU.mult), [em.t, rt.t], [oh.t]))
                ops.append((lambda: V_.tensor_scalar(out=em.ap[:], in0=em.ap[:], scalar1=r_[:, 17:18], scalar2=r_[:, 29:30], op0=ALU.is_equal, op1=ALU.mult), [em.t, rt.t], [em.t]))
                ops.append((lambda c=c: V_.tensor_tensor(out=gw.ap[:, c, :], in0=oh.ap[:], in1=em.ap[:], op=ALU.add), [oh.t, em.t], [gw.t]))
                for o_ in ops:
                    fw.op(DVE, o_[0], reads=o_[1], writes=o_[2])

            import os
            norm_modulate(BA, BS, hT, scr, ss, rstd, hT32_cb=(None if os.environ.get('NOROUTER') else router_cb))
            dump("gw%d" % i, gw.ap[:], [128, NCH, NE], [gw.t])
            dump("hTm%d" % i, hT.ap[:], [128, KC, S], [hT.t])

            n_exp = NE if upto not in ("moe_few", "moe_router") else (2 if upto == "moe_few" else 0)
            rot = [0, 0]
            for e in range(n_exp):
                if e + 1 < n_exp:
                    for u in eu[e + 1]:
                        ws.ensure(u)
                Wg, Wu, Wd = [ws.get(u) for u in eu[e]]
                wgv = Wg.ap[:, :].rearrange("p (kc f) -> p kc f", kc=KC)
                wuv = Wu.ap[:, :].rearrange("p (kc f) -> p kc f", kc=KC)
                wdv = Wd.ap[:, :].rearrange("p (fc d) -> p fc d", fc=4)
                for fc in range(4):
                    fw.op(DVE, lambda fc=fc: nc.vector.tensor_tensor(out=wdv[:, fc, :], in0=wdv[:, fc, :], in1=BG.ap[:], op=ALU.mult), reads=[Wd.t, BG.t], writes=[Wd.t])
                for tt in range(4):
                    ts = slice(tt * 512, (tt + 1) * 512)
                    hd = hid[tt % 2]
                    for f in range(4):
                        rot[0] += 1
                        pg = PS[(2 * rot[0]) % 4]
                        pu = PS[(2 * rot[0]) % 4 + 1]
                        fw.op(PE, [lambda kc=kc, pg=pg, f=f: nc.tensor.matmul(pg.ap[:], wgv[:, kc, f * 128:(f + 1) * 128], hT.ap[:, kc, ts], start=(kc == 0), stop=(kc == KC - 1))
                                   for kc in range(KC)], reads=[Wg.t, hT.t], writes=[pg.t])
                        fw.op(PE, [lambda kc=kc, pu=pu, f=f: nc.tensor.matmul(pu.ap[:], wuv[:, kc, f * 128:(f + 1) * 128], hT.ap[:, kc, ts], start=(kc == 0), stop=(kc == KC - 1))
                                   for kc in range(KC)], reads=[Wu.t, hT.t], writes=[pu.t])
                        s_ = sg[rot[0] % 2]
                        fw.op(ACT, lambda pg=pg, s_=s_: nc.scalar.activation(out=s_.ap[:], in_=pg.ap[:], func=AF.Silu), reads=[pg.t], writes=[s_.t])
                        fw.op(DVE, lambda pu=pu, s_=s_, f=f, hd=hd: nc.vector.tensor_tensor(out=hd.ap[:, f, :], in0=pu.ap[:], in1=s_.ap[:], op=ALU.mult),
                              reads=[pu.t, s_.t], writes=[hd.t])
                    for cc in range(4):
                        c = 4 * tt + cc
                        for hf in range(2):
                            rot[1] += 1
                            ps = PS[4 + rot[1] % 4]
                            fw.op(PE, [lambda f=f, ps=ps, cc=cc, hf=hf, hd=hd: nc.tensor.matmul(ps.ap[:], hd.ap[:, f, cc * 128:(cc + 1) * 128], wdv[:, f, hf * 512:(hf + 1) * 512],
                                                                                         start=(f == 0), stop=(f == 3)) for f in range(4)],
                                  reads=[hd.t, Wd.t], writes=[ps.t])
                            xs = X[:, c, hf * 512:(hf + 1) * 512]
                            fw.op(DVE, lambda ps=ps, xs=xs, c=c, e=e: nc.vector.scalar_tensor_tensor(out=xs, in0=ps.ap[:], scalar=gw.ap[:, c, e:e + 1], in1=xs, op0=ALU.mult, op1=ALU.add),
                                  reads=[ps.t, gw.t, TX[c]], writes=[TX[c]])
                for u in eu[e]:
                    ws.release(u)
            fw.barrier()

    def moe_layer_sparse(i):
        V_ = nc.vector
        sbm_bf = cbf.ap[:, CBF_SBM:CBF_SBM + 128]
        iota_p = c32.ap[:, C32_IOTA:C32_IOTA + 1]
        with ExitStack() as st:
            dest = [mk(st, "dest%d" % k, [128, NCH], I32) for k in range(2)]
            w12 = mk(st, "w12", [128, NCH, 2])
            idxb = mk(st, "idxb", [128, 2, 64], I32)
            u_sh = add_mod_units(i, 3)
            u_sc = add_mod_units(i, 4)
            u_g = add_mod_units(i, 5)
            with ExitStack() as st1:
                hb = mk(st1, "hb", [128, NCH, D], BF16)
                hTc = [mk(st1, "hTc%d" % k, [128, KC, 128], BF16) for k in range(2)]
                scr = [mk(st1, "sscr%d" % k, [128, D]) for k in range(2)]
                ss = mk(st1, "sss", [128, NCH])
                rstd = mk(st1, "srstd", [128, NCH])
                hlo = mk(st1, "shlo", [128, KC, 128], BF16)
                rwhi = mk(st1, "srwhi", [128, KC, 36], BF16)
                rwlo = mk(st1, "srwlo", [128, KC, 36], BF16)
                rw = mk(st1, "srw", [128, KC, 36])
                rb = mk(st1, "srb", [128, 36])
                lg = mk(st1, "slg", [128, 36])
                rt = mk(st1, "srt", [128, 64])
                em = mk(st1, "sem", [128, 32])
                m12 = [mk(st1, "m12_%d" % k, [128, NCH, NE]) for k in range(2)]
                ohb = mk(st1, "ohb", [128, NCH, NE], BF16)
                rank = mk(st1, "rank", [128, NCH, NE])
                cmpb = mk(st1, "cmpb", [128, 64, NE])
                cnt = mk(st1, "cnt", [128, 6, NE])
                ebf = mk(st1, "ebf", [128, 64])
                destf = mk(st1, "destf", [128, NCH])
                fw.dma(SP, [(rw.ap[:], rw_d[i].rearrange("(kc p) n -> p kc n", p=128))], writes=[rw.t])
                fw.dma(SP, [(rb.ap[:], rb_d[i:i + 1, :].broadcast_to([128, 36]))], writes=[rb.t])
                fw.op(DVE, lambda: V_.tensor_copy(out=rwhi.ap[:], in_=rw.ap[:]), reads=[rw.t], writes=[rwhi.t])
                fw.op(DVE, lambda: V_.tensor_tensor(out=rwlo.ap[:], in0=rw.ap[:], in1=rwhi.ap[:], op=ALU.subtract), reads=[rw.t, rwhi.t], writes=[rwlo.t])
                gen_mod(i, 3, BS, u_sh)
                gen_mod(i, 4, BA, u_sc, norm_g=n2_d[i:i + 1, :], tmp=scr[0])
                gen_mod(i, 5, BG, u_g)
                r_ = rt.ap

                lga = mk(st1, "lga", [128, NCH, 36])
                ema = mk(st1, "ema", [128, NCH, NE])
                g4 = [mk(st1, "g4_%d" % k, [128, NCH, 4]) for k in range(3)]
                r16 = [mk(st1, "r16_%d" % k, [128, NCH]) for k in range(8)]

                def router_cb(c, h, pss, hk):
                    for b in range(2):
                        fw.op(DVE, lambda b=b: V_.tensor_tensor(out=hlo.ap[:, 4 * b:4 * b + 4, :], in0=pss[b].ap[:, :].rearrange("p (k t) -> p k t", k=4),
                                                               in1=hk.ap[:, 4 * b:4 * b + 4, :], op=ALU.subtract), reads=[pss[b].t, hk.t], writes=[hlo.t])
                    fw.op(ACT, lambda: nc.scalar.copy(out=hb.ap[:, c, :], in_=h.ap[:]), reads=[h.t], writes=[hb.t])
                    ps = PS[c % 2]
                    fns = []
                    for kc in range(KC):
                        fns.append(lambda kc=kc: nc.tensor.matmul(ps.ap[:, 0:36], hk.ap[:, kc, :], rwhi.ap[:, kc, :], start=(kc == 0), stop=False))
                        fns.append(lambda kc=kc: nc.tensor.matmul(ps.ap[:, 0:36], hlo.ap[:, kc, :], rwhi.ap[:, kc, :], start=False, stop=False))
                        fns.append(lambda kc=kc: nc.tensor.matmul(ps.ap[:, 0:36], hk.ap[:, kc, :], rwlo.ap[:, kc, :], start=False, stop=(kc == KC - 1)))
                    fw.op(PE, fns, reads=[hk.t, hlo.t, rwhi.t, rwlo.t], writes=[ps.t])
                    fw.op(DVE, lambda: V_.tensor_tensor(out=lga.ap[:, c, :], in0=ps.ap[:, 0:36], in1=rb.ap[:], op=ALU.add), reads=[ps.t, rb.t], writes=[lga.t])

                norm_modulate(BA, BS, None, scr, ss, rstd, hT32_cb=router_cb, hT_chunks=hTc)

                def bc(ap2, n):
                    return ap2.unsqueeze(2).to_broadcast([128, NCH, n])
                gl = lga.ap[:, :, 0:4]
                gmax, gsum, pg, top1, top2, dd, ed, tmp = [r.ap[:] for r in r16]
                T16 = [r.t for r in r16]
                G4 = [g.t for g in g4]

                def dv(fn, reads, writes):
                    fw.op(DVE, fn, reads=reads, writes=writes)
                dv(lambda: V_.tensor_reduce(out=gmax, in_=gl, axis=AX.X, op=ALU.max), [lga.t], [T16[0]])
                dv(lambda: V_.tensor_tensor(out=g4[0].ap[:], in0=gl, in1=bc(gmax, 4), op=ALU.is_equal), [lga.t, T16[0]], [G4[0]])
                dv(lambda: V_.tensor_tensor(out=g4[1].ap[:], in0=gl, in1=bc(gmax, 4), op=ALU.subtract), [lga.t, T16[0]], [G4[1]])
                fw.op(ACT, lambda: nc.scalar.activation(out=g4[1].ap[:], in_=g4[1].ap[:], func=AF.Exp), reads=[G4[1]], writes=[G4[1]])
                dv(lambda: V_.tensor_reduce(out=gsum, in_=g4[1].ap[:], axis=AX.X, op=ALU.add), [G4[1]], [T16[1]])
                dv(lambda: V_.reciprocal(out=pg, in_=gsum), [T16[1]], [T16[2]])
                dv(lambda: V_.tensor_scalar(out=g4[2].ap[:], in0=g4[0].ap[:], scalar1=-1.0, scalar2=1.0e9, op0=ALU.add, op1=ALU.mult), [G4[0]], [G4[2]])
                em4 = ema.ap[:, :, :].rearrange("p c (g e) -> p c g e", g=4)
                ea4 = lga.ap[:, :, 4:36].rearrange("p c (g e) -> p c g e", g=4)
                dv(lambda: V_.tensor_tensor(out=em4, in0=ea4, in1=g4[2].ap[:].unsqueeze(3).to_broadcast([128, NCH, 4, 8]), op=ALU.add), [lga.t, G4[2]], [ema.t])
                dv(lambda: V_.tensor_reduce(out=top1, in_=ema.ap[:], axis=AX.X, op=ALU.max), [ema.t], [T16[3]])
                dv(lambda: V_.tensor_tensor(out=m12[0].ap[:], in0=ema.ap[:], in1=bc(top1, NE), op=ALU.is_equal), [ema.t, T16[3]], [m12[0].t])
                dv(lambda: V_.scalar_tensor_tensor(out=ema.ap[:], in0=m12[0].ap[:], scalar=-1.0e9, in1=ema.ap[:], op0=ALU.mult, op1=ALU.add), [m12[0].t, ema.t], [ema.t])
                dv(lambda: V_.tensor_reduce(out=top2, in_=ema.ap[:], axis=AX.X, op=ALU.max), [ema.t], [T16[4]])
                dv(lambda: V_.tensor_tensor(out=m12[1].ap[:], in0=ema.ap[:], in1=bc(top2, NE), op=ALU.is_equal), [ema.t, T16[4]], [m12[1].t])
                dv(lambda: V_.tensor_tensor(out=dd, in0=top2, in1=top1, op=ALU.subtract), [T16[3], T16[4]], [T16[5]])
                fw.op(ACT, lambda: nc.scalar.activation(out=ed, in_=dd, func=AF.Exp), reads=[T16[5]], writes=[T16[6]])
                dv(lambda: V_.tensor_scalar(out=tmp, in0=ed, scalar1=1.0, scalar2=None, op0=ALU.add), [T16[6]], [T16[7]])
                dv(lambda: V_.reciprocal(out=tmp, in_=tmp), [T16[7]], [T16[7]])
                dv(lambda: V_.tensor_tensor(out=w12.ap[:, :, 0], in0=tmp, in1=pg, op=ALU.mult), [T16[7], T16[2]], [w12.t])
                dv(lambda: V_.tensor_tensor(out=w12.ap[:, :, 1], in0=w12.ap[:, :, 0], in1=ed, op=ALU.mult), [w12.t, T16[6]], [w12.t])
                dv(lambda: V_.tensor_tensor(out=ohb.ap[:], in0=m12[0].ap[:], in1=m12[1].ap[:], op=ALU.add), [m12[0].t, m12[1].t], [ohb.t])
                pr = PS[2]
                fns = []
                for c in range(NCH):
                    for c2 in range(c):
                        fns.append(lambda c=c, c2=c2: nc.tensor.matmul(pr.ap[:, c * 32:(c + 1) * 32], ones_bf, ohb.ap[:, c2, :], start=(c2 == 0), stop=False))
                    fns.append(lambda c=c: nc.tensor.matmul(pr.ap[:, c * 32:(c + 1) * 32], sbm_bf, ohb.ap[:, c, :], start=(c == 0), stop=True))
                fw.op(PE, fns, reads=[ohb.t, cbf.t], writes=[pr.t])
                dv(lambda: V_.tensor_copy(out=rank.ap[:], in_=pr.ap[:, :].rearrange("p (c e) -> p c e", e=NE)), [pr.t], [rank.t])
                pc = PS[1]
                fw.op(PE, [lambda c2=c2: nc.tensor.matmul(pc.ap[:, 0:32], ones_bf, ohb.ap[:, c2, :], start=(c2 == 0), stop=(c2 == NCH - 1)) for c2 in range(NCH)],
                      reads=[ohb.t, cbf.t], writes=[pc.t])
                cn = cnt.ap
                fw.op(DVE, lambda: V_.tensor_copy(out=cn[:, 0, :], in_=pc.ap[:, 0:32]), reads=[pc.t], writes=[cnt.t])
                fw.op(DVE, lambda: V_.memset(cn[:, 1, :], 0.0), writes=[cnt.t])
                for j in range(16):
                    fw.op(DVE, lambda j=j: V_.scalar_tensor_tensor(out=cn[:, 1, :], in0=cn[:, 0, :], scalar=float(128 * j), in1=cn[:, 1, :], op0=ALU.is_gt, op1=ALU.add),
                          reads=[cnt.t], writes=[cnt.t])
                fw.op(DVE, lambda: V_.tensor_tensor_scan(out=cn[:, 2, :], data0=cn[:, 1, :], data1=cn[:, 1, :], initial=0.0, op0=ALU.add, op1=ALU.bypass),
                      reads=[cnt.t], writes=[cnt.t])
                fw.op(DVE, lambda: V_.tensor_tensor(out=cn[:, 3, :], in0=cn[:, 2, :], in1=cn[:, 1, :], op=ALU.subtract), reads=[cnt.t], writes=[cnt.t])
                fw.op(DVE, lambda: V_.tensor_scalar(out=cn[:, 3, :], in0=cn[:, 3, :], scalar1=128.0, scalar2=None, op0=ALU.mult), reads=[cnt.t], writes=[cnt.t])
                dump("cnt%d" % i, cn[:, 0:4, :], [128, 4, NE], [cnt.t])
                for c in range(NCH):
                    fw.op(DVE, lambda c=c: V_.tensor_tensor(out=rank.ap[:, c, :], in0=rank.ap[:, c, :], in1=cn[:, 3, :], op=ALU.add), reads=[rank.t, cnt.t], writes=[rank.t])
                for k in range(2):
                    fw.op(DVE, lambda k=k: V_.tensor_tensor(out=m12[k].ap[:], in0=m12[k].ap[:], in1=rank.ap[:], op=ALU.mult), reads=[m12[k].t, rank.t], writes=[m12[k].t])
                    fw.op(DVE, lambda k=k: V_.reduce_sum(out=destf.ap[:], in_=m12[k].ap[:], axis=AX.X), reads=[m12[k].t], writes=[destf.t])
                    fw.op(DVE, lambda k=k: V_.tensor_copy(out=dest[k].ap[:], in_=destf.ap[:]), reads=[destf.t], writes=[dest[k].t])
                    dump("dest%d_%d" % (k, i), destf.ap[:], [128, NCH], [destf.t])
                for b in range(64):
                    fw.op(DVE, lambda b=b: V_.tensor_scalar(out=cmpb.ap[:, b, :], in0=cn[:, 2, :], scalar1=float(b), scalar2=None, op0=ALU.is_le), reads=[cnt.t], writes=[cmpb.t])
                fw.op(DVE, lambda: V_.reduce_sum(out=ebf.ap[:], in_=cmpb.ap[:], axis=AX.X), reads=[cmpb.t], writes=[ebf.t])
                fw.op(DVE, lambda: V_.tensor_scalar(out=destf.ap[:, 0:1], in0=ebf.ap[:, 0:1], scalar1=0.0, scalar2=None, op0=ALU.mult), reads=[ebf.t], writes=[destf.t])
                ebig = mk(st1, "ebig", [128, 64])
                fw.op(DVE, lambda: V_.tensor_scalar(out=ebig.ap[:], in0=ebf.ap[:], scalar1=float(NE), scalar2=1.0e9, op0=ALU.is_ge, op1=ALU.mult), reads=[ebf.t], writes=[ebig.t])
                esame = mk(st1, "esame", [128, 64])
                fw.op(DVE, lambda: V_.memset(esame.ap[:], 0.0), writes=[esame.t])
                fw.op(DVE, lambda: V_.tensor_tensor(out=esame.ap[:, 1:64], in0=ebf.ap[:, 1:64], in1=ebf.ap[:, 0:63], op=ALU.is_equal), reads=[ebf.t], writes=[esame.t])
                fw.op(DVE, lambda: V_.memset(esame.ap[:, 32:33], 0.0), writes=[esame.t])
                fw.op(DVE, lambda: V_.scalar_tensor_tensor(out=ebig.ap[:], in0=esame.ap[:], scalar=1.0e9, in1=ebig.ap[:], op0=ALU.mult, op1=ALU.add),
                      reads=[esame.t, ebig.t], writes=[ebig.t])
                fw.op(DVE, lambda: V_.tensor_scalar(out=ebf.ap[:], in0=ebf.ap[:], scalar1=float(NE - 1), scalar2=128.0, op0=ALU.min, op1=ALU.mult), reads=[ebf.t], writes=[ebf.t])
                fw.op(DVE, lambda: V_.tensor_scalar(out=ebf.ap[:], in0=ebf.ap[:], scalar1=iota_p, scalar2=None, op0=ALU.add), reads=[ebf.t, c32.t], writes=[ebf.t])
                fw.op(DVE, lambda: V_.tensor_scalar(out=ebf.ap[:], in0=ebf.ap[:], scalar1=2.0, scalar2=float(i * NE * 128 * 2), op0=ALU.mult, op1=ALU.add), reads=[ebf.t], writes=[ebf.t])
                fw.op(DVE, lambda: V_.tensor_tensor(out=ebf.ap[:], in0=ebf.ap[:], in1=ebig.ap[:], op=ALU.add), reads=[ebf.t, ebig.t], writes=[ebf.t])
                fw.op(DVE, lambda: V_.tensor_copy(out=idxb.ap[:, 0, :], in_=ebf.ap[:]), reads=[ebf.t], writes=[idxb.t])
                fw.op(DVE, lambda: V_.tensor_scalar(out=ebf.ap[:], in0=ebf.ap[:], scalar1=1.0, scalar2=None, op0=ALU.add), reads=[ebf.t], writes=[ebf.t])
                fw.op(DVE, lambda: V_.tensor_copy(out=idxb.ap[:, 1, :], in_=ebf.ap[:]), reads=[ebf.t], writes=[idxb.t])
                dump("ebf%d" % i, ebf.ap[:], [128, 64], [ebf.t])
                for c in range(NCH):
                    for k in range(2):
                        fw.dma_ind(xs_d[:, :], hb.ap[:, c, :], dest[k].ap[:, c:c + 1], True, reads=[hb.t, dest[k].t], writes=[xs_t], waw=False)
            fw.barrier()
            with ExitStack() as st2:
                xsb = [mk(st2, "xsb%d" % k, [128, D], BF16) for k in range(3)]
                xsT = [mk(st2, "xsT%d" % k, [128, KC, 128], BF16) for k in range(2)]
                sgb = [mk(st2, "sgb%d" % k, [128, 512], BF16) for k in range(2)]
                hdb = [mk(st2, "hdb%d" % k, [128, 512], BF16) for k in range(2)]
                hdT = [mk(st2, "hdT%d" % k, [128, 4, 128], BF16) for k in range(2)]
                yst = [mk(st2, "yst%d" % k, [128, D]) for k in range(2)]
                tabs = [wg_d.rearrange("l e (p a k) f -> (l e p a) (k f)", a=2, k=4), wu_d.rearrange("l e (p a k) f -> (l e p a) (k f)", a=2, k=4),
                        wd_d.rearrange("l e (p a k) d -> (l e p a) (k d)", a=2, k=2)]

                def blk_units(b):
                    us = []
                    for tab in tabs:
                        def f(slot_ap, tab=tab, b=b):
                            def emit(slot):
                                fw.dma_ind([slot.ap[:, hf * 2048:(hf + 1) * 2048] for hf in range(2)], [tab[:, :]] * 2,
                                           [idxb.ap[:, hf, b:b + 1] for hf in range(2)], False, reads=[idxb.t], writes=[slot.t], bounds=2 * NE * 128 * 2 - 1)
                            return emit
                        us.append(ws.add(f))
                    return us
                NB = 64 if upto != "moe_few" else 3
                bu = [blk_units(b) for b in range(NB)]
                for u in bu[0]:
                    ws.ensure(u)
                pT = PS[0].ap[:, :].bitcast(BF16)
                pH = PS[5].ap[:, :].bitcast(BF16)

                order = [k // 2 + 32 * (k % 2) for k in range(64)] if NB == 64 else list(range(NB))
                def stA(i):
                    b = order[i]
                    for u in bu[b]:
                        ws.ensure(u)
                    x_ = xsb[i % 3]
                    if i == 0:
                        fw.dma(SP, [(x_.ap[:], xs_d[b * 128:(b + 1) * 128, :])], reads=[xs_t], writes=[x_.t])
                    if i + 1 < NB:
                        xn = xsb[(i + 1) % 3]
                        bn = order[i + 1]
                        fw.dma(SP, [(xn.ap[:], xs_d[bn * 128:(bn + 1) * 128, :])], reads=[xs_t], writes=[xn.t])
                    xv = x_.ap[:, :].rearrange("s (p k) -> s k p", k=8)
                    fw.op(PE, [lambda kc=kc: nc.tensor.transpose(pT[:, kc * 128:(kc + 1) * 128], xv[:, kc, :], ident_bf) for kc in range(KC)],
                          reads=[x_.t, cbf.t], writes=[PS[0].t])
                    xT = xsT[i % 2]
                    fw.op(ACT, lambda: nc.scalar.copy(out=xT.ap[:, 0:4, :], in_=pT[:, 0:512].rearrange("p (k t) -> p k t", k=4)), reads=[PS[0].t], writes=[xT.t])
                    fw.op(DVE, lambda: V_.tensor_copy(out=xT.ap[:, 4:8, :], in_=pT[:, 512:1024].rearrange("p (k t) -> p k t", k=4)), reads=[PS[0].t], writes=[xT.t])
                    Wg, Wu, Wd = [ws.get(u) for u in bu[b]]
                    wgv = Wg.ap[:, :].rearrange("p (k f) -> p k f", k=8)
                    wuv = Wu.ap[:, :].rearrange("p (k f) -> p k f", k=8)
                    pg = PS[1 + i % 2]
                    pu = PS[3 + i % 2]
                    fw.op(PE, [lambda kc=kc: nc.tensor.matmul(pg.ap[:], xT.ap[:, kc, :], wgv[:, kc, :], start=(kc == 0), stop=(kc == KC - 1)) for kc in range(KC)],
                          reads=[xT.t, Wg.t], writes=[pg.t])
                    fw.op(PE, [lambda kc=kc: nc.tensor.matmul(pu.ap[:], xT.ap[:, kc, :], wuv[:, kc, :], start=(kc == 0), stop=(kc == KC - 1)) for kc in range(KC)],
                          reads=[xT.t, Wu.t], writes=[pu.t])

                def stB(i):
                    b = order[i]
                    Wg, Wu, Wd = [ws.get(u) for u in bu[b]]
                    wdv = Wd.ap[:, :].rearrange("p (k d) -> p k d", k=4)
                    pg = PS[1 + i % 2]
                    pu = PS[3 + i % 2]
                    sg_ = sgb[i % 2]
                    hd = hdb[i % 2]
                    hT_ = hdT[i % 2]
                    ys_ = yst[i % 2]
                    fw.op(ACT, lambda: nc.scalar.activation(out=sg_.ap[:], in_=pg.ap[:], func=AF.Silu), reads=[pg.t], writes=[sg_.t])
                    fw.op(DVE, lambda: V_.tensor_tensor(out=hd.ap[:], in0=pu.ap[:], in1=sg_.ap[:], op=ALU.mult), reads=[pu.t, sg_.t], writes=[hd.t])
                    hv = hd.ap[:, :].rearrange("s (p k) -> s k p", k=4)
                    fw.op(PE, [lambda fc=fc: nc.tensor.transpose(pH[:, fc * 128:(fc + 1) * 128], hv[:, fc, :], ident_bf) for fc in range(4)],
                          reads=[hd.t, cbf.t], writes=[PS[5].t])
                    fw.op(DVE, lambda: V_.tensor_copy(out=hT_.ap[:], in_=pH[:, 0:512].rearrange("p (k t) -> p k t", k=4)), reads=[PS[5].t], writes=[hT_.t])
                    for hf in range(2):
                        py = PS[6 + hf]
                        fw.op(PE, [lambda fc=fc, py=py, hf=hf: nc.tensor.matmul(py.ap[:], hT_.ap[:, fc, :], wdv[:, fc, hf * 512:(hf + 1) * 512], start=(fc == 0), stop=(fc == 3))
                                   for fc in range(4)], reads=[hT_.t, Wd.t], writes=[py.t])
                        fw.op(DVE, lambda py=py, hf=hf: V_.tensor_tensor(out=ys_.ap[:, hf * 512:(hf + 1) * 512], in0=py.ap[:], in1=BG.ap[:, hf * 512:(hf + 1) * 512], op=ALU.mult),
                              reads=[py.t, BG.t], writes=[ys_.t])
                    fw.dma(SP, [(ys_d[b * 128:(b + 1) * 128, :], ys_.ap[:])], reads=[ys_.t], writes=[ys_t], waw=False)
                    for u in bu[b]:
                        ws.release(u)

                stA(0)
                for i in range(NB):
                    if i + 1 < NB:
                        stA(i + 1)
                    stB(i)
            fw.barrier()
            with ExitStack() as st3:
                y0 = [mk(st3, "y0_%d" % k, [128, D]) for k in range(2)]
                y1 = [mk(st3, "y1_%d" % k, [128, D]) for k in range(2)]
                for c in range(NCH):
                    a0 = y0[c % 2]
                    a1 = y1[c % 2]
                    fw.dma_ind(a0.ap[:], ys_d[:, :], dest[0].ap[:, c:c + 1], False, reads=[ys_t, dest[0].t], writes=[a0.t])
                    fw.dma_ind(a1.ap[:], ys_d[:, :], dest[1].ap[:, c:c + 1], False, reads=[ys_t, dest[1].t], writes=[a1.t])
                    fw.op(DVE, lambda: V_.scalar_tensor_tensor(out=X[:, c, :], in0=a0.ap[:], scalar=w12.ap[:, c, 0:1], in1=X[:, c, :], op0=ALU.mult, op1=ALU.add),
                          reads=[a0.t, w12.t, TX[c]], writes=[TX[c]])
                    fw.op(DVE, lambda: V_.scalar_tensor_tensor(out=X[:, c, :], in0=a1.ap[:], scalar=w12.ap[:, c, 1:2], in1=X[:, c, :], op0=ALU.mult, op1=ALU.add),
                          reads=[a1.t, w12.t, TX[c]], writes=[TX[c]])
            fw.barrier()

    def sb_attention_heads(st, hT):
        QT = mk(st, "sQT", [128, 2, S], BF16)
        fw.op(DVE, lambda: nc.vector.memset(QT.ap[:], 0.0), writes=[QT.t])
        KT = mk(st, "sKT", [128, S], BF16)
        V = mk(st, "sV", [128, NCH, 128], BF16)
        OT = mk(st, "sOT", [128, S], BF16)
        e1 = [mk(st, "e1_%d" % k, [128, 512]) for k in range(1)]
        spb = [mk(st, "sp_%d" % k, [128, 512], BF16) for k in range(6)]
        tt_ = [mk(st, "t_%d" % k, [128, 512]) for k in range(3)]
        Ab = [mk(st, "A_%d" % k, [128, 512], BF16) for k in range(4)]
        sbm_bf = cbf.ap[:, CBF_SBM:CBF_SBM + 128]
        tle = mk(st, "tle", [128, 128], BF16)
        zer = mk(st, "zer", [128, 512], BF16)
        fw.op(DVE, lambda: nc.vector.memset(zer.ap[:], 0.0), writes=[zer.t])
        fw.op(DVE, lambda: nc.vector.tensor_tensor(out=tle.ap[:], in0=ones_bf, in1=tgt_bf, op=ALU.subtract), reads=[cbf.t], writes=[tle.t])

        def pair_unit(p):
            def f(slot):
                v = slot[:, 0:3072].rearrange("p (kc f) -> p kc f", kc=KC)
                return [(v[:, :, j * 128:(j + 1) * 128], swin_d[0, :, j * D + p * 128: j * D + (p + 1) * 128].rearrange("(kc p) f -> p kc f", p=128))
                        for j in range(3)]
            return ws.add(f)
        u_pair = [pair_unit(p) for p in range(8)]
        u_wo = [ws.add(lambda slot, hf=hf: [(slot[:, :].rearrange("p (h f) -> p h f", h=8),
                                              swout_d[0, :, hf * 512:(hf + 1) * 512].rearrange("(h p) f -> p h f", p=128))]) for hf in range(2)]
        ws.ensure(u_pair[0])
        ws.ensure(u_wo[0])
        ws.ensure(u_wo[1])
        Wo = []
        for hf in range(2):
            w = ws.get(u_wo[hf])
            wv = w.ap[:, :].rearrange("p (h f) -> p h f", h=8)
            for h in range(8):
                fw.op(DVE, lambda wv=wv, h=h, hf=hf: nc.vector.tensor_tensor(out=wv[:, h, :], in0=wv[:, h, :], in1=BG.ap[:, hf * 512:(hf + 1) * 512], op=ALU.mult),
                      reads=[w.t, BG.t], writes=[w.t])
            Wo.append((w, wv))
        ev = [0]

        def evac(dst_ap, dst_t, ps, scale=None, src_ap=None):
            src = ps.ap[:] if src_ap is None else src_ap
            ev[0] += 1
            if ev[0] % 2 == 0:
                if scale is None:
                    fw.op(ACT, lambda: nc.scalar.copy(out=dst_ap, in_=src), reads=[ps.t], writes=[dst_t])
                else:
                    fw.op(ACT, lambda: nc.scalar.mul(out=dst_ap, in_=src, mul=scale), reads=[ps.t], writes=[dst_t])
            else:
                if scale is None:
                    fw.op(DVE, lambda: nc.vector.tensor_copy(out=dst_ap, in_=src), reads=[ps.t], writes=[dst_t])
                else:
                    fw.op(DVE, lambda: nc.vector.tensor_scalar(out=dst_ap, in0=src, scalar1=scale, scalar2=None, op0=ALU.mult), reads=[ps.t], writes=[dst_t])
        rot = [0]

        def zbank():
            rot[0] += 1
            return PS[4 + rot[0] % 4]

        n_pairs = 8 if upto != "attn1_few" else 1
        for p in range(n_pairs):
            if p + 1 < n_pairs:
                ws.ensure(u_pair[p + 1])
            W = ws.get(u_pair[p])
            Wv = W.ap[:, 0:3072].rearrange("p (kc f) -> p kc f", kc=KC)
            for which, dst, scale in ((0, QT, 0.125), (1, KT, None)):
                for tq in range(4):
                    ps = zbank()
                    fw.op(PE, [lambda kc=kc, ps=ps, which=which, tq=tq: nc.tensor.matmul(ps.ap[:], Wv[:, kc, which * 128:(which + 1) * 128],
                                                                                        hT.ap[:, kc, tq * 512:(tq + 1) * 512], start=(kc == 0), stop=(kc == KC - 1))
                               for kc in range(KC)], reads=[W.t, hT.t], writes=[ps.t])
                    if which == 0:
                        for m in range(2):
                            rws = slice(m * 64, (m + 1) * 64)
                            evac(QT.ap[rws, m, tq * 512:(tq + 1) * 512], QT.t, ps, scale, src_ap=ps.ap[rws, :])
                    else:
                        evac(dst.ap[:, tq * 512:(tq + 1) * 512], dst.t, ps, scale)
            for g in range(4):
                ps = zbank()
                fw.op(PE, [lambda kc=kc, k=k, ps=ps, g=g: nc.tensor.matmul(ps.ap[:, k * 128:(k + 1) * 128], hT.ap[:, kc, (4 * g + k) * 128:(4 * g + k + 1) * 128],
                                                                          Wv[:, kc, 256:384], start=(kc == 0), stop=(kc == KC - 1))
                           for k in range(4) for kc in range(KC)], reads=[W.t, hT.t], writes=[ps.t])
                evac(V.ap[:, 4 * g:4 * g + 4, :], V.t, ps, None, src_ap=ps.ap[:, :].rearrange("p (k e) -> p k e", k=4))
            ws.release(u_pair[p])
            Oacc = [PS[0], PS[1]]
            Bacc = [PS[2], PS[3]]
            items = [(qt, sblk, hh) for qt in range(4) for sblk in range(4 * qt + 3, -1, -1) for hh in range(2)]
            LOOK = 2

            def bufs(n):
                return PS[4 + n % 4], e1[0], spb[n % 6], tt_[n % 3], Ab[n % 4]

            def geo(n):
                qt, sblk, hh = items[n]
                r = sblk - 4 * qt
                c0 = max(0, r) * 128
                return qt, sblk, hh, qt * 512, r, c0, slice(c0, 512)

            def s1_pa(n):
                qt, sblk, hh, q0, r, c0, cs = geo(n)
                pz, E1, SP_, T_, A_ = bufs(n)
                rows = slice(hh * 64, (hh + 1) * 64)
                fw.op(PE, lambda: nc.tensor.matmul(pz.ap[:, c0:512], KT.ap[:, sblk * 128:(sblk + 1) * 128], QT.ap[:, hh, q0 + c0:q0 + 512], start=True, stop=True),
                      reads=[KT.t, QT.t], writes=[pz.t])
                fw.op(ACT, lambda: nc.scalar.activation(out=E1.ap[:, cs], in_=pz.ap[:, cs], func=AF.Exp), reads=[pz.t], writes=[E1.t])
                fw.op(ACT, lambda: nc.scalar.activation(out=SP_.ap[:, cs], in_=E1.ap[:, cs], func=AF.Ln, bias=1.0, scale=1.0), reads=[E1.t], writes=[SP_.t])

            def s1_d(n):
                qt, sblk, hh, q0, r, c0, cs = geo(n)
                pz, E1, SP_, T_, A_ = bufs(n)
                if r >= 0:
                    fw.op(DVE, lambda: nc.vector.tensor_tensor(out=SP_.ap[:, c0:c0 + 128], in0=SP_.ap[:, c0:c0 + 128], in1=sbm_bf, op=ALU.mult),
                          reads=[SP_.t, cbf.t], writes=[SP_.t])
                fw.op(DVE, lambda: nc.vector.tensor_tensor(out=T_.ap[:, cs], in0=pz.ap[:, cs], in1=SP_.ap[:, cs], op=ALU.subtract), reads=[pz.t, SP_.t], writes=[T_.t])

            def s2_p(n):
                qt, sblk, hh, q0, r, c0, cs = geo(n)
                pz, E1, SP_, T_, A_ = bufs(n)
                B = Bacc[hh]
                Oa = Oacc[hh]
                if sblk == 4 * qt + 3:
                    for acc in (B, Oa):
                        fw.op(PE, lambda acc=acc: nc.tensor.matmul(acc.ap[:, 0:512], tgt_bf, zer.ap[:, 0:512], start=True, stop=False), reads=[zer.t, cbf.t], writes=[acc.t])
                fw.op(PE, lambda: nc.tensor.matmul(B.ap[:, cs], tgt_bf, SP_.ap[:, cs], start=False, stop=(sblk == 0)), reads=[SP_.t, cbf.t], writes=[B.t])

            def s2_d(n):
                qt, sblk, hh, q0, r, c0, cs = geo(n)
                pz, E1, SP_, T_, A_ = bufs(n)
                B = Bacc[hh]
                fw.op(DVE, lambda: nc.vector.tensor_tensor(out=T_.ap[:, cs], in0=T_.ap[:, cs], in1=B.ap[:, cs], op=ALU.subtract), reads=[T_.t, B.t], writes=[T_.t])

            def s2_a(n):
                qt, sblk, hh, q0, r, c0, cs = geo(n)
                pz, E1, SP_, T_, A_ = bufs(n)
                fw.op(ACT, lambda: nc.scalar.activation(out=A_.ap[:, cs], in_=T_.ap[:, cs], func=AF.Exp), reads=[T_.t], writes=[A_.t])
                if r >= 0:
                    fw.op(DVE, lambda: nc.vector.tensor_tensor(out=A_.ap[:, c0:c0 + 128], in0=A_.ap[:, c0:c0 + 128], in1=sbm_bf, op=ALU.mult),
                          reads=[A_.t, cbf.t], writes=[A_.t])

            def s2b(n):
                qt, sblk, hh, q0, r, c0, cs = geo(n)
                pz, E1, SP_, T_, A_ = bufs(n)
                B = Bacc[hh]
                Oa = Oacc[hh]
                last = (sblk == 0)
                if not last:
                    fw.op(PE, lambda: nc.tensor.matmul(B.ap[:, cs], tle.ap[:], SP_.ap[:, cs], start=False, stop=False), reads=[SP_.t, tle.t], writes=[B.t])
                fw.op(PE, lambda: nc.tensor.matmul(Oa.ap[:, cs], V.ap[:, sblk, :], A_.ap[:, cs], start=False, stop=last), reads=[V.t, A_.t], writes=[Oa.t])
                if last:
                    rows = slice(hh * 64, (hh + 1) * 64)
                    evac(OT.ap[rows, q0:q0 + 512], OT.t, Oa, None, src_ap=Oa.ap[rows, :])

            NI = len(items)
            for n in range(min(LOOK, NI)):
                s1_pa(n)
                s1_d(n)
            for t in range(NI + 1):
                if t + LOOK < NI:
                    s1_pa(t + LOOK)
                if t < NI:
                    s2_p(t)
                    s2_d(t)
                if t + LOOK < NI:
                    s1_d(t + LOOK)
                if t < NI:
                    s2_a(t)
                if t >= 1:
                    s2b(t - 1)
            if p == 0:
                dump("sOT0", OT.ap[:], [128, S], [OT.t])
            for c in range(NCH):
                for hf in range(2):
                    ps = zbank()
                    w, wv = Wo[hf]
                    fw.op(PE, lambda ps=ps, c=c, wv=wv, p=p: nc.tensor.matmul(ps.ap[:], OT.ap[:, c * 128:(c + 1) * 128], wv[:, p, :], start=True, stop=True),
                          reads=[OT.t, w.t], writes=[ps.t])
                    xs = X[:, c, hf * 512:(hf + 1) * 512]
                    fw.op(DVE, lambda ps=ps, xs=xs: nc.vector.tensor_tensor(out=xs, in0=ps.ap[:], in1=xs, op=ALU.add), reads=[ps.t, TX[c]], writes=[TX[c]])
        ws.release(u_wo[0])
        ws.release(u_wo[1])

    def final_norm():
        with ExitStack() as st:
            scr = [mk(st, "fscr%d" % k, [128, D]) for k in range(2)]
            ss = mk(st, "fss", [128, NCH])
            rstd = mk(st, "frstd", [128, NCH])
            fw.dma(SP, [(BA.ap[:], fng_d[0:1, :].broadcast_to([128, D]))], writes=[BA.t])
            for c in range(NCH):
                fw.op(ACT, lambda c=c: nc.scalar.activation(out=scr[0].ap[:], in_=X[:, c, :], func=AF.Square, accum_out=ss.ap[:, c:c + 1]),
                      reads=[TX[c]], writes=[scr[0].t, ss.t])
            fw.op(DVE, lambda: nc.vector.tensor_scalar(out=rstd.ap[:], in0=ss.ap[:], scalar1=1.0 / D, scalar2=RMS_EPS, op0=ALU.mult, op1=ALU.add),
                  reads=[ss.t], writes=[rstd.t])
            fw.op(POOL, lambda: nc.gpsimd.tensor_tensor(out=rstd.ap[:], in0=rstd.ap[:], in1=neghalf.ap[:, 0:NCH], op=ALU.pow),
                  reads=[rstd.t, neghalf.t], writes=[rstd.t])
            for c in range(NCH):
                o = scr[c % 2]
                fw.op(DVE, lambda c=c, o=o: nc.vector.scalar_tensor_tensor(out=o.ap[:], in0=X[:, c, :], scalar=rstd.ap[:, c:c + 1], in1=BA.ap[:], op0=ALU.mult, op1=ALU.mult),
                      reads=[TX[c], rstd.t, BA.t], writes=[o.t])
                fw.dma(SP, [(out_d[c * 128:(c + 1) * 128, :], o.ap[:])], reads=[o.t])
            fw.barrier()

    stages = ["attn0", "moe0", "attn1", "moe1", "all"]
    lvl = {"norm1": 0, "attn": 0, "attn0": 0, "moe_router": 1, "moe_few": 1, "moe0": 1, "attn1_few": 2, "attn1": 2, "moe1": 3, "all": 4}[upto]
    with ExitStack() as st:
        with ExitStack() as st2:
            hT, lambda_init = attn_norm(0, st, st2)
        fw.barrier()
        if upto != "norm1":
            diff_attention_heads(st, hT, lambda_init)
        fw.barrier()
    moe_fn = moe_layer if SPARSE_MOE is False else moe_layer_sparse
    if lvl >= 1:
        moe_fn(0)
    if lvl >= 2:
        with ExitStack() as st:
            with ExitStack() as st2:
                hT, _ = attn_norm(1, st, st2)
            fw.barrier()
            sb_attention_heads(st, hT)
            fw.barrier()
    if lvl >= 3:
        moe_fn(1)
    if lvl >= 4:
        final_norm()
    else:
        for g in range(4):
            dst = out_d[g * 512:(g + 1) * 512, :].rearrange("(c p) d -> p c d", p=128)
            fw.dma(SP, [(dst, X[:, 4 * g:4 * g + 4, :])], reads=TX[4 * g:4 * g + 4])
        fw.barrier()
    return nc


_CACHE = {}


def _prep_inputs(inputs):
    f = lambda a: np.ascontiguousarray(np.asarray(a, dtype=np.float32))
    cbf, c32 = _host_consts()
    rw = np.concatenate([np.asarray(inputs["router_group_w"]), np.asarray(inputs["router_expert_w"])], axis=-1)
    rb = np.concatenate([np.asarray(inputs["router_group_b"]), np.asarray(inputs["router_expert_b"]).reshape(2, 32)], axis=-1)
    shared = {
        "norm1_g": f(inputs["norm1_g"]), "norm2_g": f(inputs["norm2_g"]),
        "ada_w": f(inputs["ada_w"]), "ada_b": f(inputs["ada_b"]),
        "diff_w_in": f(inputs["diff_w_in"]), "diff_w_out": f(inputs["diff_w_out"]),
        "diff_lambda_q1": f(inputs["diff_lambda_q1"]), "diff_lambda_k1": f(inputs["diff_lambda_k1"]),
        "diff_lambda_q2": f(inputs["diff_lambda_q2"]), "diff_lambda_k2": f(inputs["diff_lambda_k2"]),
        "subgT": f(np.asarray(inputs["diff_subln_g"]).reshape(128, 1)),
        "sb_w_in": f(inputs["sb_w_in"]), "sb_w_out": f(inputs["sb_w_out"]),
        "router_w": f(rw), "router_b": f(rb),
        "expert_w_gate": f(inputs["expert_w_gate"]), "expert_w_up": f(inputs["expert_w_up"]),
        "expert_w_down": f(inputs["expert_w_down"]),
        "final_norm_g": f(np.asarray(inputs["final_norm_g"]).reshape(1, D)),
        "cbf": cbf, "c32": c32,
    }
    x = np.asarray(inputs["x"], dtype=np.float32)
    c = np.asarray(inputs["c"], dtype=np.float32)
    maps = []
    for b in range(x.shape[0]):
        m = dict(shared)
        m["x"] = np.ascontiguousarray(x[b])
        m["cT"] = np.ascontiguousarray(c[b].reshape(KC, 128).T)
        maps.append(m)
    return maps


def kernel(**inputs):
    maps = _prep_inputs(inputs)
    if "nc" not in _CACHE:
        _CACHE["nc"] = build_program()
    nc = _CACHE["nc"]
    res = run_bass_kernel_spmd(nc, maps, core_ids=list(range(len(maps))))
    out = np.stack([np.asarray(r["out"], dtype=np.float32) for r in res.results], axis=0)
    return out
```

```python
import math
from contextlib import ExitStack
import numpy as np
import concourse.bass as bass
import concourse.mybir as mybir
from concourse.bass_utils import run_bass_kernel_spmd
from concourse.alu_op_type import AluOpType as ALU

F32 = mybir.dt.float32
BF16 = mybir.dt.bfloat16
I32 = mybir.dt.int32
AF = mybir.ActivationFunctionType
AX = mybir.AxisListType

D = 1024
S = 2048
NCH = 16
KC = 8
NE = 32
FH = 512
RMS_EPS = 1e-6
SUBLN_EPS = 1e-5
NEG = -30000.0


class Trk:
    __slots__ = ("w", "r", "dsem", "dval", "name")

    def __init__(self, name=""):
        self.w = None
        self.r = {}
        self.dsem = None
        self.dval = 0
        self.name = name


class Eng:
    def __init__(self, nc, name, eng, selfsync):
        self.name = name
        self.eng = eng
        self.selfsync = selfsync
        self.sem = nc.alloc_semaphore(name="es_" + name)
        self.count = 0
        self.waited = {}


class FW:
    def __init__(self, nc):
        self.nc = nc
        self.pe = Eng(nc, "pe", nc.tensor, False)
        self.act = Eng(nc, "act", nc.scalar, True)
        self.dve = Eng(nc, "dve", nc.vector, True)
        self.pool = Eng(nc, "pool", nc.gpsimd, True)
        self.sp = Eng(nc, "sp", nc.sync, False)
        self.engs = [self.pe, self.act, self.dve, self.pool, self.sp]
        self.dsems = []
        self._bregs = {}
        self.ninst = 0

    def _wait(self, e, deps):
        best = {}
        for d in deps:
            if d is None:
                continue
            sem, val = d
            k = id(sem)
            if k not in best or best[k][1] < val:
                best[k] = (sem, val)
        for k, (sem, val) in best.items():
            if sem is e.sem and not e.selfsync:
                continue
            if e.waited.get(k, 0) >= val:
                continue
            e.eng.wait_ge(sem, val)
            e.waited[k] = val

    @staticmethod
    def _deps(reads, writes, waw=True):
        deps = []
        for t in reads:
            deps.append(t.w)
        for t in writes:
            if waw:
                deps.append(t.w)
            deps.extend(t.r.values())
        return deps

    def op(self, e, fns, reads=(), writes=()):
        self._wait(e, self._deps(reads, writes))
        if callable(fns):
            fns = [fns]
        inst = None
        for f in fns:
            inst = f()
            self.ninst += 1
        e.count += 1
        inst.then_inc(e.sem, 1)
        tag = (e.sem, e.count)
        for t in writes:
            t.w = tag
            t.r = {}
        for t in reads:
            t.r[e.name] = tag
        return tag

    def dma(self, e, pairs, reads=(), writes=(), waw=True, **kw):
        self._wait(e, self._deps(reads, writes, waw))
        owner = writes[0] if writes else reads[0]
        if owner.dsem is None:
            owner.dsem = self.nc.alloc_semaphore(name="ds%d" % len(self.dsems))
            self.dsems.append(owner)
        for (o, i) in pairs:
            e.eng.dma_start(out=o, in_=i, **kw).then_inc(owner.dsem, 16)
            owner.dval += 16
            self.ninst += 1
        tag = (owner.dsem, owner.dval)
        for t in writes:
            t.w = tag
            if waw:
                t.r = {}
        for t in reads:
            t.r["dma%d" % id(owner)] = tag
        return tag

    def dma_ind(self, out_ap, in_ap, idx_ap, scatter, reads=(), writes=(), waw=True, bounds=None):
        e = self.pool
        self._wait(e, self._deps(reads, writes, waw))
        owner = writes[0]
        if owner.dsem is None:
            owner.dsem = self.nc.alloc_semaphore(name="ds%d" % len(self.dsems))
            self.dsems.append(owner)
        if not isinstance(out_ap, list):
            out_ap, in_ap, idx_ap = [out_ap], [in_ap], [idx_ap]
        for o_, i_, x_ in zip(out_ap, in_ap, idx_ap):
            off = bass.IndirectOffsetOnAxis(ap=x_, axis=0)
            if scatter:
                self.nc.gpsimd.indirect_dma_start(out=o_, out_offset=off, in_=i_, in_offset=None).then_inc(owner.dsem, 16)
            else:
                kw = {}
                if bounds is not None:
                    if bounds not in self._bregs:
                        self._bregs[bounds] = self.nc.gpsimd.to_reg(bounds)
                    kw = dict(bounds_check=self._bregs[bounds], oob_is_err=False)
                self.nc.gpsimd.indirect_dma_start(out=o_, out_offset=None, in_=i_, in_offset=off, **kw).then_inc(owner.dsem, 16)
            owner.dval += 16
            self.ninst += 1
        tag = (owner.dsem, owner.dval)
        for t in writes:
            t.w = tag
            if waw:
                t.r = {}
        for t in reads:
            t.r["dma%d" % id(owner)] = tag
        return tag

    def barrier(self):
        deps = [(e.sem, e.count) for e in self.engs if e.count > 0]
        deps += [(t.dsem, t.dval) for t in self.dsems if t.dval > 0]
        for e in self.engs:
            self._wait(e, deps)


class Buf:
    def __init__(self, ap, name=""):
        self.ap = ap
        self.t = Trk(name)


def _host_consts():
    p = np.arange(128, dtype=np.float32)[:, None]
    j = np.arange(128, dtype=np.float32)[None, :]
    ident = (p == j).astype(np.float32)
    ones = np.ones((128, 128), np.float32)
    t_gt = (p > j).astype(np.float32)
    mb = np.zeros((128, 8, 128), np.float32)
    for h in range(8):
        slope = 2.0 ** (-(h + 1))
        allowed = (p // 64) <= (j // 64)
        corr = -2.0 * slope * np.maximum(p - j, 0.0)
        mb[:, h, :] = np.where(allowed, corr, NEG)
    sbm = (p < j).astype(np.float32)
    cbf = np.concatenate([ident, ones, t_gt, mb.reshape(128, 1024), sbm], axis=1)
    ab = np.zeros((128, 8, 16), np.float32)
    for h in range(8):
        slope = 2.0 ** (-(h + 1))
        for r in range(16):
            ab[:, h, r] = slope * (p[:, 0] + 1.0 - 128.0 * (r + 1))
    c32 = np.concatenate([ident, ones, ab.reshape(128, 128), sbm, np.repeat(p, 8, axis=1)], axis=1)
    return np.ascontiguousarray(cbf), np.ascontiguousarray(c32)


CBF_IDENT, CBF_ONES, CBF_TGT, CBF_MB, CBF_SBM = 0, 128, 256, 384, 1408
CBF_W = 1536
C32_IDENT, C32_ONES, C32_AB, C32_SBM = 0, 128, 256, 384
C32_IOTA = 512
C32_W = 520

SPARSE_MOE = True
NSLOT = 6


def build_program(upto="all", debug=None):
    nc = bass.Bass("TRN2", target_bir_lowering=False)
    fw = FW(nc)
    dbg_outs = {}

    def dump(name, ap, shape, trks):
        if debug is None or name not in debug:
            return
        d = nc.dram_tensor("dbg_" + name, list(shape), F32, kind="ExternalOutput").ap()
        fw.dma(fw.pool, [(d, ap)], reads=list(trks))
        dbg_outs[name] = d

    PE, ACT, DVE, POOL, SP = fw.pe, fw.act, fw.dve, fw.pool, fw.sp

    def dram_in(name, shape):
        return nc.dram_tensor(name, list(shape), F32, kind="ExternalInput").ap()

    x_d = dram_in("x", [S, D])
    cT_d = dram_in("cT", [128, KC])
    n1_d = dram_in("norm1_g", [2, D])
    n2_d = dram_in("norm2_g", [2, D])
    adaw_d = dram_in("ada_w", [2, D, 6 * D])
    adab_d = dram_in("ada_b", [2, 6 * D])
    dwin_d = dram_in("diff_w_in", [1, D, 3 * D])
    dwout_d = dram_in("diff_w_out", [1, D, D])
    lq1_d = dram_in("diff_lambda_q1", [1, 64])
    lk1_d = dram_in("diff_lambda_k1", [1, 64])
    lq2_d = dram_in("diff_lambda_q2", [1, 64])
    lk2_d = dram_in("diff_lambda_k2", [1, 64])
    subg_d = dram_in("subgT", [128, 1])
    swin_d = dram_in("sb_w_in", [1, D, 3 * D])
    swout_d = dram_in("sb_w_out", [1, D, D])
    rw_d = dram_in("router_w", [2, D, 36])
    rb_d = dram_in("router_b", [2, 36])
    wg_d = dram_in("expert_w_gate", [2, NE, D, FH])
    wu_d = dram_in("expert_w_up", [2, NE, D, FH])
    wd_d = dram_in("expert_w_down", [2, NE, FH, D])
    fng_d = dram_in("final_norm_g", [1, D])
    cbf_d = dram_in("cbf", [128, CBF_W])
    c32_d = dram_in("c32", [128, C32_W])
    out_d = nc.dram_tensor("out", [S, D], F32, kind="ExternalOutput").ap()
    NSLOTS = 8192
    xs_d = nc.dram_tensor("xs_scratch", [NSLOTS, D], BF16, kind="Internal").ap()
    ys_d = nc.dram_tensor("ys_scratch", [NSLOTS, D], F32, kind="Internal").ap()
    xs_t = Trk("xs_d")
    ys_t = Trk("ys_d")

    def sb(name, shape, dt=F32):
        return Buf(nc.alloc_sbuf_tensor("s_" + name, list(shape), dt).ap(), name)

    X = nc.alloc_sbuf_tensor("s_X", [128, NCH, D], F32).ap()
    TX = [Trk("X%d" % c) for c in range(NCH)]
    cbf = sb("cbf", [128, CBF_W], BF16)
    c32 = sb("c32", [128, C32_W], F32)
    BA = sb("BA", [128, D])
    BS = sb("BS", [128, D])
    BG = sb("BG", [128, D])
    condT = sb("condT", [128, KC])
    condrep = sb("condrep", [128, KC, 128], BF16)
    neghalf = sb("neghalf", [128, NCH])
    small = sb("small", [128, 64])
    WB = [sb("WB%d" % i, [128, 4096], BF16) for i in range(NSLOT)]
    PS = [Buf(nc.alloc_psum_tensor("ps%d" % i, [128, 512], F32).ap(), "ps%d" % i) for i in range(8)]

    ident_bf = cbf.ap[:, CBF_IDENT:CBF_IDENT + 128]
    ones_bf = cbf.ap[:, CBF_ONES:CBF_ONES + 128]
    tgt_bf = cbf.ap[:, CBF_TGT:CBF_TGT + 128]
    ident32 = c32.ap[:, C32_IDENT:C32_IDENT + 128]
    ones32 = c32.ap[:, C32_ONES:C32_ONES + 128]

    class WStream:
        def __init__(self):
            self.units = []
            self.slot_of = {}
            self.free = list(range(NSLOT))

        def add(self, pairs_fn):
            self.units.append(pairs_fn)
            return len(self.units) - 1

        def ensure(self, u):
            if u is None or u in self.slot_of:
                return
            assert self.free, "weight ring exhausted"
            si = self.free.pop(0)
            self.slot_of[u] = si
            slot = WB[si]
            r = self.units[u](slot.ap)
            if callable(r):
                r(slot)
            else:
                fw.dma(POOL, r, writes=[slot.t])

        def get(self, u):
            return WB[self.slot_of[u]]

        def release(self, u):
            self.free.append(self.slot_of[u])

    ws = WStream()

    for g in range(4):
        src = x_d[g * 512:(g + 1) * 512, :].rearrange("(c p) d -> p c d", p=128)
        fw.dma(SP, [(X[:, 4 * g:4 * g + 4, :], src)], writes=TX[4 * g:4 * g + 4])
    fw.dma(POOL, [(cbf.ap[:], cbf_d[:])], writes=[cbf.t])
    fw.dma(SP, [(c32.ap[:], c32_d[:])], writes=[c32.t])
    fw.dma(SP, [(condT.ap[:], cT_d[:])], writes=[condT.t])
    fw.op(DVE, lambda: nc.vector.memset(neghalf.ap[:], -0.5), writes=[neghalf.t])
    fw.op(ACT, lambda: nc.scalar.activation(out=condT.ap[:], in_=condT.ap[:], func=AF.Silu), reads=[condT.t], writes=[condT.t])
    for kc in range(KC):
        fw.op(DVE, lambda kc=kc: nc.vector.tensor_scalar(out=condrep.ap[:, kc, :], in0=ones32, scalar1=condT.ap[:, kc:kc + 1],
                                                         scalar2=None, op0=ALU.mult),
              reads=[condT.t, c32.t], writes=[condrep.t])

    if SPARSE_MOE:
        zt = sb("zt", [128, D], BF16)
        fw.op(DVE, lambda: nc.vector.memset(zt.ap[:], 0.0), writes=[zt.t])
        for b in range(64):
            fw.dma(SP, [(xs_d[b * 128:(b + 1) * 128, :], zt.ap[:])], reads=[zt.t], writes=[xs_t], waw=False)

    def add_mod_units(i, j):
        us = []
        for hf in range(2):
            src = adaw_d[i, :, j * D + hf * 512: j * D + (hf + 1) * 512].rearrange("(kc p) f -> p kc f", p=128)
            us.append(ws.add(lambda slot, src=src: [(slot[:, :].rearrange("p (kc f) -> p kc f", kc=KC), src)]))
        return us

    def gen_mod(i, j, dst, units, norm_g=None, tmp=None):
        fw.dma(SP, [(dst.ap[:], adab_d[i:i + 1, j * D:(j + 1) * D].broadcast_to([128, D]))], writes=[dst.t])
        if norm_g is not None:
            fw.dma(SP, [(tmp.ap[:], norm_g.broadcast_to([128, D]))], writes=[tmp.t])
        for hf in range(2):
            u = units[hf]
            ws.ensure(u)
            w = ws.get(u)
            wv = w.ap[:, :].rearrange("p (kc f) -> p kc f", kc=KC)
            ps = PS[hf]
            fw.op(PE, [lambda kc=kc: nc.tensor.matmul(ps.ap[:], condrep.ap[:, kc, :], wv[:, kc, :], start=(kc == 0), stop=(kc == KC - 1))
                       for kc in range(KC)], reads=[condrep.t, w.t], writes=[ps.t])
            ws.release(u)
            sl = slice(hf * 512, (hf + 1) * 512)
            fw.op(DVE, lambda: nc.vector.tensor_tensor(out=dst.ap[:, sl], in0=ps.ap[:], in1=dst.ap[:, sl], op=ALU.add),
                  reads=[ps.t, dst.t], writes=[dst.t])
        if norm_g is not None:
            fw.op(DVE, lambda: nc.vector.scalar_tensor_tensor(out=dst.ap[:], in0=dst.ap[:], scalar=1.0, in1=tmp.ap[:],
                                                             op0=ALU.add, op1=ALU.mult),
                  reads=[dst.t, tmp.t], writes=[dst.t])

    def norm_modulate(a, sh, hT, scr, ss, rstd, hT32_cb=None, hT_chunks=None):
        for c in range(NCH):
            fw.op(ACT, lambda c=c: nc.scalar.activation(out=scr[0].ap[:], in_=X[:, c, :], func=AF.Square,
                                                        accum_out=ss.ap[:, c:c + 1]),
                  reads=[TX[c]], writes=[scr[0].t, ss.t])
        fw.op(DVE, lambda: nc.vector.tensor_scalar(out=rstd.ap[:], in0=ss.ap[:], scalar1=1.0 / D, scalar2=RMS_EPS, op0=ALU.mult, op1=ALU.add),
              reads=[ss.t], writes=[rstd.t])
        fw.op(POOL, lambda: nc.gpsimd.tensor_tensor(out=rstd.ap[:], in0=rstd.ap[:], in1=neghalf.ap[:, 0:NCH], op=ALU.pow),
              reads=[rstd.t, neghalf.t], writes=[rstd.t])
        for c in range(NCH):
            h = scr[c % 2]
            fw.op(DVE, lambda c=c, h=h: nc.vector.scalar_tensor_tensor(out=h.ap[:], in0=X[:, c, :], scalar=rstd.ap[:, c:c + 1], in1=a.ap[:],
                                                                      op0=ALU.mult, op1=ALU.mult),
                  reads=[TX[c], rstd.t, a.t], writes=[h.t])
            fw.op(DVE, lambda h=h: nc.vector.tensor_tensor(out=h.ap[:], in0=h.ap[:], in1=sh.ap[:], op=ALU.add),
                  reads=[h.t, sh.t], writes=[h.t])
            pss = []
            for b in range(2):
                ps = PS[4 + (2 * c + b) % 4]
                fw.op(PE, [lambda k=k, ps=ps, h=h, b=b: nc.tensor.transpose(ps.ap[:, k * 128:(k + 1) * 128], h.ap[:, (4 * b + k) * 128:(4 * b + k + 1) * 128], ident32)
                           for k in range(4)], reads=[h.t, c32.t], writes=[ps.t])
                if hT is not None:
                    fw.op(ACT, lambda ps=ps, b=b, c=c: nc.scalar.copy(out=hT.ap[:, 4 * b:4 * b + 4, c * 128:(c + 1) * 128],
                                                                      in_=ps.ap[:, :].rearrange("p (k t) -> p k t", k=4)),
                          reads=[ps.t], writes=[hT.t])
                if hT_chunks is not None:
                    hk = hT_chunks[c % 2]
                    fw.op(ACT, lambda ps=ps, b=b, hk=hk: nc.scalar.copy(out=hk.ap[:, 4 * b:4 * b + 4, :], in_=ps.ap[:, :].rearrange("p (k t) -> p k t", k=4)),
                          reads=[ps.t], writes=[hk.t])
                pss.append(ps)
            if hT32_cb is not None:
                if hT_chunks is not None:
                    hT32_cb(c, h, pss, hT_chunks[c % 2])
                else:
                    hT32_cb(c, h, pss)

    def attn_norm(i, st, st2):
        lambda_init = 0.8 - 0.6 * math.exp(-0.3 * i)
        hT = Buf(st.enter_context(nc.sbuf_tensor("s_hT%d" % i, [128, KC, S], BF16)).ap(), "hT")
        scr = [Buf(st2.enter_context(nc.sbuf_tensor("s_scr%d_%d" % (k, i), [128, D], F32)).ap(), "scr%d" % k) for k in range(2)]
        ss = Buf(st2.enter_context(nc.sbuf_tensor("s_ss%d" % i, [128, NCH], F32)).ap(), "ss")
        rstd = Buf(st2.enter_context(nc.sbuf_tensor("s_rstd%d" % i, [128, NCH], F32)).ap(), "rstd")
        u_sh = add_mod_units(i, 0)
        u_sc = add_mod_units(i, 1)
        u_g = add_mod_units(i, 2)
        gen_mod(i, 0, BS, u_sh)
        gen_mod(i, 1, BA, u_sc, norm_g=n1_d[i:i + 1, :], tmp=scr[0])
        gen_mod(i, 2, BG, u_g)
        dump("BS", BS.ap[:], [128, D], [BS.t]); dump("BA", BA.ap[:], [128, D], [BA.t]); dump("BG", BG.ap[:], [128, D], [BG.t])
        norm_modulate(BA, BS, hT, scr, ss, rstd)
        dump("hT", hT.ap[:], [128, KC, S], [hT.t])
        dump("rstd", rstd.ap[:], [128, NCH], [rstd.t])
        return hT, lambda_init

    uniq = [0]

    def mk(st, name, shape, dt=F32):
        uniq[0] += 1
        return Buf(st.enter_context(nc.sbuf_tensor("s_%s_%d" % (name, uniq[0]), list(shape), dt)).ap(), name)

    def diff_attention_heads(st, hT, lambda_init):
        QT = mk(st, "QT", [128, 2, S], BF16)
        fw.op(DVE, lambda: nc.vector.memset(QT.ap[:], 0.0), writes=[QT.t])
        KT = mk(st, "KT", [128, S], BF16)
        V = mk(st, "V", [128, NCH, 128], BF16)
        Eb = [mk(st, "E%d" % k, [128, 512], BF16) for k in range(4)]
        ou = mk(st, "ou", [128, S])
        osq = [mk(st, "osq%d" % k, [128, 512], BF16) for k in range(2)]
        OT = mk(st, "OT", [128, S], BF16)
        tA = mk(st, "tA", [128, 512])
        tB = mk(st, "tB", [128, 512])
        zer = mk(st, "dzer", [128, 512], BF16)
        fw.op(DVE, lambda: nc.vector.memset(zer.ap[:], 0.0), writes=[zer.t])
        class _V:
            def __init__(self, ap, t):
                self.ap = ap
                self.t = t
        lam4 = [_V(tA.ap[:, 0:64], tA.t), _V(tA.ap[:, 64:128], tA.t), _V(tB.ap[:, 0:64], tB.t), _V(tB.ap[:, 64:128], tB.t)]
        MB = cbf.ap[:, CBF_MB:CBF_MB + 1024]
        AB = c32.ap[:, C32_AB:C32_AB + 128]
        for k, src in enumerate([lq1_d, lk1_d, lq2_d, lk2_d]):
            fw.dma(SP, [(lam4[k].ap[:], src[0:1, :].broadcast_to([128, 64]))], writes=[lam4[k].t])
        sm = small.ap
        for k in range(2):
            fw.op(DVE, lambda k=k: nc.vector.tensor_tensor(out=lam4[2 * k].ap[:], in0=lam4[2 * k].ap[:], in1=lam4[2 * k + 1].ap[:], op=ALU.mult),
                  reads=[lam4[2 * k].t, lam4[2 * k + 1].t], writes=[lam4[2 * k].t])
            fw.op(DVE, lambda k=k: nc.vector.reduce_sum(out=sm[:, k:k + 1], in_=lam4[2 * k].ap[:], axis=AX.X),
                  reads=[lam4[2 * k].t], writes=[small.t])
        fw.op(ACT, lambda: nc.scalar.activation(out=sm[:, 2:4], in_=sm[:, 0:2], func=AF.Exp), reads=[small.t], writes=[small.t])
        fw.op(DVE, lambda: nc.vector.tensor_tensor(out=sm[:, 4:5], in0=sm[:, 3:4], in1=sm[:, 2:3], op=ALU.subtract), reads=[small.t], writes=[small.t])
        fw.op(DVE, lambda: nc.vector.tensor_scalar(out=sm[:, 4:5], in0=sm[:, 4:5], scalar1=-lambda_init, scalar2=None, op0=ALU.add), reads=[small.t], writes=[small.t])
        neglam = sm[:, 4:5]
        fw.dma(SP, [(sm[:, 5:6], subg_d[:])], writes=[small.t])
        fw.op(DVE, lambda: nc.vector.tensor_scalar(out=sm[:, 5:6], in0=sm[:, 5:6], scalar1=1.0 - lambda_init, scalar2=None, op0=ALU.mult), reads=[small.t], writes=[small.t])
        gsub = sm[:, 5:6]
        fw.op(DVE, lambda: nc.vector.memset(sm[:, 6:7], SUBLN_EPS), writes=[small.t])
        epsb = sm[:, 6:7]
        dump("small", sm[:, 0:8], [128, 8], [small.t])

        def head_unit(h):
            def f(slot):
                v = slot[:, 0:3072].rearrange("p (kc f) -> p kc f", kc=KC)
                return [(v[:, :, j * 128:(j + 1) * 128], dwin_d[0, :, j * D + h * 128: j * D + (h + 1) * 128].rearrange("(kc p) f -> p kc f", p=128))
                        for j in range(3)]
            return ws.add(f)
        u_head = [head_unit(h) for h in range(8)]
        u_wo = [ws.add(lambda slot, hf=hf: [(slot[:, :].rearrange("p (h f) -> p h f", h=8),
                                              dwout_d[0, :, hf * 512:(hf + 1) * 512].rearrange("(h p) f -> p h f", p=128))]) for hf in range(2)]
        ws.ensure(u_head[0])
        ws.ensure(u_wo[0])
        ws.ensure(u_wo[1])
        Wo = []
        for hf in range(2):
            w = ws.get(u_wo[hf])
            wv = w.ap[:, :].rearrange("p (h f) -> p h f", h=8)
            for h in range(8):
                fw.op(DVE, lambda wv=wv, h=h, hf=hf: nc.vector.tensor_tensor(out=wv[:, h, :], in0=wv[:, h, :], in1=BG.ap[:, hf * 512:(hf + 1) * 512], op=ALU.mult),
                      reads=[w.t, BG.t], writes=[w.t])
            Wo.append((w, wv))

        evac_i = [0]

        def evac(dst_ap, dst_t, ps, scale=None, src_ap=None):
            src = ps.ap[:] if src_ap is None else src_ap
            evac_i[0] += 1
            if evac_i[0] % 2 == 0:
                if scale is None:
                    fw.op(ACT, lambda: nc.scalar.copy(out=dst_ap, in_=src), reads=[ps.t], writes=[dst_t])
                else:
                    fw.op(ACT, lambda: nc.scalar.mul(out=dst_ap, in_=src, mul=scale), reads=[ps.t], writes=[dst_t])
            else:
                if scale is None:
                    fw.op(DVE, lambda: nc.vector.tensor_copy(out=dst_ap, in_=src), reads=[ps.t], writes=[dst_t])
                else:
                    fw.op(DVE, lambda: nc.vector.tensor_scalar(out=dst_ap, in0=src, scalar1=scale, scalar2=None, op0=ALU.mult), reads=[ps.t], writes=[dst_t])

        rot = [0]

        def sbank():
            rot[0] += 1
            return PS[4 + rot[0] % 4]

        for h in range(8):
            slope = 2.0 ** (-(h + 1))
            Wref = 128 if h == 0 else (256 if h == 1 else 512)
            if h + 1 < 8:
                ws.ensure(u_head[h + 1])
            W = ws.get(u_head[h])
            Wv = W.ap[:, 0:3072].rearrange("p (kc f) -> p kc f", kc=KC)
            for which, dst, scale in ((0, QT, 0.125), (1, KT, None)):
                for tt in range(4):
                    ps = sbank()
                    fw.op(PE, [lambda kc=kc, ps=ps, which=which, tt=tt: nc.tensor.matmul(ps.ap[:], Wv[:, kc, which * 128:(which + 1) * 128],
                                                                                        hT.ap[:, kc, tt * 512:(tt + 1) * 512], start=(kc == 0), stop=(kc == KC - 1))
                               for kc in range(KC)], reads=[W.t, hT.t], writes=[ps.t])
                    if which == 0:
                        for m in range(2):
                            rows = slice(m * 64, (m + 1) * 64)
                            evac(QT.ap[rows, m, tt * 512:(tt + 1) * 512], QT.t, ps, scale, src_ap=ps.ap[rows, :])
                    else:
                        evac(dst.ap[:, tt * 512:(tt + 1) * 512], dst.t, ps, scale)
            for g in range(4):
                ps = sbank()
                fw.op(PE, [lambda kc=kc, k=k, ps=ps, g=g: nc.tensor.matmul(ps.ap[:, k * 128:(k + 1) * 128], hT.ap[:, kc, (4 * g + k) * 128:(4 * g + k + 1) * 128],
                                                                          Wv[:, kc, 256:384], start=(kc == 0), stop=(kc == KC - 1))
                           for k in range(4) for kc in range(KC)], reads=[W.t, hT.t], writes=[ps.t])
                evac(V.ap[:, 4 * g:4 * g + 4, :], V.t, ps, None, src_ap=ps.ap[:, :].rearrange("p (k e) -> p k e", k=4))
            ws.release(u_head[h])
            if h == 0:
                dump("KT0", KT.ap[:], [128, S], [KT.t]); dump("V0", V.ap[:], [128, NCH, 128], [V.t])
            O = [PS[0], PS[1]]
            Dn = [PS[2], PS[3]]
            steps = [(qt, kb) for qt in range(4) for kb in range(4 * qt + 4)]

            def stage1(n):
                qt, kb = steps[n]
                r = kb - 4 * qt
                c0 = max(0, r) * 128
                q0 = qt * 512
                for m in range(2):
                    ps = PS[4 + (2 * n + m) % 4]
                    E = Eb[(2 * n + m) % 4]
                    lhs = KT.ap[:, kb * 128:(kb + 1) * 128]
                    fns = []
                    if r < 0:
                        fns.append(lambda ps=ps, lhs=lhs, m=m, q0=q0: nc.tensor.matmul(ps.ap[:, 0:512], lhs, QT.ap[:, m, q0:q0 + 512], start=True, stop=True))
                    else:
                        fns.append(lambda ps=ps, lhs=lhs, m=m, q0=q0, c0=c0: nc.tensor.matmul(ps.ap[:, c0:512], lhs, QT.ap[:, m, q0 + c0:q0 + 512], start=True, stop=False))
                        fns.append(lambda ps=ps, c0=c0: nc.tensor.matmul(ps.ap[:, c0:c0 + 128], ident_bf, MB[:, h * 128:(h + 1) * 128], start=False, stop=True))
                    fw.op(PE, fns, reads=[KT.t, QT.t, cbf.t], writes=[ps.t])
                    fns = []
                    a = (c0 // Wref) * Wref
                    while a < 512:
                        b = a + Wref
                        lo = max(a, c0)
                        rr = (q0 + b - kb * 128) // 128 - 1
                        fns.append(lambda ps=ps, E=E, lo=lo, b=b, rr=rr: nc.scalar.activation(out=E.ap[:, lo:b], in_=ps.ap[:, lo:b], func=AF.Exp,
                                                                                         bias=AB[:, h * 16 + rr:h * 16 + rr + 1], scale=1.0))
                        a = b
                    fw.op(ACT, fns, reads=[ps.t, c32.t], writes=[E.t])

            def stage2(n):
                qt, kb = steps[n]
                r = kb - 4 * qt
                c0 = max(0, r) * 128
                lastk = (kb == 4 * qt + 3)
                for m in range(2):
                    E = Eb[(2 * n + m) % 4]
                    Vk = V.ap[:, kb, :]
                    fns = []
                    for acc, lt in ((O[m], Vk), (Dn[m], ones_bf)):
                        if kb == 0:
                            fns.append(lambda acc=acc: nc.tensor.matmul(acc.ap[:, 0:512], ones_bf, zer.ap[:, 0:512], start=True, stop=False))
                        fns.append(lambda acc=acc, lt=lt, E=E, c0=c0, lastk=lastk: nc.tensor.matmul(acc.ap[:, c0:512], lt, E.ap[:, c0:512], start=False, stop=lastk))
                    fw.op(PE, fns, reads=[V.t, E.t, cbf.t, zer.t], writes=[O[m].t, Dn[m].t])

            stage1(0)
            for n in range(len(steps)):
                if n + 1 < len(steps):
                    stage1(n + 1)
                stage2(n)
                qt, kb = steps[n]
                if kb == 4 * qt + 3:
                    qs = slice(qt * 512, (qt + 1) * 512)
                    fw.op(DVE, lambda: nc.vector.reciprocal(out=tA.ap[:], in_=Dn[0].ap[:]), reads=[Dn[0].t], writes=[tA.t])
                    fw.op(DVE, lambda: nc.vector.tensor_tensor(out=tA.ap[:], in0=O[0].ap[:], in1=tA.ap[:], op=ALU.mult), reads=[O[0].t, tA.t], writes=[tA.t])
                    fw.op(DVE, lambda: nc.vector.reciprocal(out=tB.ap[:], in_=Dn[1].ap[:]), reads=[Dn[1].t], writes=[tB.t])
                    fw.op(DVE, lambda: nc.vector.tensor_tensor(out=tB.ap[:], in0=O[1].ap[:], in1=tB.ap[:], op=ALU.mult), reads=[O[1].t, tB.t], writes=[tB.t])
                    fw.op(DVE, lambda qs=qs: nc.vector.scalar_tensor_tensor(out=ou.ap[:, qs], in0=tB.ap[:], scalar=neglam, in1=tA.ap[:], op0=ALU.mult, op1=ALU.add),
                          reads=[tA.t, tB.t, small.t], writes=[ou.t])
            if h == 0:
                dump("ou0", ou.ap[:], [128, S], [ou.t])
            for tt in range(4):
                ps = sbank()
                ts = slice(tt * 512, (tt + 1) * 512)
                oq = osq[tt % 2]
                fw.op(DVE, lambda ts=ts, oq=oq: nc.vector.tensor_tensor(out=oq.ap[:], in0=ou.ap[:, ts], in1=ou.ap[:, ts], op=ALU.mult), reads=[ou.t], writes=[oq.t])
                fw.op(PE, lambda ps=ps, oq=oq: nc.tensor.matmul(ps.ap[:], ones_bf, oq.ap[:], start=True, stop=True), reads=[oq.t, cbf.t], writes=[ps.t])
                tS = tA if tt % 2 == 0 else tB
                fw.op(ACT, lambda ps=ps, tS=tS: nc.scalar.activation(out=tS.ap[:], in_=ps.ap[:], func=AF.Sqrt, bias=epsb, scale=1.0 / 128),
                      reads=[ps.t, small.t], writes=[tS.t])
                fw.op(DVE, lambda tS=tS: nc.vector.reciprocal(out=tS.ap[:], in_=tS.ap[:]), reads=[tS.t], writes=[tS.t])
                fw.op(DVE, lambda ts=ts, tS=tS: nc.vector.scalar_tensor_tensor(out=OT.ap[:, ts], in0=ou.ap[:, ts], scalar=gsub, in1=tS.ap[:], op0=ALU.mult, op1=ALU.mult),
                      reads=[ou.t, tS.t, small.t], writes=[OT.t])
            if h == 0:
                dump("OT0", OT.ap[:], [128, S], [OT.t])
            for c in range(NCH):
                for hf in range(2):
                    ps = sbank()
                    w, wv = Wo[hf]
                    fw.op(PE, lambda ps=ps, c=c, wv=wv, h=h: nc.tensor.matmul(ps.ap[:], OT.ap[:, c * 128:(c + 1) * 128], wv[:, h, :], start=True, stop=True),
                          reads=[OT.t, w.t], writes=[ps.t])
                    xs = X[:, c, hf * 512:(hf + 1) * 512]
                    fw.op(DVE, lambda ps=ps, xs=xs: nc.vector.tensor_tensor(out=xs, in0=ps.ap[:], in1=xs, op=ALU.add), reads=[ps.t, TX[c]], writes=[TX[c]])
        ws.release(u_wo[0])
        ws.release(u_wo[1])

    def moe_layer(i):
        with ExitStack() as st:
            hT = mk(st, "hTm", [128, KC, S], BF16)
            scr = [mk(st, "mscr%d" % k, [128, D]) for k in range(2)]
            ss = mk(st, "mss", [128, NCH])
            rstd = mk(st, "mrstd", [128, NCH])
            hlo = mk(st, "hlo", [128, KC, 128], BF16)
            rwhi = mk(st, "rwhi", [128, KC, 36], BF16)
            rwlo = mk(st, "rwlo", [128, KC, 36], BF16)
            rw = mk(st, "rw", [128, KC, 36])
            rb = mk(st, "rb", [128, 36])
            lg = mk(st, "lg", [128, 36])
            rt = mk(st, "rt", [128, 64])
            em = mk(st, "em", [128, 32])
            oh = mk(st, "oh", [128, 32])
            gw = mk(st, "gw", [128, NCH, NE])
            hid = [mk(st, "hid%d" % k, [128, 4, 512], BF16) for k in range(2)]
            sg = [mk(st, "sg%d" % k, [128, 512], BF16) for k in range(2)]
            u_sh = add_mod_units(i, 3)
            u_sc = add_mod_units(i, 4)
            u_g = add_mod_units(i, 5)
            fw.dma(SP, [(rw.ap[:], rw_d[i].rearrange("(kc p) n -> p kc n", p=128))], writes=[rw.t])
            fw.dma(SP, [(rb.ap[:], rb_d[i:i + 1, :].broadcast_to([128, 36]))], writes=[rb.t])
            fw.op(DVE, lambda: nc.vector.tensor_copy(out=rwhi.ap[:], in_=rw.ap[:]), reads=[rw.t], writes=[rwhi.t])
            fw.op(DVE, lambda: nc.vector.tensor_tensor(out=rwlo.ap[:], in0=rw.ap[:], in1=rwhi.ap[:], op=ALU.subtract), reads=[rw.t, rwhi.t], writes=[rwlo.t])
            gen_mod(i, 3, BS, u_sh)
            gen_mod(i, 4, BA, u_sc, norm_g=n2_d[i:i + 1, :], tmp=scr[0])
            gen_mod(i, 5, BG, u_g)

            def expert_units(e):
                ug = ws.add(lambda slot, e=e: [(slot[:, :].rearrange("p (kc f) -> p kc f", kc=KC), wg_d[i, e].rearrange("(kc p) f -> p kc f", p=128))])
                uu = ws.add(lambda slot, e=e: [(slot[:, :].rearrange("p (kc f) -> p kc f", kc=KC), wu_d[i, e].rearrange("(kc p) f -> p kc f", p=128))])
                ud = ws.add(lambda slot, e=e: [(slot[:, :].rearrange("p (fc d) -> p fc d", fc=4), wd_d[i, e].rearrange("(fc p) d -> p fc d", p=128))])
                return (ug, uu, ud)
            eu = [expert_units(e) for e in range(NE)]
            for u in eu[0]:
                ws.ensure(u)

            r_ = rt.ap

            def router_cb(c, h, pss):
                cs = slice(c * 128, (c + 1) * 128)
                for b in range(2):
                    fw.op(DVE, lambda b=b: nc.vector.tensor_tensor(out=hlo.ap[:, 4 * b:4 * b + 4, :], in0=pss[b].ap[:, :].rearrange("p (k t) -> p k t", k=4),
                                                                  in1=hT.ap[:, 4 * b:4 * b + 4, cs], op=ALU.subtract),
                          reads=[pss[b].t, hT.t], writes=[hlo.t])
                ps = PS[0]
                fns = []
                for kc in range(KC):
                    fns.append(lambda kc=kc: nc.tensor.matmul(ps.ap[:, 0:36], hT.ap[:, kc, cs], rwhi.ap[:, kc, :], start=(kc == 0), stop=False))
                    fns.append(lambda kc=kc: nc.tensor.matmul(ps.ap[:, 0:36], hlo.ap[:, kc, :], rwhi.ap[:, kc, :], start=False, stop=False))
                    fns.append(lambda kc=kc: nc.tensor.matmul(ps.ap[:, 0:36], hT.ap[:, kc, cs], rwlo.ap[:, kc, :], start=False, stop=(kc == KC - 1)))
                fw.op(PE, fns, reads=[hT.t, hlo.t, rwhi.t, rwlo.t], writes=[ps.t])
                V_ = nc.vector
                import os
                if os.environ.get('ROUTER_MM_ONLY'):
                    fw.op(DVE, lambda: V_.tensor_tensor(out=lg.ap[:], in0=ps.ap[:, 0:36], in1=rb.ap[:], op=ALU.add), reads=[ps.t, rb.t], writes=[lg.t])
                    return
                ops = []
                ops.append((lambda: V_.tensor_tensor(out=lg.ap[:], in0=ps.ap[:, 0:36], in1=rb.ap[:], op=ALU.add), [ps.t, rb.t], [lg.t]))
                ops.append((lambda: V_.reduce_max(out=r_[:, 0:1], in_=lg.ap[:, 0:4], axis=AX.X), [lg.t], [rt.t]))
                ops.append((lambda: V_.tensor_scalar(out=r_[:, 1:2], in0=r_[:, 0:1], scalar1=-1.0, scalar2=None, op0=ALU.mult), [rt.t], [rt.t]))
                ops.append((lambda: V_.tensor_scalar(out=r_[:, 4:8], in0=lg.ap[:, 0:4], scalar1=r_[:, 0:1], scalar2=None, op0=ALU.is_equal), [lg.t, rt.t], [rt.t]))
                for o_ in ops:
                    fw.op(DVE, o_[0], reads=o_[1], writes=o_[2])
                fw.op(ACT, lambda: nc.scalar.activation(out=r_[:, 8:12], in_=lg.ap[:, 0:4], func=AF.Exp, bias=r_[:, 1:2], scale=1.0), reads=[lg.t, rt.t], writes=[rt.t])
                ops = []
                ops.append((lambda: V_.reduce_sum(out=r_[:, 2:3], in_=r_[:, 8:12], axis=AX.X), [rt.t], [rt.t]))
                ops.append((lambda: V_.reciprocal(out=r_[:, 3:4], in_=r_[:, 2:3]), [rt.t], [rt.t]))
                ops.append((lambda: V_.tensor_scalar(out=r_[:, 12:16], in0=r_[:, 4:8], scalar1=-1.0, scalar2=1.0e9, op0=ALU.add, op1=ALU.mult), [rt.t], [rt.t]))
                for g in range(4):
                    ops.append((lambda g=g: V_.tensor_scalar(out=em.ap[:, 8 * g:8 * g + 8], in0=lg.ap[:, 4 + 8 * g:12 + 8 * g], scalar1=r_[:, 12 + g:13 + g], scalar2=None, op0=ALU.add),
                                [lg.t, rt.t], [em.t]))
                ops.append((lambda: V_.max(out=r_[:, 16:24], in_=em.ap[:]), [em.t], [rt.t]))
                ops.append((lambda: V_.tensor_tensor(out=r_[:, 24:25], in0=r_[:, 17:18], in1=r_[:, 16:17], op=ALU.subtract), [rt.t], [rt.t]))
                for o_ in ops:
                    fw.op(DVE, o_[0], reads=o_[1], writes=o_[2])
                fw.op(ACT, lambda: nc.scalar.activation(out=r_[:, 25:26], in_=r_[:, 24:25], func=AF.Exp), reads=[rt.t], writes=[rt.t])
                ops = []
                ops.append((lambda: V_.tensor_scalar(out=r_[:, 26:27], in0=r_[:, 25:26], scalar1=1.0, scalar2=None, op0=ALU.add), [rt.t], [rt.t]))
                ops.append((lambda: V_.reciprocal(out=r_[:, 27:28], in_=r_[:, 26:27]), [rt.t], [rt.t]))
                ops.append((lambda: V_.tensor_tensor(out=r_[:, 28:29], in0=r_[:, 27:28], in1=r_[:, 3:4], op=ALU.mult), [rt.t], [rt.t]))
                ops.append((lambda: V_.tensor_tensor(out=r_[:, 29:30], in0=r_[:, 28:29], in1=r_[:, 25:26], op=ALU.mult), [rt.t], [rt.t]))
                ops.append((lambda: V_.tensor_scalar(out=oh.ap[:], in0=em.ap[:], scalar1=r_[:, 16:17], scalar2=r_[:, 28:29], op0=ALU.is_equal, op1=ALU.mult), [em.t, rt.t], [oh.t]))
                ops.append((lambda: V_.tensor_scalar(out=em.ap[:], in0=em.ap[:], scalar1=r_[:, 17:18], scalar2=r_[:, 29:30], op0=ALU.is_equal, op1=ALU.mult), [em.t, rt.t], [em.t]))
                ops.append((lambda c=c: V_.tensor_tensor(out=gw.ap[:, c, :], in0=oh.ap[:], in1=em.ap[:], op=ALU.add), [oh.t, em.t], [gw.t]))
                for o_ in ops:
                    fw.op(DVE, o_[0], reads=o_[1], writes=o_[2])

            import os
            norm_modulate(BA, BS, hT, scr, ss, rstd, hT32_cb=(None if os.environ.get('NOROUTER') else router_cb))
            dump("gw%d" % i, gw.ap[:], [128, NCH, NE], [gw.t])
            dump("hTm%d" % i, hT.ap[:], [128, KC, S], [hT.t])

            n_exp = NE if upto not in ("moe_few", "moe_router") else (2 if upto == "moe_few" else 0)
            rot = [0, 0]
            for e in range(n_exp):
                if e + 1 < n_exp:
                    for u in eu[e + 1]:
                        ws.ensure(u)
                Wg, Wu, Wd = [ws.get(u) for u in eu[e]]
                wgv = Wg.ap[:, :].rearrange("p (kc f) -> p kc f", kc=KC)
                wuv = Wu.ap[:, :].rearrange("p (kc f) -> p kc f", kc=KC)
                wdv = Wd.ap[:, :].rearrange("p (fc d) -> p fc d", fc=4)
                for fc in range(4):
                    fw.op(DVE, lambda fc=fc: nc.vector.tensor_tensor(out=wdv[:, fc, :], in0=wdv[:, fc, :], in1=BG.ap[:], op=ALU.mult), reads=[Wd.t, BG.t], writes=[Wd.t])
                for tt in range(4):
                    ts = slice(tt * 512, (tt + 1) * 512)
                    hd = hid[tt % 2]
                    for f in range(4):
                        rot[0] += 1
                        pg = PS[(2 * rot[0]) % 4]
                        pu = PS[(2 * rot[0]) % 4 + 1]
                        fw.op(PE, [lambda kc=kc, pg=pg, f=f: nc.tensor.matmul(pg.ap[:], wgv[:, kc, f * 128:(f + 1) * 128], hT.ap[:, kc, ts], start=(kc == 0), stop=(kc == KC - 1))
                                   for kc in range(KC)], reads=[Wg.t, hT.t], writes=[pg.t])
                        fw.op(PE, [lambda kc=kc, pu=pu, f=f: nc.tensor.matmul(pu.ap[:], wuv[:, kc, f * 128:(f + 1) * 128], hT.ap[:, kc, ts], start=(kc == 0), stop=(kc == KC - 1))
                                   for kc in range(KC)], reads=[Wu.t, hT.t], writes=[pu.t])
                        s_ = sg[rot[0] % 2]
                        fw.op(ACT, lambda pg=pg, s_=s_: nc.scalar.activation(out=s_.ap[:], in_=pg.ap[:], func=AF.Silu), reads=[pg.t], writes=[s_.t])
                        fw.op(DVE, lambda pu=pu, s_=s_, f=f, hd=hd: nc.vector.tensor_tensor(out=hd.ap[:, f, :], in0=pu.ap[:], in1=s_.ap[:], op=ALU.mult),
                              reads=[pu.t, s_.t], writes=[hd.t])
                    for cc in range(4):
                        c = 4 * tt + cc
                        for hf in range(2):
                            rot[1] += 1
                            ps = PS[4 + rot[1] % 4]
                            fw.op(PE, [lambda f=f, ps=ps, cc=cc, hf=hf, hd=hd: nc.tensor.matmul(ps.ap[:], hd.ap[:, f, cc * 128:(cc + 1) * 128], wdv[:, f, hf * 512:(hf + 1) * 512],
                                                                                         start=(f == 0), stop=(f == 3)) for f in range(4)],
                                  reads=[hd.t, Wd.t], writes=[ps.t])
                            xs = X[:, c, hf * 512:(hf + 1) * 512]
                            fw.op(DVE, lambda ps=ps, xs=xs, c=c, e=e: nc.vector.scalar_tensor_tensor(out=xs, in0=ps.ap[:], scalar=gw.ap[:, c, e:e + 1], in1=xs, op0=ALU.mult, op1=ALU.add),
                                  reads=[ps.t, gw.t, TX[c]], writes=[TX[c]])
                for u in eu[e]:
                    ws.release(u)
            fw.barrier()

    def moe_layer_sparse(i):
        V_ = nc.vector
        sbm_bf = cbf.ap[:, CBF_SBM:CBF_SBM + 128]
        iota_p = c32.ap[:, C32_IOTA:C32_IOTA + 1]
        with ExitStack() as st:
            dest = [mk(st, "dest%d" % k, [128, NCH], I32) for k in range(2)]
            w12 = mk(st, "w12", [128, NCH, 2])
            idxb = mk(st, "idxb", [128, 2, 64], I32)
            u_sh = add_mod_units(i, 3)
            u_sc = add_mod_units(i, 4)
            u_g = add_mod_units(i, 5)
            with ExitStack() as st1:
                hb = mk(st1, "hb", [128, NCH, D], BF16)
                hTc = [mk(st1, "hTc%d" % k, [128, KC, 128], BF16) for k in range(2)]
                scr = [mk(st1, "sscr%d" % k, [128, D]) for k in range(2)]
                ss = mk(st1, "sss", [128, NCH])
                rstd = mk(st1, "srstd", [128, NCH])
                hlo = mk(st1, "shlo", [128, KC, 128], BF16)
                rwhi = mk(st1, "srwhi", [128, KC, 36], BF16)
                rwlo = mk(st1, "srwlo", [128, KC, 36], BF16)
                rw = mk(st1, "srw", [128, KC, 36])
                rb = mk(st1, "srb", [128, 36])
                lg = mk(st1, "slg", [128, 36])
                rt = mk(st1, "srt", [128, 64])
                em = mk(st1, "sem", [128, 32])
                m12 = [mk(st1, "m12_%d" % k, [128, NCH, NE]) for k in range(2)]
                ohb = mk(st1, "ohb", [128, NCH, NE], BF16)
                rank = mk(st1, "rank", [128, NCH, NE])
                cmpb = mk(st1, "cmpb", [128, 64, NE])
                cnt = mk(st1, "cnt", [128, 6, NE])
                ebf = mk(st1, "ebf", [128, 64])
                destf = mk(st1, "destf", [128, NCH])
                fw.dma(SP, [(rw.ap[:], rw_d[i].rearrange("(kc p) n -> p kc n", p=128))], writes=[rw.t])
                fw.dma(SP, [(rb.ap[:], rb_d[i:i + 1, :].broadcast_to([128, 36]))], writes=[rb.t])
                fw.op(DVE, lambda: V_.tensor_copy(out=rwhi.ap[:], in_=rw.ap[:]), reads=[rw.t], writes=[rwhi.t])
                fw.op(DVE, lambda: V_.tensor_tensor(out=rwlo.ap[:], in0=rw.ap[:], in1=rwhi.ap[:], op=ALU.subtract), reads=[rw.t, rwhi.t], writes=[rwlo.t])
                gen_mod(i, 3, BS, u_sh)
                gen_mod(i, 4, BA, u_sc, norm_g=n2_d[i:i + 1, :], tmp=scr[0])
                gen_mod(i, 5, BG, u_g)
                r_ = rt.ap

                lga = mk(st1, "lga", [128, NCH, 36])
                ema = mk(st1, "ema", [128, NCH, NE])
                g4 = [mk(st1, "g4_%d" % k, [128, NCH, 4]) for k in range(3)]
                r16 = [mk(st1, "r16_%d" % k, [128, NCH]) for k in range(8)]

                def router_cb(c, h, pss, hk):
                    for b in range(2):
                        fw.op(DVE, lambda b=b: V_.tensor_tensor(out=hlo.ap[:, 4 * b:4 * b + 4, :], in0=pss[b].ap[:, :].rearrange("p (k t) -> p k t", k=4),
                                                               in1=hk.ap[:, 4 * b:4 * b + 4, :], op=ALU.subtract), reads=[pss[b].t, hk.t], writes=[hlo.t])
                    fw.op(ACT, lambda: nc.scalar.copy(out=hb.ap[:, c, :], in_=h.ap[:]), reads=[h.t], writes=[hb.t])
                    ps = PS[c % 2]
                    fns = []
                    for kc in range(KC):
                        fns.append(lambda kc=kc: nc.tensor.matmul(ps.ap[:, 0:36], hk.ap[:, kc, :], rwhi.ap[:, kc, :], start=(kc == 0), stop=False))
                        fns.append(lambda kc=kc: nc.tensor.matmul(ps.ap[:, 0:36], hlo.ap[:, kc, :], rwhi.ap[:, kc, :], start=False, stop=False))
                        fns.append(lambda kc=kc: nc.tensor.matmul(ps.ap[:, 0:36], hk.ap[:, kc, :], rwlo.ap[:, kc, :], start=False, stop=(kc == KC - 1)))
                    fw.op(PE, fns, reads=[hk.t, hlo.t, rwhi.t, rwlo.t], writes=[ps.t])
                    fw.op(DVE, lambda: V_.tensor_tensor(out=lga.ap[:, c, :], in0=ps.ap[:, 0:36], in1=rb.ap[:], op=ALU.add), reads=[ps.t, rb.t], writes=[lga.t])

                norm_modulate(BA, BS, None, scr, ss, rstd, hT32_cb=router_cb, hT_chunks=hTc)

                def bc(ap2, n):
                    return ap2.unsqueeze(2).to_broadcast([128, NCH, n])
                gl = lga.ap[:, :, 0:4]
                gmax, gsum, pg, top1, top2, dd, ed, tmp = [r.ap[:] for r in r16]
                T16 = [r.t for r in r16]
                G4 = [g.t for g in g4]

                def dv(fn, reads, writes):
                    fw.op(DVE, fn, reads=reads, writes=writes)
                dv(lambda: V_.tensor_reduce(out=gmax, in_=gl, axis=AX.X, op=ALU.max), [lga.t], [T16[0]])
                dv(lambda: V_.tensor_tensor(out=g4[0].ap[:], in0=gl, in1=bc(gmax, 4), op=ALU.is_equal), [lga.t, T16[0]], [G4[0]])
                dv(lambda: V_.tensor_tensor(out=g4[1].ap[:], in0=gl, in1=bc(gmax, 4), op=ALU.subtract), [lga.t, T16[0]], [G4[1]])
                fw.op(ACT, lambda: nc.scalar.activation(out=g4[1].ap[:], in_=g4[1].ap[:], func=AF.Exp), reads=[G4[1]], writes=[G4[1]])
                dv(lambda: V_.tensor_reduce(out=gsum, in_=g4[1].ap[:], axis=AX.X, op=ALU.add), [G4[1]], [T16[1]])
                dv(lambda: V_.reciprocal(out=pg, in_=gsum), [T16[1]], [T16[2]])
                dv(lambda: V_.tensor_scalar(out=g4[2].ap[:], in0=g4[0].ap[:], scalar1=-1.0, scalar2=1.0e9, op0=ALU.add, op1=ALU.mult), [G4[0]], [G4[2]])
                em4 = ema.ap[:, :, :].rearrange("p c (g e) -> p c g e", g=4)
                ea4 = lga.ap[:, :, 4:36].rearrange("p c (g e) -> p c g e", g=4)
                dv(lambda: V_.tensor_tensor(out=em4, in0=ea4, in1=g4[2].ap[:].unsqueeze(3).to_broadcast([128, NCH, 4, 8]), op=ALU.add), [lga.t, G4[2]], [ema.t])
                dv(lambda: V_.tensor_reduce(out=top1, in_=ema.ap[:], axis=AX.X, op=ALU.max), [ema.t], [T16[3]])
                dv(lambda: V_.tensor_tensor(out=m12[0].ap[:], in0=ema.ap[:], in1=bc(top1, NE), op=ALU.is_equal), [ema.t, T16[3]], [m12[0].t])
                dv(lambda: V_.scalar_tensor_tensor(out=ema.ap[:], in0=m12[0].ap[:], scalar=-1.0e9, in1=ema.ap[:], op0=ALU.mult, op1=ALU.add), [m12[0].t, ema.t], [ema.t])
                dv(lambda: V_.tensor_reduce(out=top2, in_=ema.ap[:], axis=AX.X, op=ALU.max), [ema.t], [T16[4]])
                dv(lambda: V_.tensor_tensor(out=m12[1].ap[:], in0=ema.ap[:], in1=bc(top2, NE), op=ALU.is_equal), [ema.t, T16[4]], [m12[1].t])
                dv(lambda: V_.tensor_tensor(out=dd, in0=top2, in1=top1, op=ALU.subtract), [T16[3], T16[4]], [T16[5]])
                fw.op(ACT, lambda: nc.scalar.activation(out=ed, in_=dd, func=AF.Exp), reads=[T16[5]], writes=[T16[6]])
                dv(lambda: V_.tensor_scalar(out=tmp, in0=ed, scalar1=1.0, scalar2=None, op0=ALU.add), [T16[6]], [T16[7]])
                dv(lambda: V_.reciprocal(out=tmp, in_=tmp), [T16[7]], [T16[7]])
                dv(lambda: V_.tensor_tensor(out=w12.ap[:, :, 0], in0=tmp, in1=pg, op=ALU.mult), [T16[7], T16[2]], [w12.t])
                dv(lambda: V_.tensor_tensor(out=w12.ap[:, :, 1], in0=w12.ap[:, :, 0], in1=ed, op=ALU.mult), [w12.t, T16[6]], [w12.t])
                dv(lambda: V_.tensor_tensor(out=ohb.ap[:], in0=m12[0].ap[:], in1=m12[1].ap[:], op=ALU.add), [m12[0].t, m12[1].t], [ohb.t])
                pr = PS[2]
                fns = []
                for c in range(NCH):
                    for c2 in range(c):
                        fns.append(lambda c=c, c2=c2: nc.tensor.matmul(pr.ap[:, c * 32:(c + 1) * 32], ones_bf, ohb.ap[:, c2, :], start=(c2 == 0), stop=False))
                    fns.append(lambda c=c: nc.tensor.matmul(pr.ap[:, c * 32:(c + 1) * 32], sbm_bf, ohb.ap[:, c, :], start=(c == 0), stop=True))
                fw.op(PE, fns, reads=[ohb.t, cbf.t], writes=[pr.t])
                dv(lambda: V_.tensor_copy(out=rank.ap[:], in_=pr.ap[:, :].rearrange("p (c e) -> p c e", e=NE)), [pr.t], [rank.t])
                pc = PS[1]
                fw.op(PE, [lambda c2=c2: nc.tensor.matmul(pc.ap[:, 0:32], ones_bf, ohb.ap[:, c2, :], start=(c2 == 0), stop=(c2 == NCH - 1)) for c2 in range(NCH)],
                      reads=[ohb.t, cbf.t], writes=[pc.t])
                cn = cnt.ap
                fw.op(DVE, lambda: V_.tensor_copy(out=cn[:, 0, :], in_=pc.ap[:, 0:32]), reads=[pc.t], writes=[cnt.t])
                fw.op(DVE, lambda: V_.memset(cn[:, 1, :], 0.0), writes=[cnt.t])
                for j in range(16):
                    fw.op(DVE, lambda j=j: V_.scalar_tensor_tensor(out=cn[:, 1, :], in0=cn[:, 0, :], scalar=float(128 * j), in1=cn[:, 1, :], op0=ALU.is_gt, op1=ALU.add),
                          reads=[cnt.t], writes=[cnt.t])
                fw.op(DVE, lambda: V_.tensor_tensor_scan(out=cn[:, 2, :], data0=cn[:, 1, :], data1=cn[:, 1, :], initial=0.0, op0=ALU.add, op1=ALU.bypass),
                      reads=[cnt.t], writes=[cnt.t])
                fw.op(DVE, lambda: V_.tensor_tensor(out=cn[:, 3, :], in0=cn[:, 2, :], in1=cn[:, 1, :], op=ALU.subtract), reads=[cnt.t], writes=[cnt.t])
                fw.op(DVE, lambda: V_.tensor_scalar(out=cn[:, 3, :], in0=cn[:, 3, :], scalar1=128.0, scalar2=None, op0=ALU.mult), reads=[cnt.t], writes=[cnt.t])
                dump("cnt%d" % i, cn[:, 0:4, :], [128, 4, NE], [cnt.t])
                for c in range(NCH):
                    fw.op(DVE, lambda c=c: V_.tensor_tensor(out=rank.ap[:, c, :], in0=rank.ap[:, c, :], in1=cn[:, 3, :], op=ALU.add), reads=[rank.t, cnt.t], writes=[rank.t])
                for k in range(2):
                    fw.op(DVE, lambda k=k: V_.tensor_tensor(out=m12[k].ap[:], in0=m12[k].ap[:], in1=rank.ap[:], op=ALU.mult), reads=[m12[k].t, rank.t], writes=[m12[k].t])
                    fw.op(DVE, lambda k=k: V_.reduce_sum(out=destf.ap[:], in_=m12[k].ap[:], axis=AX.X), reads=[m12[k].t], writes=[destf.t])
                    fw.op(DVE, lambda k=k: V_.tensor_copy(out=dest[k].ap[:], in_=destf.ap[:]), reads=[destf.t], writes=[dest[k].t])
                    dump("dest%d_%d" % (k, i), destf.ap[:], [128, NCH], [destf.t])
                for b in range(64):
                    fw.op(DVE, lambda b=b: V_.tensor_scalar(out=cmpb.ap[:, b, :], in0=cn[:, 2, :], scalar1=float(b), scalar2=None, op0=ALU.is_le), reads=[cnt.t], writes=[cmpb.t])
                fw.op(DVE, lambda: V_.reduce_sum(out=ebf.ap[:], in_=cmpb.ap[:], axis=AX.X), reads=[cmpb.t], writes=[ebf.t])
                fw.op(DVE, lambda: V_.tensor_scalar(out=destf.ap[:, 0:1], in0=ebf.ap[:, 0:1], scalar1=0.0, scalar2=None, op0=ALU.mult), reads=[ebf.t], writes=[destf.t])
                ebig = mk(st1, "ebig", [128, 64])
                fw.op(DVE, lambda: V_.tensor_scalar(out=ebig.ap[:], in0=ebf.ap[:], scalar1=float(NE), scalar2=1.0e9, op0=ALU.is_ge, op1=ALU.mult), reads=[ebf.t], writes=[ebig.t])
                esame = mk(st1, "esame", [128, 64])
                fw.op(DVE, lambda: V_.memset(esame.ap[:], 0.0), writes=[esame.t])
                fw.op(DVE, lambda: V_.tensor_tensor(out=esame.ap[:, 1:64], in0=ebf.ap[:, 1:64], in1=ebf.ap[:, 0:63], op=ALU.is_equal), reads=[ebf.t], writes=[esame.t])
                fw.op(DVE, lambda: V_.memset(esame.ap[:, 32:33], 0.0), writes=[esame.t])
                fw.op(DVE, lambda: V_.scalar_tensor_tensor(out=ebig.ap[:], in0=esame.ap[:], scalar=1.0e9, in1=ebig.ap[:], op0=ALU.mult, op1=ALU.add),
                      reads=[esame.t, ebig.t], writes=[ebig.t])
                fw.op(DVE, lambda: V_.tensor_scalar(out=ebf.ap[:], in0=ebf.ap[:], scalar1=float(NE - 1), scalar2=128.0, op0=ALU.min, op1=ALU.mult), reads=[ebf.t], writes=[ebf.t])
                fw.op(DVE, lambda: V_.tensor_scalar(out=ebf.ap[:], in0=ebf.ap[:], scalar1=iota_p, scalar2=None, op0=ALU.add), reads=[ebf.t, c32.t], writes=[ebf.t])
                fw.op(DVE, lambda: V_.tensor_scalar(out=ebf.ap[:], in0=ebf.ap[:], scalar1=2.0, scalar2=float(i * NE * 128 * 2), op0=ALU.mult, op1=ALU.add), reads=[ebf.t], writes=[ebf.t])
                fw.op(DVE, lambda: V_.tensor_tensor(out=ebf.ap[:], in0=ebf.ap[:], in1=ebig.ap[:], op=ALU.add), reads=[ebf.t, ebig.t], writes=[ebf.t])
                fw.op(DVE, lambda: V_.tensor_copy(out=idxb.ap[:, 0, :], in_=ebf.ap[:]), reads=[ebf.t], writes=[idxb.t])
                fw.op(DVE, lambda: V_.tensor_scalar(out=ebf.ap[:], in0=ebf.ap[:], scalar1=1.0, scalar2=None, op0=ALU.add), reads=[ebf.t], writes=[ebf.t])
                fw.op(DVE, lambda: V_.tensor_copy(out=idxb.ap[:, 1, :], in_=ebf.ap[:]), reads=[ebf.t], writes=[idxb.t])
                dump("ebf%d" % i, ebf.ap[:], [128, 64], [ebf.t])
                for c in range(NCH):
                    for k in range(2):
                        fw.dma_ind(xs_d[:, :], hb.ap[:, c, :], dest[k].ap[:, c:c + 1], True, reads=[hb.t, dest[k].t], writes=[xs_t], waw=False)
            fw.barrier()
            with ExitStack() as st2:
                xsb = [mk(st2, "xsb%d" % k, [128, D], BF16) for k in range(3)]
                xsT = [mk(st2, "xsT%d" % k, [128, KC, 128], BF16) for k in range(2)]
                sgb = [mk(st2, "sgb%d" % k, [128, 512], BF16) for k in range(2)]
                hdb = [mk(st2, "hdb%d" % k, [128, 512], BF16) for k in range(2)]
                hdT = [mk(st2, "hdT%d" % k, [128, 4, 128], BF16) for k in range(2)]
                yst = [mk(st2, "yst%d" % k, [128, D]) for k in range(2)]
                tabs = [wg_d.rearrange("l e (p a k) f -> (l e p a) (k f)", a=2, k=4), wu_d.rearrange("l e (p a k) f -> (l e p a) (k f)", a=2, k=4),
                        wd_d.rearrange("l e (p a k) d -> (l e p a) (k d)", a=2, k=2)]

                def blk_units(b):
                    us = []
                    for tab in tabs:
                        def f(slot_ap, tab=tab, b=b):
                            def emit(slot):
                                fw.dma_ind([slot.ap[:, hf * 2048:(hf + 1) * 2048] for hf in range(2)], [tab[:, :]] * 2,
                                           [idxb.ap[:, hf, b:b + 1] for hf in range(2)], False, reads=[idxb.t], writes=[slot.t], bounds=2 * NE * 128 * 2 - 1)
                            return emit
                        us.append(ws.add(f))
                    return us
                NB = 64 if upto != "moe_few" else 3
                bu = [blk_units(b) for b in range(NB)]
                for u in bu[0]:
                    ws.ensure(u)
                pT = PS[0].ap[:, :].bitcast(BF16)
                pH = PS[5].ap[:, :].bitcast(BF16)

                order = [k // 2 + 32 * (k % 2) for k in range(64)] if NB == 64 else list(range(NB))
                def stA(i):
                    b = order[i]
                    for u in bu[b]:
                        ws.ensure(u)
                    x_ = xsb[i % 3]
                    if i == 0:
                        fw.dma(SP, [(x_.ap[:], xs_d[b * 128:(b + 1) * 128, :])], reads=[xs_t], writes=[x_.t])
                    if i + 1 < NB:
                        xn = xsb[(i + 1) % 3]
                        bn = order[i + 1]
                        fw.dma(SP, [(xn.ap[:], xs_d[bn * 128:(bn + 1) * 128, :])], reads=[xs_t], writes=[xn.t])
                    xv = x_.ap[:, :].rearrange("s (p k) -> s k p", k=8)
                    fw.op(PE, [lambda kc=kc: nc.tensor.transpose(pT[:, kc * 128:(kc + 1) * 128], xv[:, kc, :], ident_bf) for kc in range(KC)],
                          reads=[x_.t, cbf.t], writes=[PS[0].t])
                    xT = xsT[i % 2]
                    fw.op(ACT, lambda: nc.scalar.copy(out=xT.ap[:, 0:4, :], in_=pT[:, 0:512].rearrange("p (k t) -> p k t", k=4)), reads=[PS[0].t], writes=[xT.t])
                    fw.op(DVE, lambda: V_.tensor_copy(out=xT.ap[:, 4:8, :], in_=pT[:, 512:1024].rearrange("p (k t) -> p k t", k=4)), reads=[PS[0].t], writes=[xT.t])
                    Wg, Wu, Wd = [ws.get(u) for u in bu[b]]
                    wgv = Wg.ap[:, :].rearrange("p (k f) -> p k f", k=8)
                    wuv = Wu.ap[:, :].rearrange("p (k f) -> p k f", k=8)
                    pg = PS[1 + i % 2]
                    pu = PS[3 + i % 2]
                    fw.op(PE, [lambda kc=kc: nc.tensor.matmul(pg.ap[:], xT.ap[:, kc, :], wgv[:, kc, :], start=(kc == 0), stop=(kc == KC - 1)) for kc in range(KC)],
                          reads=[xT.t, Wg.t], writes=[pg.t])
                    fw.op(PE, [lambda kc=kc: nc.tensor.matmul(pu.ap[:], xT.ap[:, kc, :], wuv[:, kc, :], start=(kc == 0), stop=(kc == KC - 1)) for kc in range(KC)],
                          reads=[xT.t, Wu.t], writes=[pu.t])

                def stB(i):
                    b = order[i]
                    Wg, Wu, Wd = [ws.get(u) for u in bu[b]]
                    wdv = Wd.ap[:, :].rearrange("p (k d) -> p k d", k=4)
                    pg = PS[1 + i % 2]
                    pu = PS[3 + i % 2]
                    sg_ = sgb[i % 2]
                    hd = hdb[i % 2]
                    hT_ = hdT[i % 2]
                    ys_ = yst[i % 2]
                    fw.op(ACT, lambda: nc.scalar.activation(out=sg_.ap[:], in_=pg.ap[:], func=AF.Silu), reads=[pg.t], writes=[sg_.t])
                    fw.op(DVE, lambda: V_.tensor_tensor(out=hd.ap[:], in0=pu.ap[:], in1=sg_.ap[:], op=ALU.mult), reads=[pu.t, sg_.t], writes=[hd.t])
                    hv = hd.ap[:, :].rearrange("s (p k) -> s k p", k=4)
                    fw.op(PE, [lambda fc=fc: nc.tensor.transpose(pH[:, fc * 128:(fc + 1) * 128], hv[:, fc, :], ident_bf) for fc in range(4)],
                          reads=[hd.t, cbf.t], writes=[PS[5].t])
                    fw.op(ACT, lambda: nc.scalar.copy(out=hT_.ap[:], in_=pH[:, 0:512].rearrange("p (k t) -> p k t", k=4)), reads=[PS[5].t], writes=[hT_.t])
                    for hf in range(2):
                        py = PS[6 + hf]
                        fw.op(PE, [lambda fc=fc, py=py, hf=hf: nc.tensor.matmul(py.ap[:], hT_.ap[:, fc, :], wdv[:, fc, hf * 512:(hf + 1) * 512], start=(fc == 0), stop=(fc == 3))
                                   for fc in range(4)], reads=[hT_.t, Wd.t], writes=[py.t])
                        fw.op(DVE, lambda py=py, hf=hf: V_.tensor_tensor(out=ys_.ap[:, hf * 512:(hf + 1) * 512], in0=py.ap[:], in1=BG.ap[:, hf * 512:(hf + 1) * 512], op=ALU.mult),
                              reads=[py.t, BG.t], writes=[ys_.t])
                    fw.dma(SP, [(ys_d[b * 128:(b + 1) * 128, :], ys_.ap[:])], reads=[ys_.t], writes=[ys_t], waw=False)
                    for u in bu[b]:
                        ws.release(u)

                stA(0)
                for i in range(NB):
                    if i + 1 < NB:
                        stA(i + 1)
                    stB(i)
            fw.barrier()
            with ExitStack() as st3:
                y0 = [mk(st3, "y0_%d" % k, [128, D]) for k in range(2)]
                y1 = [mk(st3, "y1_%d" % k, [128, D]) for k in range(2)]
                for c in range(NCH):
                    a0 = y0[c % 2]
                    a1 = y1[c % 2]
                    fw.dma_ind(a0.ap[:], ys_d[:, :], dest[0].ap[:, c:c + 1], False, reads=[ys_t, dest[0].t], writes=[a0.t])
                    fw.dma_ind(a1.ap[:], ys_d[:, :], dest[1].ap[:, c:c + 1], False, reads=[ys_t, dest[1].t], writes=[a1.t])
                    fw.op(DVE, lambda: V_.scalar_tensor_tensor(out=X[:, c, :], in0=a0.ap[:], scalar=w12.ap[:, c, 0:1], in1=X[:, c, :], op0=ALU.mult, op1=ALU.add),
                          reads=[a0.t, w12.t, TX[c]], writes=[TX[c]])
                    fw.op(DVE, lambda: V_.scalar_tensor_tensor(out=X[:, c, :], in0=a1.ap[:], scalar=w12.ap[:, c, 1:2], in1=X[:, c, :], op0=ALU.mult, op1=ALU.add),
                          reads=[a1.t, w12.t, TX[c]], writes=[TX[c]])
            fw.barrier()

    def sb_attention_heads(st, hT):
        QT = mk(st, "sQT", [128, 2, S], BF16)
        fw.op(DVE, lambda: nc.vector.memset(QT.ap[:], 0.0), writes=[QT.t])
        KT = mk(st, "sKT", [128, S], BF16)
        V = mk(st, "sV", [128, NCH, 128], BF16)
        OT = mk(st, "sOT", [128, S], BF16)
        e1 = [mk(st, "e1_%d" % k, [128, 512]) for k in range(1)]
        spb = [mk(st, "sp_%d" % k, [128, 512], BF16) for k in range(6)]
        tt_ = [mk(st, "t_%d" % k, [128, 512]) for k in range(3)]
        Ab = [mk(st, "A_%d" % k, [128, 512], BF16) for k in range(4)]
        sbm_bf = cbf.ap[:, CBF_SBM:CBF_SBM + 128]
        tle = mk(st, "tle", [128, 128], BF16)
        zer = mk(st, "zer", [128, 512], BF16)
        fw.op(DVE, lambda: nc.vector.memset(zer.ap[:], 0.0), writes=[zer.t])
        fw.op(DVE, lambda: nc.vector.tensor_tensor(out=tle.ap[:], in0=ones_bf, in1=tgt_bf, op=ALU.subtract), reads=[cbf.t], writes=[tle.t])

        def pair_unit(p):
            def f(slot):
                v = slot[:, 0:3072].rearrange("p (kc f) -> p kc f", kc=KC)
                return [(v[:, :, j * 128:(j + 1) * 128], swin_d[0, :, j * D + p * 128: j * D + (p + 1) * 128].rearrange("(kc p) f -> p kc f", p=128))
                        for j in range(3)]
            return ws.add(f)
        u_pair = [pair_unit(p) for p in range(8)]
        u_wo = [ws.add(lambda slot, hf=hf: [(slot[:, :].rearrange("p (h f) -> p h f", h=8),
                                              swout_d[0, :, hf * 512:(hf + 1) * 512].rearrange("(h p) f -> p h f", p=128))]) for hf in range(2)]
        ws.ensure(u_pair[0])
        ws.ensure(u_wo[0])
        ws.ensure(u_wo[1])
        Wo = []
        for hf in range(2):
            w = ws.get(u_wo[hf])
            wv = w.ap[:, :].rearrange("p (h f) -> p h f", h=8)
            for h in range(8):
                fw.op(DVE, lambda wv=wv, h=h, hf=hf: nc.vector.tensor_tensor(out=wv[:, h, :], in0=wv[:, h, :], in1=BG.ap[:, hf * 512:(hf + 1) * 512], op=ALU.mult),
                      reads=[w.t, BG.t], writes=[w.t])
            Wo.append((w, wv))
        ev = [0]

        def evac(dst_ap, dst_t, ps, scale=None, src_ap=None):
            src = ps.ap[:] if src_ap is None else src_ap
            ev[0] += 1
            if ev[0] % 2 == 0:
                if scale is None:
                    fw.op(ACT, lambda: nc.scalar.copy(out=dst_ap, in_=src), reads=[ps.t], writes=[dst_t])
                else:
                    fw.op(ACT, lambda: nc.scalar.mul(out=dst_ap, in_=src, mul=scale), reads=[ps.t], writes=[dst_t])
            else:
                if scale is None:
                    fw.op(DVE, lambda: nc.vector.tensor_copy(out=dst_ap, in_=src), reads=[ps.t], writes=[dst_t])
                else:
                    fw.op(DVE, lambda: nc.vector.tensor_scalar(out=dst_ap, in0=src, scalar1=scale, scalar2=None, op0=ALU.mult), reads=[ps.t], writes=[dst_t])
        rot = [0]

        def zbank():
            rot[0] += 1
            return PS[4 + rot[0] % 4]

        n_pairs = 8 if upto != "attn1_few" else 1
        for p in range(n_pairs):
            if p + 1 < n_pairs:
                ws.ensure(u_pair[p + 1])
            W = ws.get(u_pair[p])
            Wv = W.ap[:, 0:3072].rearrange("p (kc f) -> p kc f", kc=KC)
            for which, dst, scale in ((0, QT, 0.125), (1, KT, None)):
                for tq in range(4):
                    ps = zbank()
                    fw.op(PE, [lambda kc=kc, ps=ps, which=which, tq=tq: nc.tensor.matmul(ps.ap[:], Wv[:, kc, which * 128:(which + 1) * 128],
                                                                                        hT.ap[:, kc, tq * 512:(tq + 1) * 512], start=(kc == 0), stop=(kc == KC - 1))
                               for kc in range(KC)], reads=[W.t, hT.t], writes=[ps.t])
                    if which == 0:
                        for m in range(2):
                            rws = slice(m * 64, (m + 1) * 64)
                            evac(QT.ap[rws, m, tq * 512:(tq + 1) * 512], QT.t, ps, scale, src_ap=ps.ap[rws, :])
                    else:
                        evac(dst.ap[:, tq * 512:(tq + 1) * 512], dst.t, ps, scale)
            for g in range(4):
                ps = zbank()
                fw.op(PE, [lambda kc=kc, k=k, ps=ps, g=g: nc.tensor.matmul(ps.ap[:, k * 128:(k + 1) * 128], hT.ap[:, kc, (4 * g + k) * 128:(4 * g + k + 1) * 128],
                                                                          Wv[:, kc, 256:384], start=(kc == 0), stop=(kc == KC - 1))
                           for k in range(4) for kc in range(KC)], reads=[W.t, hT.t], writes=[ps.t])
                evac(V.ap[:, 4 * g:4 * g + 4, :], V.t, ps, None, src_ap=ps.ap[:, :].rearrange("p (k e) -> p k e", k=4))
            ws.release(u_pair[p])
            Oacc = [PS[0], PS[1]]
            Bacc = [PS[2], PS[3]]
            items = [(qt, sblk, hh) for qt in range(4) for sblk in range(4 * qt + 3, -1, -1) for hh in range(2)]
            LOOK = 2

            def bufs(n):
                return PS[4 + n % 4], e1[0], spb[n % 6], tt_[n % 3], Ab[n % 4]

            def geo(n):
                qt, sblk, hh = items[n]
                r = sblk - 4 * qt
                c0 = max(0, r) * 128
                return qt, sblk, hh, qt * 512, r, c0, slice(c0, 512)

            def s1_pa(n):
                qt, sblk, hh, q0, r, c0, cs = geo(n)
                pz, E1, SP_, T_, A_ = bufs(n)
                rows = slice(hh * 64, (hh + 1) * 64)
                fw.op(PE, lambda: nc.tensor.matmul(pz.ap[:, c0:512], KT.ap[:, sblk * 128:(sblk + 1) * 128], QT.ap[:, hh, q0 + c0:q0 + 512], start=True, stop=True),
                      reads=[KT.t, QT.t], writes=[pz.t])
                fw.op(ACT, lambda: nc.scalar.activation(out=E1.ap[:, cs], in_=pz.ap[:, cs], func=AF.Exp), reads=[pz.t], writes=[E1.t])
                fw.op(ACT, lambda: nc.scalar.activation(out=SP_.ap[:, cs], in_=E1.ap[:, cs], func=AF.Ln, bias=1.0, scale=1.0), reads=[E1.t], writes=[SP_.t])

            def s1_d(n):
                qt, sblk, hh, q0, r, c0, cs = geo(n)
                pz, E1, SP_, T_, A_ = bufs(n)
                if r >= 0:
                    fw.op(DVE, lambda: nc.vector.tensor_tensor(out=SP_.ap[:, c0:c0 + 128], in0=SP_.ap[:, c0:c0 + 128], in1=sbm_bf, op=ALU.mult),
                          reads=[SP_.t, cbf.t], writes=[SP_.t])
                fw.op(DVE, lambda: nc.vector.tensor_tensor(out=T_.ap[:, cs], in0=pz.ap[:, cs], in1=SP_.ap[:, cs], op=ALU.subtract), reads=[pz.t, SP_.t], writes=[T_.t])

            def s2_p(n):
                qt, sblk, hh, q0, r, c0, cs = geo(n)
                pz, E1, SP_, T_, A_ = bufs(n)
                B = Bacc[hh]
                Oa = Oacc[hh]
                if sblk == 4 * qt + 3:
                    for acc in (B, Oa):
                        fw.op(PE, lambda acc=acc: nc.tensor.matmul(acc.ap[:, 0:512], tgt_bf, zer.ap[:, 0:512], start=True, stop=False), reads=[zer.t, cbf.t], writes=[acc.t])
                fw.op(PE, lambda: nc.tensor.matmul(B.ap[:, cs], tgt_bf, SP_.ap[:, cs], start=False, stop=(sblk == 0)), reads=[SP_.t, cbf.t], writes=[B.t])

            def s2_d(n):
                qt, sblk, hh, q0, r, c0, cs = geo(n)
                pz, E1, SP_, T_, A_ = bufs(n)
                B = Bacc[hh]
                fw.op(DVE, lambda: nc.vector.tensor_tensor(out=T_.ap[:, cs], in0=T_.ap[:, cs], in1=B.ap[:, cs], op=ALU.subtract), reads=[T_.t, B.t], writes=[T_.t])

            def s2_a(n):
                qt, sblk, hh, q0, r, c0, cs = geo(n)
                pz, E1, SP_, T_, A_ = bufs(n)
                fw.op(ACT, lambda: nc.scalar.activation(out=A_.ap[:, cs], in_=T_.ap[:, cs], func=AF.Exp), reads=[T_.t], writes=[A_.t])
                if r >= 0:
                    fw.op(DVE, lambda: nc.vector.tensor_tensor(out=A_.ap[:, c0:c0 + 128], in0=A_.ap[:, c0:c0 + 128], in1=sbm_bf, op=ALU.mult),
                          reads=[A_.t, cbf.t], writes=[A_.t])

            def s2b(n):
                qt, sblk, hh, q0, r, c0, cs = geo(n)
                pz, E1, SP_, T_, A_ = bufs(n)
                B = Bacc[hh]
                Oa = Oacc[hh]
                last = (sblk == 0)
                if not last:
                    fw.op(PE, lambda: nc.tensor.matmul(B.ap[:, cs], tle.ap[:], SP_.ap[:, cs], start=False, stop=False), reads=[SP_.t, tle.t], writes=[B.t])
                fw.op(PE, lambda: nc.tensor.matmul(Oa.ap[:, cs], V.ap[:, sblk, :], A_.ap[:, cs], start=False, stop=last), reads=[V.t, A_.t], writes=[Oa.t])
                if last:
                    rows = slice(hh * 64, (hh + 1) * 64)
                    evac(OT.ap[rows, q0:q0 + 512], OT.t, Oa, None, src_ap=Oa.ap[rows, :])

            NI = len(items)
            for n in range(min(LOOK, NI)):
                s1_pa(n)
                s1_d(n)
            for t in range(NI + 1):
                if t + LOOK < NI:
                    s1_pa(t + LOOK)
                if t < NI:
                    s2_p(t)
                    s2_d(t)
                if t + LOOK < NI:
                    s1_d(t + LOOK)
                if t < NI:
                    s2_a(t)
                if t >= 1:
                    s2b(t - 1)
            if p == 0:
                dump("sOT0", OT.ap[:], [128, S], [OT.t])
            for c in range(NCH):
                for hf in range(2):
                    ps = zbank()
                    w, wv = Wo[hf]
                    fw.op(PE, lambda ps=ps, c=c, wv=wv, p=p: nc.tensor.matmul(ps.ap[:], OT.ap[:, c * 128:(c + 1) * 128], wv[:, p, :], start=True, stop=True),
                          reads=[OT.t, w.t], writes=[ps.t])
                    xs = X[:, c, hf * 512:(hf + 1) * 512]
                    fw.op(DVE, lambda ps=ps, xs=xs: nc.vector.tensor_tensor(out=xs, in0=ps.ap[:], in1=xs, op=ALU.add), reads=[ps.t, TX[c]], writes=[TX[c]])
        ws.release(u_wo[0])
        ws.release(u_wo[1])

    def final_norm():
        with ExitStack() as st:
            scr = [mk(st, "fscr%d" % k, [128, D]) for k in range(2)]
            ss = mk(st, "fss", [128, NCH])
            rstd = mk(st, "frstd", [128, NCH])
            fw.dma(SP, [(BA.ap[:], fng_d[0:1, :].broadcast_to([128, D]))], writes=[BA.t])
            for c in range(NCH):
                fw.op(ACT, lambda c=c: nc.scalar.activation(out=scr[0].ap[:], in_=X[:, c, :], func=AF.Square, accum_out=ss.ap[:, c:c + 1]),
                      reads=[TX[c]], writes=[scr[0].t, ss.t])
            fw.op(DVE, lambda: nc.vector.tensor_scalar(out=rstd.ap[:], in0=ss.ap[:], scalar1=1.0 / D, scalar2=RMS_EPS, op0=ALU.mult, op1=ALU.add),
                  reads=[ss.t], writes=[rstd.t])
            fw.op(POOL, lambda: nc.gpsimd.tensor_tensor(out=rstd.ap[:], in0=rstd.ap[:], in1=neghalf.ap[:, 0:NCH], op=ALU.pow),
                  reads=[rstd.t, neghalf.t], writes=[rstd.t])
            for c in range(NCH):
                o = scr[c % 2]
                fw.op(DVE, lambda c=c, o=o: nc.vector.scalar_tensor_tensor(out=o.ap[:], in0=X[:, c, :], scalar=rstd.ap[:, c:c + 1], in1=BA.ap[:], op0=ALU.mult, op1=ALU.mult),
                      reads=[TX[c], rstd.t, BA.t], writes=[o.t])
                fw.dma(SP, [(out_d[c * 128:(c + 1) * 128, :], o.ap[:])], reads=[o.t])
            fw.barrier()

    stages = ["attn0", "moe0", "attn1", "moe1", "all"]
    lvl = {"norm1": 0, "attn": 0, "attn0": 0, "moe_router": 1, "moe_few": 1, "moe0": 1, "attn1_few": 2, "attn1": 2, "moe1": 3, "all": 4}[upto]
    with ExitStack() as st:
        with ExitStack() as st2:
            hT, lambda_init = attn_norm(0, st, st2)
        fw.barrier()
        if upto != "norm1":
            diff_attention_heads(st, hT, lambda_init)
        fw.barrier()
    moe_fn = moe_layer if SPARSE_MOE is False else moe_layer_sparse
    if lvl >= 1:
        moe_fn(0)
    if lvl >= 2:
        with ExitStack() as st:
            with ExitStack() as st2:
                hT, _ = attn_norm(1, st, st2)
            fw.barrier()
            sb_attention_heads(st, hT)
            fw.barrier()
    if lvl >= 3:
        moe_fn(1)
    if lvl >= 4:
        final_norm()
    else:
        for g in range(4):
            dst = out_d[g * 512:(g + 1) * 512, :].rearrange("(c p) d -> p c d", p=128)
            fw.dma(SP, [(dst, X[:, 4 * g:4 * g + 4, :])], reads=TX[4 * g:4 * g + 4])
        fw.barrier()
    return nc


_CACHE = {}


def _prep_inputs(inputs):
    f = lambda a: np.ascontiguousarray(np.asarray(a, dtype=np.float32))
    cbf, c32 = _host_consts()
    rw = np.concatenate([np.asarray(inputs["router_group_w"]), np.asarray(inputs["router_expert_w"])], axis=-1)
    rb = np.concatenate([np.asarray(inputs["router_group_b"]), np.asarray(inputs["router_expert_b"]).reshape(2, 32)], axis=-1)
    shared = {
        "norm1_g": f(inputs["norm1_g"]), "norm2_g": f(inputs["norm2_g"]),
        "ada_w": f(inputs["ada_w"]), "ada_b": f(inputs["ada_b"]),
        "diff_w_in": f(inputs["diff_w_in"]), "diff_w_out": f(inputs["diff_w_out"]),
        "diff_lambda_q1": f(inputs["diff_lambda_q1"]), "diff_lambda_k1": f(inputs["diff_lambda_k1"]),
        "diff_lambda_q2": f(inputs["diff_lambda_q2"]), "diff_lambda_k2": f(inputs["diff_lambda_k2"]),
        "subgT": f(np.asarray(inputs["diff_subln_g"]).reshape(128, 1)),
        "sb_w_in": f(inputs["sb_w_in"]), "sb_w_out": f(inputs["sb_w_out"]),
        "router_w": f(rw), "router_b": f(rb),
        "expert_w_gate": f(inputs["expert_w_gate"]), "expert_w_up": f(inputs["expert_w_up"]),
        "expert_w_down": f(inputs["expert_w_down"]),
        "final_norm_g": f(np.asarray(inputs["final_norm_g"]).reshape(1, D)),
        "cbf": cbf, "c32": c32,
    }
    x = np.asarray(inputs["x"], dtype=np.float32)
    c = np.asarray(inputs["c"], dtype=np.float32)
    maps = []
    for b in range(x.shape[0]):
        m = dict(shared)
        m["x"] = np.ascontiguousarray(x[b])
        m["cT"] = np.ascontiguousarray(c[b].reshape(KC, 128).T)
        maps.append(m)
    return maps


def kernel(**inputs):
    maps = _prep_inputs(inputs)
    if "nc" not in _CACHE:
        _CACHE["nc"] = build_program()
    nc = _CACHE["nc"]
    res = run_bass_kernel_spmd(nc, maps, core_ids=list(range(len(maps))))
    out = np.stack([np.asarray(r["out"], dtype=np.float32) for r in res.results], axis=0)
    return out
```
